# Optimizing a Trainium2 kernel written in Bass

```python
import math
import jax, jax.numpy as jnp
from jax import lax
import numpy as np

D_MODEL = 1024
BATCH = 8
SEQ = 2048
DEPTH = 4

N_MEM = 256
HEAD_DIM = 64
N_HEADS = D_MODEL // HEAD_DIM
ROPE_THETA = 500000.0
ROPE_DIM = HEAD_DIM // 4
Q_BLOCK = 128
N_MIXERS = 4

NSA_KV_GROUPS = 4
NSA_CMP_BLOCK = 32
NSA_CMP_STRIDE = 16
NSA_CMP_HIDDEN = 2 * HEAD_DIM
NSA_SLC_BLOCK = 64
NSA_SLC_TOPN = 8
NSA_WINDOW = 512
NSA_QCHUNK = 64
NSA_FORCE_SCORE = 1e4

MLA_Q_RANK = D_MODEL // 4
MLA_KV_RANK = D_MODEL // 8
MLA_NOPE_DIM = 64
MLA_ROPE_DIM = 32
MLA_V_DIM = 64

MOBA_BLOCK = 256
MOBA_TOPK = 3
MOBA_QCHUNK = 8

SWA_KV_HEADS = 2
SWA_WINDOW = 128

XATTN_HEADS = 4
XATTN_HEAD_DIM = D_MODEL // XATTN_HEADS

D_FF = (D_MODEL * 7) // 2
N_EXPERTS = 8
TOP_K = 2
D_FF_EXPERT = D_FF

DEEPNORM_ALPHA = (2.0 * DEPTH) ** 0.25
DEEPNORM_BETA = (8.0 * DEPTH) ** -0.25
LN_EPS = 1e-5
RMS_EPS = 1e-6

kernel_name = "hybrid_nsa_mla_moba_swa_deepnorm_moe"


def layer_norm(x, g, b):
    xf = x.astype(jnp.float32)
    mu = jnp.mean(xf, axis=-1, keepdims=True)
    var = jnp.mean(jnp.square(xf - mu), axis=-1, keepdims=True)
    y = (xf - mu) * lax.rsqrt(var + LN_EPS)
    return (y * g.astype(jnp.float32) + b.astype(jnp.float32)).astype(x.dtype)


def rms_norm(x, g):
    xf = x.astype(jnp.float32)
    y = xf * lax.rsqrt(jnp.mean(jnp.square(xf), axis=-1, keepdims=True) + RMS_EPS)
    return (y * g.astype(jnp.float32)).astype(x.dtype)


def rope_tables(positions, dim):
    inv = ROPE_THETA ** (-jnp.arange(0, dim, 2, dtype=jnp.float32) / dim)
    ang = positions.astype(jnp.float32)[..., None] * inv
    return jnp.cos(ang), jnp.sin(ang)


def apply_rope(x, cos, sin):
    rd = 2 * cos.shape[-1]
    x1, x2, xp = x[..., : rd // 2], x[..., rd // 2: rd], x[..., rd:]
    c = cos[:, None].astype(x.dtype)
    s = sin[:, None].astype(x.dtype)
    return jnp.concatenate([x1 * c - x2 * s, x2 * c + x1 * s, xp], axis=-1)


def masked_softmax(s, mask, sink=None):
    s = jnp.where(mask, s.astype(jnp.float32), -jnp.inf)
    m = jnp.max(s, axis=-1, keepdims=True)
    if sink is not None:
        m = jnp.maximum(m, sink)
    m = jnp.where(jnp.isfinite(m), m, 0.0)
    e = jnp.exp(s - m)
    denom = jnp.sum(e, axis=-1, keepdims=True)
    if sink is not None:
        denom = denom + jnp.exp(sink - m)
    return e / jnp.maximum(denom, 1e-30)


def causal_attention(q, k, v, scale):
    B, G, R, S, dk = q.shape
    nq = S // Q_BLOCK
    qb = jnp.moveaxis(q.reshape(B, G, R, nq, Q_BLOCK, dk), 3, 0)
    kpos = jnp.arange(S)

    def block(args):
        i, qi = args
        qpos = i * Q_BLOCK + jnp.arange(Q_BLOCK)
        s = jnp.einsum('bgrqd,bgkd->bgrqk', qi, k) * scale
        p = masked_softmax(s, kpos[None, :] <= qpos[:, None])
        return jnp.einsum('bgrqk,bgkd->bgrqd', p.astype(v.dtype), v)

    out = lax.map(block, (jnp.arange(nq), qb))
    return jnp.moveaxis(out, 0, 3).reshape(B, G, R, S, v.shape[-1])


def banded_attention(q, k, v, window, scale, sink=None):
    B, G, R, S, dk = q.shape
    nq = S // Q_BLOCK
    pad = -(-window // Q_BLOCK) * Q_BLOCK
    span = pad + Q_BLOCK
    kp = jnp.pad(k, ((0, 0), (0, 0), (pad, 0), (0, 0)))
    vp = jnp.pad(v, ((0, 0), (0, 0), (pad, 0), (0, 0)))
    qb = jnp.moveaxis(q.reshape(B, G, R, nq, Q_BLOCK, dk), 3, 0)

    def block(args):
        i, qi = args
        start = i * Q_BLOCK
        ki = lax.dynamic_slice_in_dim(kp, start, span, axis=2)
        vi = lax.dynamic_slice_in_dim(vp, start, span, axis=2)
        qpos = start + jnp.arange(Q_BLOCK)
        kpos = start - pad + jnp.arange(span)
        delta = qpos[:, None] - kpos[None, :]
        mask = (delta >= 0) & (delta < window) & (kpos[None, :] >= 0)
        s = jnp.einsum('bgrqd,bgkd->bgrqk', qi, ki) * scale
        p = masked_softmax(s, mask, sink)
        return jnp.einsum('bgrqk,bgkd->bgrqd', p.astype(v.dtype), vi)

    out = lax.map(block, (jnp.arange(nq), qb))
    return jnp.moveaxis(out, 0, 3).reshape(B, G, R, S, v.shape[-1])


def nsa_mixer(x, cos, sin, w_in, cmp_pe, cmp_w1, cmp_w2, w_out):
    B, S, _ = x.shape
    H, G, dh = N_HEADS, NSA_KV_GROUPS, HEAD_DIM
    R = H // G
    scale = dh ** -0.5
    q, kv, gates = jnp.split(x @ w_in, [H * dh, H * dh + 6 * G * dh], axis=-1)
    q = apply_rope(q.reshape(B, S, H, dh).transpose(0, 2, 1, 3), cos, sin).reshape(B, G, R, S, dh)
    kv = kv.reshape(B, S, 6, G, dh).transpose(2, 0, 3, 1, 4)
    k_c, v_c = apply_rope(kv[0], cos, sin), kv[1]
    k_s, v_s = apply_rope(kv[2], cos, sin), kv[3]
    k_w, v_w = apply_rope(kv[4], cos, sin), kv[5]
    tpos = jnp.arange(S)

    nc = (S - NSA_CMP_BLOCK) // NSA_CMP_STRIDE + 1
    c_start = np.arange(nc) * NSA_CMP_STRIDE
    idx = c_start[:, None] + np.arange(NSA_CMP_BLOCK)[None, :]

    def compress(t, j):
        blocks = t[:, :, idx] + cmp_pe[j]
        h = jax.nn.gelu(blocks.reshape(B, G, nc, NSA_CMP_BLOCK * dh) @ cmp_w1[j])
        return h @ cmp_w2[j]

    kc, vc = compress(k_c, 0), compress(v_c, 1)
    cmp_end = jnp.asarray(c_start + NSA_CMP_BLOCK - 1)
    s_cmp = jnp.einsum('bgrsd,bgcd->bgrsc', q, kc) * scale
    p_cmp = masked_softmax(s_cmp, cmp_end[None, :] <= tpos[:, None])
    o_cmp = jnp.einsum('bgrsc,bgcd->bgrsd', p_cmp.astype(vc.dtype), vc)

    nb = S // NSA_SLC_BLOCK
    b_start = np.arange(nb) * NSA_SLC_BLOCK
    overlap = np.clip(np.minimum(c_start[:, None] + NSA_CMP_BLOCK, b_start[None, :] + NSA_SLC_BLOCK)
                      - np.maximum(c_start[:, None], b_start[None, :]), 0, None) / NSA_CMP_BLOCK
    imp = jnp.einsum('bgrsc,cj->bgsj', p_cmp, jnp.asarray(overlap, jnp.float32))
    blk = jnp.arange(nb)[None, :]
    cur = (tpos // NSA_SLC_BLOCK)[:, None]
    forced = (blk == 0) | (blk == cur) | (blk == cur - 1)
    score = jnp.where(blk <= cur, jnp.where(forced, NSA_FORCE_SCORE, imp), -jnp.inf)
    n_sel = min(NSA_SLC_TOPN, nb)
    _, sel = lax.top_k(score, n_sel)

    ks_blk = k_s.reshape(B, G, nb, NSA_SLC_BLOCK, dh)
    vs_blk = v_s.reshape(B, G, nb, NSA_SLC_BLOCK, dh)
    nqc = S // NSA_QCHUNK
    q_ch = jnp.moveaxis(q.reshape(B, G, R, nqc, NSA_QCHUNK, dh), 3, 0)
    sel_ch = jnp.moveaxis(sel.reshape(B, G, nqc, NSA_QCHUNK, n_sel), 2, 0)
    bi = jnp.arange(B)[:, None, None, None]
    gi = jnp.arange(G)[None, :, None, None]
    L = n_sel * NSA_SLC_BLOCK

    def chunk(args):
        c, qc, sc = args
        kg = ks_blk[bi, gi, sc].reshape(B, G, NSA_QCHUNK, L, dh)
        vg = vs_blk[bi, gi, sc].reshape(B, G, NSA_QCHUNK, L, dh)
        qpos = c * NSA_QCHUNK + jnp.arange(NSA_QCHUNK)
        kpos = (sc[..., None] * NSA_SLC_BLOCK + jnp.arange(NSA_SLC_BLOCK)).reshape(B, G, NSA_QCHUNK, L)
        mask = (kpos <= qpos[:, None])[:, :, None]
        s = jnp.einsum('bgrqd,bgqkd->bgrqk', qc, kg) * scale
        p = masked_softmax(s, mask)
        return jnp.einsum('bgrqk,bgqkd->bgrqd', p.astype(vg.dtype), vg)

    o_slc = lax.map(chunk, (jnp.arange(nqc), q_ch, sel_ch))
    o_slc = jnp.moveaxis(o_slc, 0, 3).reshape(B, G, R, S, dh)

    o_win = banded_attention(q, k_w, v_w, NSA_WINDOW, scale)

    g = jax.nn.sigmoid(gates).reshape(B, S, 3, G, R).transpose(2, 0, 3, 4, 1)[..., None]
    o = g[0] * o_cmp + g[1] * o_slc + g[2] * o_win
    return o.reshape(B, H, S, dh).transpose(0, 2, 1, 3).reshape(B, S, H * dh) @ w_out


def mla_mixer(x, cos_m, sin_m, w_down, q_norm, kv_norm, w_uq, w_ukv, w_out):
    B, S, _ = x.shape
    H = N_HEADS
    cq, ckv, k_rope = jnp.split(x @ w_down, [MLA_Q_RANK, MLA_Q_RANK + MLA_KV_RANK], axis=-1)
    q = (rms_norm(cq, q_norm) @ w_uq).reshape(B, S, H, MLA_NOPE_DIM + MLA_ROPE_DIM).transpose(0, 2, 1, 3)
    kv = (rms_norm(ckv, kv_norm) @ w_ukv).reshape(B, S, H, MLA_NOPE_DIM + MLA_V_DIM).transpose(0, 2, 1, 3)
    q = jnp.concatenate([q[..., :MLA_NOPE_DIM], apply_rope(q[..., MLA_NOPE_DIM:], cos_m, sin_m)], axis=-1)
    k_rope = apply_rope(k_rope[:, None], cos_m, sin_m)
    k = jnp.concatenate([kv[..., :MLA_NOPE_DIM],
                         jnp.broadcast_to(k_rope, (B, H, S, MLA_ROPE_DIM))], axis=-1)
    v = kv[..., MLA_NOPE_DIM:]
    scale = (MLA_NOPE_DIM + MLA_ROPE_DIM) ** -0.5
    o = causal_attention(q[:, :, None], k, v, scale)[:, :, 0]
    return o.transpose(0, 2, 1, 3).reshape(B, S, H * MLA_V_DIM) @ w_out


def moba_mixer(x, cos, sin, w_in, w_out):
    B, S, _ = x.shape
    H, dh = N_HEADS, HEAD_DIM
    scale = dh ** -0.5
    q, k, v = [t.reshape(B, S, H, dh).transpose(0, 2, 1, 3) for t in jnp.split(x @ w_in, 3, axis=-1)]
    q, k = apply_rope(q, cos, sin), apply_rope(k, cos, sin)
    nb = -(-S // MOBA_BLOCK)
    sp = nb * MOBA_BLOCK
    padw = ((0, 0), (0, 0), (0, sp - S), (0, 0))
    q, k, v = jnp.pad(q, padw), jnp.pad(k, padw), jnp.pad(v, padw)
    k_blk = k.reshape(B, H, nb, MOBA_BLOCK, dh)
    v_blk = v.reshape(B, H, nb, MOBA_BLOCK, dh)
    k_mean = jnp.mean(k_blk, axis=3)
    cur = (jnp.arange(sp) // MOBA_BLOCK)[:, None]
    gate = jnp.einsum('bhsd,bhjd->bhsj', q, k_mean)
    gate = jnp.where(jnp.arange(nb)[None, :] < cur, gate, -jnp.inf)
    n_sel = max(1, min(MOBA_TOPK, nb - 1))
    _, sel = lax.top_k(gate, n_sel)
    nqc = sp // MOBA_QCHUNK
    q_ch = jnp.moveaxis(q.reshape(B, H, nqc, MOBA_QCHUNK, dh), 2, 0)
    sel_ch = jnp.moveaxis(sel.reshape(B, H, nqc, MOBA_QCHUNK, n_sel), 2, 0)
    bi = jnp.arange(B)[:, None, None, None]
    hi = jnp.arange(H)[None, :, None, None]

    def chunk(args):
        c, qc, sc = args
        q0 = c * MOBA_QCHUNK
        own = q0 // MOBA_BLOCK
        qpos = q0 + jnp.arange(MOBA_QCHUNK)
        k_own = lax.dynamic_slice_in_dim(k, own * MOBA_BLOCK, MOBA_BLOCK, axis=2)
        v_own = lax.dynamic_slice_in_dim(v, own * MOBA_BLOCK, MOBA_BLOCK, axis=2)
        own_pos = own * MOBA_BLOCK + jnp.arange(MOBA_BLOCK)
        k_sel = k_blk[bi, hi, sc].reshape(B, H, MOBA_QCHUNK, n_sel * MOBA_BLOCK, dh)
        v_sel = v_blk[bi, hi, sc].reshape(B, H, MOBA_QCHUNK, n_sel * MOBA_BLOCK, dh)
        s = jnp.concatenate([jnp.einsum('bhqd,bhkd->bhqk', qc, k_own),
                             jnp.einsum('bhqd,bhqkd->bhqk', qc, k_sel)], axis=-1) * scale
        m_own = jnp.broadcast_to(own_pos[None, :] <= qpos[:, None], (B, H, MOBA_QCHUNK, MOBA_BLOCK))
        m_sel = jnp.repeat(sc < own, MOBA_BLOCK, axis=-1)
        p = masked_softmax(s, jnp.concatenate([m_own, m_sel], axis=-1)).astype(v.dtype)
        return (jnp.einsum('bhqk,bhkd->bhqd', p[..., :MOBA_BLOCK], v_own)
                + jnp.einsum('bhqk,bhqkd->bhqd', p[..., MOBA_BLOCK:], v_sel))

    o = lax.map(chunk, (jnp.arange(nqc), q_ch, sel_ch))
    o = jnp.moveaxis(o, 0, 2).reshape(B, H, sp, dh)[:, :, :S]
    return o.transpose(0, 2, 1, 3).reshape(B, S, H * dh) @ w_out


def swa_mixer(x, cos, sin, w_in, sinks, w_out):
    B, S, _ = x.shape
    H, G, dh = N_HEADS, SWA_KV_HEADS, HEAD_DIM
    R = H // G
    q, k, v = jnp.split(x @ w_in, [H * dh, H * dh + G * dh], axis=-1)
    q = apply_rope(q.reshape(B, S, H, dh).transpose(0, 2, 1, 3), cos, sin).reshape(B, G, R, S, dh)
    k = apply_rope(k.reshape(B, S, G, dh).transpose(0, 2, 1, 3), cos, sin)
    v = v.reshape(B, S, G, dh).transpose(0, 2, 1, 3)
    sink = sinks.astype(jnp.float32).reshape(1, G, R, 1, 1)
    o = banded_attention(q, k, v, SWA_WINDOW, dh ** -0.5, sink)
    return o.reshape(B, H, S, dh).transpose(0, 2, 1, 3).reshape(B, S, H * dh) @ w_out


def cross_attention(x, mem, w_q, w_kv, w_o):
    B, S, D = x.shape
    M = mem.shape[1]
    q = (x @ w_q).reshape(B, S, XATTN_HEADS, XATTN_HEAD_DIM)
    k, v = [t.reshape(B, M, XATTN_HEADS, XATTN_HEAD_DIM) for t in jnp.split(mem @ w_kv, 2, axis=-1)]
    s = jnp.einsum('bshd,bmhd->bhsm', q, k) * XATTN_HEAD_DIM ** -0.5
    p = jax.nn.softmax(s.astype(jnp.float32), axis=-1).astype(x.dtype)
    return jnp.einsum('bhsm,bmhd->bshd', p, v).reshape(B, S, D) @ w_o


def swiglu(x, w_in, w_out):
    g, u = jnp.split(x @ w_in, 2, axis=-1)
    return (jax.nn.silu(g) * u) @ w_out


def moe_ffn(x, router, router_bias, w_in, w_out):
    B, S, D = x.shape
    t = x.reshape(B * S, D)
    logits = (t @ router).astype(jnp.float32) + router_bias.astype(jnp.float32)
    top_val, top_idx = lax.top_k(logits, TOP_K)
    gates = jax.nn.softmax(top_val, axis=-1)
    comb = jnp.sum(jax.nn.one_hot(top_idx, N_EXPERTS, dtype=jnp.float32) * gates[..., None], axis=1)
    y = jnp.zeros_like(t)
    for e in range(N_EXPERTS):
        y = y + comb[:, e:e + 1].astype(t.dtype) * swiglu(t, w_in[e], w_out[e])
    return y.reshape(B, S, D)


def setup_inputs(seed: int = 0) -> dict:
    key = jax.random.key(seed)
    ks = iter(jax.random.split(key, 96))
    f32 = jnp.float32

    def nrm(shape, std):
        return jax.random.normal(next(ks), shape, f32) * std

    def gain(n):
        return 1.0 + nrm((n,), 0.05)

    D, H, dh, beta = D_MODEL, N_HEADS, HEAD_DIM, DEEPNORM_BETA
    inp = {}
    inp["x"] = nrm((BATCH, SEQ, D), 1.0)
    inp["mem"] = nrm((BATCH, N_MEM, D), 1.0)
    offset = jax.random.randint(next(ks), (BATCH, 1), 0, 4096, dtype=jnp.int32)
    inp["positions"] = offset + jnp.arange(SEQ, dtype=jnp.int32)[None, :]

    def common_attn(i):
        inp[f"l{i}_ln1_g"] = gain(D)
        inp[f"l{i}_ln1_b"] = nrm((D,), 0.02)
        inp[f"l{i}_xq"] = nrm((D, D), D ** -0.5)
        inp[f"l{i}_xkv"] = nrm((D, 2 * D), D ** -0.5)
        inp[f"l{i}_xo"] = nrm((D, D), D ** -0.5 * beta)
        inp[f"l{i}_ln2_g"] = gain(D)
        inp[f"l{i}_ln2_b"] = nrm((D,), 0.02)

    def dense_ffn(i):
        inp[f"l{i}_ffn_w_in"] = nrm((D, 2 * D_FF), D ** -0.5)
        inp[f"l{i}_ffn_w_out"] = nrm((D_FF, D), D_FF ** -0.5 * beta)
        inp[f"l{i}_ln3_g"] = gain(D)
        inp[f"l{i}_ln3_b"] = nrm((D,), 0.02)

    def moe(i):
        inp[f"l{i}_moe_router"] = nrm((D, N_EXPERTS), D ** -0.5)
        inp[f"l{i}_moe_bias"] = nrm((N_EXPERTS,), 0.01)
        inp[f"l{i}_moe_w_in"] = nrm((N_EXPERTS, D, 2 * D_FF_EXPERT), D ** -0.5)
        inp[f"l{i}_moe_w_out"] = nrm((N_EXPERTS, D_FF_EXPERT, D), D_FF_EXPERT ** -0.5 * beta)
        inp[f"l{i}_ln3_g"] = gain(D)
        inp[f"l{i}_ln3_b"] = nrm((D,), 0.02)

    G = NSA_KV_GROUPS
    inp["l0_nsa_w_in"] = nrm((D, H * dh + 6 * G * dh + 3 * H), D ** -0.5)
    inp["l0_nsa_cmp_pe"] = nrm((2, NSA_CMP_BLOCK, dh), 0.1)
    inp["l0_nsa_cmp_w1"] = nrm((2, NSA_CMP_BLOCK * dh, NSA_CMP_HIDDEN), (NSA_CMP_BLOCK * dh) ** -0.5)
    inp["l0_nsa_cmp_w2"] = nrm((2, NSA_CMP_HIDDEN, dh), NSA_CMP_HIDDEN ** -0.5)
    inp["l0_nsa_w_out"] = nrm((H * dh, D), (H * dh) ** -0.5 * beta)
    common_attn(0)
    dense_ffn(0)

    inp["l1_mla_w_down"] = nrm((D, MLA_Q_RANK + MLA_KV_RANK + MLA_ROPE_DIM), D ** -0.5)
    inp["l1_mla_q_norm"] = gain(MLA_Q_RANK)
    inp["l1_mla_kv_norm"] = gain(MLA_KV_RANK)
    inp["l1_mla_w_uq"] = nrm((MLA_Q_RANK, H * (MLA_NOPE_DIM + MLA_ROPE_DIM)), MLA_Q_RANK ** -0.5)
    inp["l1_mla_w_ukv"] = nrm((MLA_KV_RANK, H * (MLA_NOPE_DIM + MLA_V_DIM)), MLA_KV_RANK ** -0.5)
    inp["l1_mla_w_out"] = nrm((H * MLA_V_DIM, D), (H * MLA_V_DIM) ** -0.5 * beta)
    common_attn(1)
    moe(1)

    inp["l2_moba_w_in"] = nrm((D, 3 * H * dh), D ** -0.5)
    inp["l2_moba_w_out"] = nrm((H * dh, D), (H * dh) ** -0.5 * beta)
    common_attn(2)
    dense_ffn(2)

    inp["l3_swa_w_in"] = nrm((D, H * dh + 2 * SWA_KV_HEADS * dh), D ** -0.5)
    inp["l3_swa_sinks"] = nrm((H,), 1.0)
    inp["l3_swa_w_out"] = nrm((H * dh, D), (H * dh) ** -0.5 * beta)
    common_attn(3)
    moe(3)
    return inp


def reference(x, mem, positions,
              l0_nsa_w_in, l0_nsa_cmp_pe, l0_nsa_cmp_w1, l0_nsa_cmp_w2, l0_nsa_w_out,
              l0_ln1_g, l0_ln1_b, l0_xq, l0_xkv, l0_xo, l0_ln2_g, l0_ln2_b,
              l0_ffn_w_in, l0_ffn_w_out, l0_ln3_g, l0_ln3_b,
              l1_mla_w_down, l1_mla_q_norm, l1_mla_kv_norm, l1_mla_w_uq, l1_mla_w_ukv, l1_mla_w_out,
              l1_ln1_g, l1_ln1_b, l1_xq, l1_xkv, l1_xo, l1_ln2_g, l1_ln2_b,
              l1_moe_router, l1_moe_bias, l1_moe_w_in, l1_moe_w_out, l1_ln3_g, l1_ln3_b,
              l2_moba_w_in, l2_moba_w_out,
              l2_ln1_g, l2_ln1_b, l2_xq, l2_xkv, l2_xo, l2_ln2_g, l2_ln2_b,
              l2_ffn_w_in, l2_ffn_w_out, l2_ln3_g, l2_ln3_b,
              l3_swa_w_in, l3_swa_sinks, l3_swa_w_out,
              l3_ln1_g, l3_ln1_b, l3_xq, l3_xkv, l3_xo, l3_ln2_g, l3_ln2_b,
              l3_moe_router, l3_moe_bias, l3_moe_w_in, l3_moe_w_out, l3_ln3_g, l3_ln3_b):
    cos_p, sin_p = rope_tables(positions, ROPE_DIM)
    cos_m, sin_m = rope_tables(positions, MLA_ROPE_DIM)

    mixers = [
        lambda h: nsa_mixer(h, cos_p, sin_p, l0_nsa_w_in, l0_nsa_cmp_pe, l0_nsa_cmp_w1, l0_nsa_cmp_w2, l0_nsa_w_out),
        lambda h: mla_mixer(h, cos_m, sin_m, l1_mla_w_down, l1_mla_q_norm, l1_mla_kv_norm,
                            l1_mla_w_uq, l1_mla_w_ukv, l1_mla_w_out),
        lambda h: moba_mixer(h, cos_p, sin_p, l2_moba_w_in, l2_moba_w_out),
        lambda h: swa_mixer(h, cos_p, sin_p, l3_swa_w_in, l3_swa_sinks, l3_swa_w_out),
    ]
    ln1 = [(l0_ln1_g, l0_ln1_b), (l1_ln1_g, l1_ln1_b), (l2_ln1_g, l2_ln1_b), (l3_ln1_g, l3_ln1_b)]
    xattn = [(l0_xq, l0_xkv, l0_xo), (l1_xq, l1_xkv, l1_xo), (l2_xq, l2_xkv, l2_xo), (l3_xq, l3_xkv, l3_xo)]
    ln2 = [(l0_ln2_g, l0_ln2_b), (l1_ln2_g, l1_ln2_b), (l2_ln2_g, l2_ln2_b), (l3_ln2_g, l3_ln2_b)]
    ffns = [
        lambda h: swiglu(h, l0_ffn_w_in, l0_ffn_w_out),
        lambda h: moe_ffn(h, l1_moe_router, l1_moe_bias, l1_moe_w_in, l1_moe_w_out),
        lambda h: swiglu(h, l2_ffn_w_in, l2_ffn_w_out),
        lambda h: moe_ffn(h, l3_moe_router, l3_moe_bias, l3_moe_w_in, l3_moe_w_out),
    ]
    ln3 = [(l0_ln3_g, l0_ln3_b), (l1_ln3_g, l1_ln3_b), (l2_ln3_g, l2_ln3_b), (l3_ln3_g, l3_ln3_b)]

    h = x
    for i in range(DEPTH):
        h = layer_norm(DEEPNORM_ALPHA * h + mixers[i % N_MIXERS](h), *ln1[i])
        h = layer_norm(DEEPNORM_ALPHA * h + cross_attention(h, mem, *xattn[i]), *ln2[i])
        h = layer_norm(DEEPNORM_ALPHA * h + ffns[i](h), *ln3[i])
    return h
```

```python
import numpy as np
import concourse.bass as bass
import concourse.mybir as mybir
from concourse.bass_utils import run_bass_kernel_spmd
from contextlib import ExitStack

F32 = mybir.dt.float32
BF16 = mybir.dt.bfloat16
I32 = mybir.dt.int32
AF = mybir.ActivationFunctionType
ALU = mybir.AluOpType
AX = mybir.AxisListType

S = 2048
D = 1024
NT = 16
DFF = 3584
NEXP = 8
ALPHA = 8.0 ** 0.25
LN_EPS = 1e-5
RMS_EPS = 1e-6
NEG = -30000.0
PI = float(np.pi)

EPOCH = 30000
NDS = 8


class Buf:
    __slots__ = ("w", "r", "psum")

    def __init__(self, psum=False):
        self.w = None
        self.r = {}
        self.psum = psum


class Tile:
    def __init__(self, t, psum=False):
        self.t = t
        self.parts = {}
        self.psum = psum

    def __getitem__(self, key):
        if self.psum:
            key = 0
        b = self.parts.get(key)
        if b is None:
            b = Buf(self.psum)
            self.parts[key] = b
        return b

    def all(self):
        return list(self.parts.values())


class KB:
    def __init__(self, nc, st):
        self.nc = nc
        self.st = st
        self.eng = {"pe": nc.tensor, "act": nc.scalar, "dve": nc.vector, "pool": nc.gpsimd, "sp": nc.sync}
        self.cnt = {e: 0 for e in ("pe", "act", "dve", "pool")}
        self.csems = {}
        self.seen = {e: {} for e in self.eng}
        self.dsems = {}
        self.dval = {}
        self.drr = {q: 0 for q in ("sp", "act", "pool")}
        self.nwait = 0
        self.nins = 0
        self.uid = 0

    def sb(self, shape, dt, name=None, st=None):
        self.uid += 1
        t = (st or self.st).enter_context(self.nc.sbuf_tensor(f"{name or 'sb'}_{self.uid}", list(shape), dt))
        return Tile(t)

    def ps(self, shape, dt, name=None):
        self.uid += 1
        t = self.st.enter_context(self.nc.psum_tensor(f"{name or 'ps'}_{self.uid}", list(shape), dt))
        return Tile(t, psum=True)

    def _csem(self, e, c):
        ep = (c - 1) // EPOCH
        k = (e, ep)
        s = self.csems.get(k)
        if s is None:
            s = self.st.enter_context(self.nc.semaphore(f"s_{e}_{ep}"))
            self.csems[k] = s
        return s, (c - 1) % EPOCH + 1

    def _dsem(self, key):
        s = self.dsems.get(key)
        if s is None:
            s = self.st.enter_context(self.nc.semaphore(f"d_{key[1]}_{key[2]}"))
            self.dsems[key] = s
            self.dval[key] = 0
        return s

    def _wait(self, e, k, v):
        eng = self.eng[e]
        if isinstance(k, tuple):
            eng.wait_ge(self._dsem(k), v)
        else:
            s, val = self._csem(k, v)
            eng.wait_ge(s, val)
        self.nwait += 1

    def _sync(self, e, reads, writes, is_dma=False):
        needs = {}

        def need(tok):
            if tok is None:
                return
            k, v = tok
            if needs.get(k, 0) < v:
                needs[k] = v

        for b in reads:
            need(b.w)
            if b.psum:
                for k, v in b.r.items():
                    if k != e:
                        need((k, v))
        for b in writes:
            if not (e == "pe" and not is_dma and b.w is not None and b.w[0] == "pe"):
                need(b.w)
            for k, v in b.r.items():
                if k != e or is_dma:
                    need((k, v))
        for k, v in needs.items():
            if self.seen[e].get(k, 0) >= v:
                continue
            self._wait(e, k, v)
            self.seen[e][k] = v

    def op(self, e, fn, reads=(), writes=()):
        reads = list(reads)
        writes = list(writes)
        self._sync(e, reads, writes)
        ins = fn(self.eng[e])
        self.cnt[e] += 1
        c = self.cnt[e]
        s, val = self._csem(e, c)
        ins.then_inc(s, 1)
        self.nins += 1
        for b in reads:
            b.r[e] = c
        for b in writes:
            b.w = (e, c)
            b.r = {}
        return ins

    def dma(self, q, out, in_, reads=(), writes=(), **kw):
        reads = list(reads)
        writes = list(writes)
        self._sync(q, reads, writes, is_dma=True)
        j = self.drr[q]
        self.drr[q] = (j + 1) % NDS
        key = ("d", q, j)
        s = self._dsem(key)
        prev = self.dval[key]
        if prev > 0 and self.seen[q].get(key, 0) < prev:
            self._wait(q, key, prev)
            self.seen[q][key] = prev
        ins = self.eng[q].dma_start(out=out, in_=in_, **kw)
        ins.then_inc(s, 16)
        self.nins += 1
        v = prev + 16
        self.dval[key] = v
        for b in reads:
            b.r[key] = v
        for b in writes:
            b.w = (key, v)
            b.r = {}
        return (key, v)

    def barrier(self):
        toks = [(e, c) for e, c in self.cnt.items() if c > 0]
        toks += [(k, v) for k, v in self.dval.items() if v > 0]
        for e in ("pe", "act", "dve", "pool", "sp"):
            for k, v in toks:
                if k == e:
                    continue
                if self.seen[e].get(k, 0) < v:
                    self._wait(e, k, v)
                    self.seen[e][k] = v

    def finish(self, toks, e="sp"):
        for k, v in toks:
            if self.seen[e].get(k, 0) < v:
                self._wait(e, k, v)
                self.seen[e][k] = v


class WStream:
    def __init__(self, kb, nbuf):
        self.kb = kb
        self.bufs = [kb.sb([128, 4096], BF16, f"wb{i}") for i in range(nbuf)]
        self.descs = []
        self.issued = 0
        self.cur = 0

    def register(self, tag, fn):
        self.descs.append((tag, fn))

    def next(self, tag):
        i = self.cur
        assert self.descs[i][0] == tag, (i, self.descs[i][0], tag)
        n = len(self.bufs)
        while self.issued < len(self.descs) and self.issued <= i + n - 2:
            j = self.issued
            self.descs[j][1](self.bufs[j % n])
            self.issued += 1
        self.cur += 1
        return self.bufs[i % n]


def _consts():
    c = {}
    c["c_ident"] = np.eye(128, dtype=np.float32)
    k = np.arange(128)[:, None]
    q = np.arange(128)[None, :]
    c["c_causal"] = np.where(k <= q, 0.0, NEG).astype(np.float32)
    c["c_far"] = np.where(k > q, 0.0, NEG).astype(np.float32)
    cc = np.arange(128)[:, None]
    qq = np.arange(S)[None, :]
    c["c_cmpmask"] = np.where((cc <= 126) & (cc * 16 + 31 <= qq), 0.0, NEG).astype(np.float32)
    kk = np.arange(S)[None, :]
    j32 = np.arange(32)[:, None]
    c["c_e32"] = (kk // 64 == j32).astype(np.float32)
    c["c_e8"] = ((kk // 256 == j32) & (j32 < 8)).astype(np.float32)
    c_start = np.arange(127) * 16
    b_start = np.arange(32) * 64
    ov = np.clip(np.minimum(c_start[:, None] + 32, b_start[None, :] + 64) - np.maximum(c_start[:, None], b_start[None, :]), 0, None) / 32.0
    ovx = np.zeros((128, 33), np.float32)
    ovx[:127, 0] = 1.0
    ovx[:127, 1:] = ov
    c["c_ovx"] = ovx
    tpos = np.arange(S)
    cur = (tpos // 64)[:, None]
    blk = np.arange(32)[None, :]
    forced = (blk == 0) | (blk == cur) | (blk == cur - 1)
    valid = blk <= cur
    A = (valid & ~forced).astype(np.float32)
    Bt = np.where(valid, np.where(forced, 1e4, 0.0), -1e30).astype(np.float32)
    c["c_nsaA"] = np.ascontiguousarray(A.reshape(16, 128, 32).transpose(1, 0, 2))
    c["c_nsaB"] = np.ascontiguousarray(Bt.reshape(16, 128, 32).transpose(1, 0, 2))
    own = (tpos // 256)[:, None]
    j8 = np.arange(8)[None, :]
    mv = (j8 < own).astype(np.float32)
    c["c_mobaB"] = np.ascontiguousarray(np.where(j8 < own, 0.0, -1e30).astype(np.float32).reshape(16, 128, 8).transpose(1, 0, 2))
    c["c_mobaV"] = np.ascontiguousarray(mv.reshape(16, 128, 8).transpose(1, 0, 2))
    c["c_mobaO"] = np.ascontiguousarray((j8 == own).astype(np.float32).reshape(16, 128, 8).transpose(1, 0, 2))
    c["c_invp"] = (500000.0 ** (-np.arange(0, 16, 2, dtype=np.float32) / 16)).astype(np.float32).reshape(1, 8)
    c["c_invm"] = (500000.0 ** (-np.arange(0, 32, 2, dtype=np.float32) / 32)).astype(np.float32).reshape(1, 16)
    return c


WSHAPES = {
    "l0_nsa_w_in": (1024, 2608), "l0_nsa_cmp_pe": (128, 32), "l0_nsa_cmp_w1": (2, 2048, 128),
    "l0_nsa_cmp_w2": (2, 128, 64), "l0_nsa_w_out": (1024, 1024),
    "l1_mla_w_down": (1024, 416), "l1_mla_q_norm": (1, 256), "l1_mla_kv_norm": (1, 128),
    "l1_mla_w_uq": (256, 1536), "l1_mla_w_ukv": (128, 2048), "l1_mla_w_out": (1024, 1024),
    "l2_moba_w_in": (1024, 3072), "l2_moba_w_out": (1024, 1024),
    "l3_swa_w_in": (1024, 1280), "l3_swa_sinks": (1, 16), "l3_swa_w_out": (1024, 1024),
}
for _i in range(4):
    for _k in (1, 2, 3):
        WSHAPES[f"l{_i}_ln{_k}_g"] = (1, 1024)
        WSHAPES[f"l{_i}_ln{_k}_b"] = (1, 1024)
    WSHAPES[f"l{_i}_xq"] = (1024, 1024)
    WSHAPES[f"l{_i}_xkv"] = (1024, 2048)
    WSHAPES[f"l{_i}_xo"] = (1024, 1024)
    if _i % 2 == 0:
        WSHAPES[f"l{_i}_ffn_w_in"] = (1024, 7168)
        WSHAPES[f"l{_i}_ffn_w_out"] = (3584, 1024)
    else:
        WSHAPES[f"l{_i}_moe_router"] = (8, 1024)
        WSHAPES[f"l{_i}_moe_bias"] = (1, 8)
        WSHAPES[f"l{_i}_moe_w_in"] = (8, 1024, 7168)
        WSHAPES[f"l{_i}_moe_w_out"] = (8, 3584, 1024)


class Prog:
    def __init__(self, plan=None, debug_ffn_experts=None):
        if plan is None:
            plan = []
            for li in range(4):
                plan += [("mixer", li), ("ln", li, 1), ("xattn", li), ("ln", li, 2), ("ffn", li), ("ln", li, 3)]
        self.plan = list(plan)
        self.dbg_experts = debug_ffn_experts
        self.nc = bass.Bass("TRN2", target_bir_lowering=False)
        nc = self.nc
        self.dram = {}

        def din(name, shape, dt=F32):
            self.dram[name] = nc.dram_tensor(name, list(shape), dt, kind="ExternalInput").ap()

        din("x", [S, D])
        din("mem", [256, D])
        din("posT", [128, NT], I32)
        self.consts = _consts()
        for k, v in self.consts.items():
            din(k, v.shape)
        self.used = []
        for k, shp in WSHAPES.items():
            li = int(k[1])
            need = False
            for stp in self.plan:
                if stp[1] != li:
                    continue
                if stp[0] == "mixer" and any(s in k for s in ("nsa", "mla", "moba", "swa")):
                    need = True
                if stp[0] == "ln" and f"_ln{stp[2]}_" in k:
                    need = True
                if stp[0] == "xattn" and any(k.endswith(s) for s in ("_xq", "_xkv", "_xo")):
                    need = True
                if stp[0] == "ffn" and ("ffn" in k or "moe" in k):
                    need = True
            if need:
                din(k, shp)
                self.used.append(k)
        self.out = nc.dram_tensor("out", [S, D], F32, kind="ExternalOutput").ap()

    def sbank(self):
        self._si = (self._si + 1) % len(self.sbanks)
        return self.sbanks[self._si]

    def obank(self):
        self._oi = (self._oi + 1) % len(self.obanks)
        return self.obanks[self._oi]

    def mbank(self):
        self._mi = (self._mi + 1) % len(self.mbanks)
        return self.mbanks[self._mi]

    def pbuf(self):
        self._pi = (self._pi + 1) % len(self.pbufs)
        return self.pbufs[self._pi]

    def hT_reads(self, t0, t1):
        return [self.hT[t] for t in range(t0, t1)]

    def build(self):
        nc = self.nc
        with ExitStack() as st:
            self.st = st
            kb = self.kb = KB(nc, st)
            self.h = kb.sb([128, NT, D], F32, "h")
            self.hT = kb.sb([128, 8, S], BF16, "hT")
            self.ws = WStream(kb, 4)
            self.ident = kb.sb([128, 128], BF16, "ident")
            self.causal = kb.sb([128, 128], BF16, "causal")
            self.far = kb.sb([128, 128], BF16, "far")
            self.memT = kb.sb([128, 8, 256], BF16, "memT")
            self.cosp = kb.sb([128, NT, 8], F32, "cosp")
            self.sinp = kb.sb([128, NT, 8], F32, "sinp")
            self.cosm = kb.sb([128, NT, 16], F32, "cosm")
            self.sinm = kb.sb([128, NT, 16], F32, "sinm")
            self.pbufs = [kb.sb([128, 512], BF16, "pb") for _ in range(3)]
            self.fin_s = [kb.sb([128, 4], F32, "fs") for _ in range(4)]
            self._fi = 0
            banks = [kb.ps([128, 512], F32, f"bank{i}") for i in range(8)]
            self.banks = banks
            self.attn_small = False
            self.sbanks = banks[0:6]
            self.obanks = banks[6:8]
            self.mbanks = banks[0:8]
            self._si = self._oi = self._mi = self._pi = 0
            self.out_toks = []
            for stp in self.plan:
                if stp[0] == "mixer":
                    [self.reg_nsa, self.reg_mla, self.reg_moba, self.reg_swa][stp[1]](stp[1])
                elif stp[0] == "xattn":
                    self.reg_xattn(stp[1])
                elif stp[0] == "ffn":
                    self.reg_ffn(stp[1])
            self.setup()
            for i, stp in enumerate(self.plan):
                last = (i == len(self.plan) - 1)
                if stp[0] == "mixer":
                    [self.nsa, self.mla, self.moba, self.swa][stp[1]](stp[1])
                elif stp[0] == "xattn":
                    self.xattn(stp[1])
                elif stp[0] == "ffn":
                    self.ffn(stp[1])
                else:
                    self.ln(stp[1], stp[2], last=last)
            self.store()
            kb.finish(self.out_toks)
            self.stats = (kb.nins, kb.nwait, dict(kb.cnt))
        return nc

    def wdma(self, buf, view, src):
        self.kb.dma("pool", view, src, writes=[buf[0]])

    def reg_cols(self, tag, wname, col_segs, kc=8, rows=None):
        ncols = sum(n for _, n in col_segs)
        W = self.dram[wname]

        def fn(buf):
            v = buf.t[:, 0:kc * ncols].rearrange("p (c n) -> p c n", c=kc)
            o = 0
            for (c0, n) in col_segs:
                src = W[:, c0:c0 + n] if rows is None else W[rows[0]:rows[1], c0:c0 + n]
                self.wdma(buf, v[:, :, o:o + n], src.rearrange("(c p) n -> p c n", p=128))
                o += n
        self.ws.register(tag, fn)

    def reg_rows(self, tag, W, r0, nrows, ncols=1024):
        kc = nrows // 128

        def fn(buf):
            v = buf.t[:, 0:kc * ncols].rearrange("p (c n) -> p c n", c=kc)
            self.wdma(buf, v, W[r0:r0 + nrows, :].rearrange("(c p) n -> p c n", p=128))
        self.ws.register(tag, fn)

    def setup(self):
        kb = self.kb
        d = self.dram
        kb.dma("pool", self.ident.t[:], d["c_ident"][:, :], writes=[self.ident[0]])
        kb.dma("pool", self.causal.t[:], d["c_causal"][:, :], writes=[self.causal[0]])
        kb.dma("pool", self.far.t[:], d["c_far"][:, :], writes=[self.far[0]])
        with ExitStack() as ls:
            self.hb = [kb.sb([128, D], BF16, "hb", ls) for _ in range(2)]
            posi = kb.sb([128, NT], I32, "posi", ls)
            posf = kb.sb([128, NT], F32, "posf", ls)
            inv = kb.sb([128, 24], F32, "inv", ls)
            ang = kb.sb([128, NT, 16], F32, "ang", ls)
            tmp = kb.sb([128, NT, 16], F32, "angt", ls)
            tmp2 = kb.sb([128, NT, 16], F32, "angt2", ls)
            ki = kb.sb([128, NT, 16], I32, "angk", ls)
            memb = kb.sb([128, 2, D], BF16, "memb", ls)
            negpi = kb.sb([128, 1], F32, "negpi", ls)
            kb.op("dve", lambda e: e.memset(negpi.t[:], -PI), writes=[negpi[0]])
            kb.dma("sp", posi.t[:], d["posT"][:, :], writes=[posi[0]])
            kb.dma("sp", inv.t[:, 0:8], d["c_invp"].partition_broadcast(128), writes=[inv[0]])
            kb.dma("sp", inv.t[:, 8:24], d["c_invm"].partition_broadcast(128), writes=[inv[1]])
            kb.op("dve", lambda e: e.tensor_copy(out=posf.t[:], in_=posi.t[:]), reads=[posi[0]], writes=[posf[0]])
            import os
            SK = os.environ.get("SKIP", "")
            for (n, o, cs, sn) in (() if "rope" in SK else ((8, 0, self.cosp, self.sinp), (16, 8, self.cosm, self.sinm))):
                a3 = ang.t[:, :, 0:n]
                t3 = tmp.t[:, :, 0:n]
                kb.op("dve", lambda e, a3=a3, n=n, o=o: e.tensor_tensor(
                    out=a3, in0=posf.t[:].unsqueeze(2).broadcast_to([128, NT, n]),
                    in1=inv.t[:, o:o + n].unsqueeze(1).broadcast_to([128, NT, n]), op=ALU.mult),
                    reads=[posf[0], inv[0], inv[1]], writes=[ang[0]])
                for (shift, dst) in ((0.0, sn), (0.5 * PI, cs)):
                    k3 = ki.t[:, :, 0:n]
                    m3 = tmp2.t[:, :, 0:n]
                    kb.op("dve", lambda e, t3=t3, a3=a3, shift=shift: e.tensor_scalar(
                        out=t3, in0=a3, scalar1=shift, scalar2=None, op0=ALU.add), reads=[ang[0]], writes=[tmp[0]])
                    kb.op("dve", lambda e, t3=t3, m3=m3: e.tensor_scalar(
                        out=m3, in0=t3, scalar1=1.0 / (2 * PI), scalar2=None, op0=ALU.mult), reads=[tmp[0]], writes=[tmp2[0]])
                    kb.op("dve", lambda e, k3=k3, m3=m3: e.tensor_copy(out=k3, in_=m3), reads=[tmp2[0]], writes=[ki[0]])
                    kb.op("dve", lambda e, k3=k3, m3=m3: e.tensor_copy(out=m3, in_=k3), reads=[ki[0]], writes=[tmp2[0]])
                    kb.op("dve", lambda e, t3=t3, m3=m3: e.scalar_tensor_tensor(
                        out=t3, in0=m3, scalar=-2 * PI, in1=t3, op0=ALU.mult, op1=ALU.add), reads=[tmp2[0], tmp[0]], writes=[tmp[0]])
                    kb.op("dve", lambda e, t3=t3, m3=m3: e.tensor_scalar(
                        out=m3, in0=t3, scalar1=PI, scalar2=-2 * PI, op0=ALU.is_gt, op1=ALU.mult), reads=[tmp[0]], writes=[tmp2[0]])
                    kb.op("dve", lambda e, t3=t3, m3=m3: e.tensor_tensor(out=t3, in0=t3, in1=m3, op=ALU.add), reads=[tmp[0], tmp2[0]], writes=[tmp[0]])
                    kb.op("dve", lambda e, t3=t3, m3=m3: e.tensor_scalar(
                        out=m3, in0=t3, scalar1=-PI, scalar2=2 * PI, op0=ALU.is_lt, op1=ALU.mult), reads=[tmp[0]], writes=[tmp2[0]])
                    kb.op("dve", lambda e, t3=t3, m3=m3: e.tensor_tensor(out=t3, in0=t3, in1=m3, op=ALU.add), reads=[tmp[0], tmp2[0]], writes=[tmp[0]])
                    kb.op("act", lambda e, t3=t3, dst=dst: e.activation(out=dst.t[:], in_=t3, func=AF.Sin), reads=[tmp[0]], writes=[dst[0]])
            for i in range(2):
                kb.dma("pool", memb.t[:, i, :], d["mem"][i * 128:(i + 1) * 128, :], writes=[memb[i]])
            for i in (() if "mem" in SK else range(2)):
                bank = self.mbank()
                pT = bank.t[:].bitcast(BF16)
                for c in range(8):
                    kb.op("pe", lambda e, c=c, i=i, pT=pT: e.transpose(out=pT[:, c * 128:(c + 1) * 128], in_=memb.t[:, i, c * 128:(c + 1) * 128], identity=self.ident.t[:]),
                          reads=[memb[i], self.ident[0]], writes=[bank[0]])
                kb.op("dve", lambda e, i=i, pT=pT: e.tensor_copy(out=self.memT.t[:, :, i * 128:(i + 1) * 128], in_=pT.rearrange("p (c n) -> p c n", c=8)),
                      reads=[bank[0]], writes=[self.memT[i]])
            for t in range(NT):
                kb.dma("sp", self.h.t[:, t, :], d["x"][t * 128:(t + 1) * 128, :], writes=[self.h[t]])
            for t in (() if "post" in SK else range(NT)):
                self.post_ln(t)
            if "bar" not in SK:
                kb.barrier()

    def post_ln(self, t):
        kb = self.kb
        ht = self.h.t[:, t, :]
        hb = self.hb[t % 2]
        kb.op("act", lambda e: e.copy(out=hb.t[:], in_=ht), reads=[self.h[t]], writes=[hb[0]])
        kb.op("act", lambda e: e.mul(out=ht, in_=ht, mul=ALPHA), reads=[self.h[t]], writes=[self.h[t]])
        bank = self.mbank()
        pT = bank.t[:].bitcast(BF16)
        for c in range(8):
            kb.op("pe", lambda e, c=c: e.transpose(out=pT[:, c * 128:(c + 1) * 128], in_=hb.t[:, c * 128:(c + 1) * 128], identity=self.ident.t[:]),
                  reads=[hb[0], self.ident[0]], writes=[bank[0]])
        kb.op("dve", lambda e: e.tensor_copy(out=self.hT.t[:, :, t * 128:(t + 1) * 128], in_=pT.rearrange("p (c n) -> p c n", c=8)),
              reads=[bank[0]], writes=[self.hT[t]])

    def ln(self, li, k, last=False):
        with ExitStack() as ls:
            kb = self.kb
            self.lnp = kb.sb([128, 2, D], F32, "lnp", ls)
            self.lnst = [kb.sb([128, 2, 6], F32, "lnst", ls) for _ in range(2)]
            self.lnmv = [kb.sb([128, 4], F32, "lnmv", ls) for _ in range(2)]
            self.hb = [kb.sb([128, D], BF16, "hb", ls) for _ in range(2)]
            self._ln(li, k, last)
            self.kb.barrier()

    def _ln(self, li, k, last=False):
        kb = self.kb
        d = self.dram
        lnp = self.lnp
        kb.dma("sp", lnp.t[:, 0, :], d[f"l{li}_ln{k}_g"].partition_broadcast(128), writes=[lnp[0]])
        kb.dma("sp", lnp.t[:, 1, :], d[f"l{li}_ln{k}_b"].partition_broadcast(128), writes=[lnp[1]])
        for t in range(NT):
            ht = self.h.t[:, t, :]
            hbuf = self.h[t]
            st_ = self.lnst[t % 2]
            mv = self.lnmv[t % 2]
            for i in range(2):
                kb.op("dve", lambda e, i=i: e.bn_stats(out=st_.t[:, i, :], in_=ht[:, i * 512:(i + 1) * 512]), reads=[hbuf], writes=[st_[i]])
            kb.op("dve", lambda e: e.bn_aggr(out=mv.t[:, 0:2], in_=st_.t[:]), reads=[st_[0], st_[1]], writes=[mv[0]])
            kb.op("dve", lambda e: e.tensor_scalar(out=mv.t[:, 3:4], in0=mv.t[:, 1:2], scalar1=LN_EPS, scalar2=None, op0=ALU.add),
                  reads=[mv[0]], writes=[mv[2]])
            kb.op("act", lambda e: e.activation(out=mv.t[:, 3:4], in_=mv.t[:, 3:4], func=AF.Sqrt), reads=[mv[2]], writes=[mv[2]])
            kb.op("dve", lambda e: e.reciprocal(out=mv.t[:, 2:3], in_=mv.t[:, 3:4]), reads=[mv[2]], writes=[mv[1]])
            kb.op("dve", lambda e: e.tensor_scalar(out=ht, in0=ht, scalar1=mv.t[:, 0:1], scalar2=mv.t[:, 2:3], op0=ALU.subtract, op1=ALU.mult),
                  reads=[hbuf, mv[0], mv[1]], writes=[hbuf])
            kb.op("dve", lambda e: e.tensor_tensor(out=ht, in0=ht, in1=lnp.t[:, 0, :], op=ALU.mult), reads=[hbuf, lnp[0]], writes=[hbuf])
            kb.op("dve", lambda e: e.tensor_tensor(out=ht, in0=ht, in1=lnp.t[:, 1, :], op=ALU.add), reads=[hbuf, lnp[1]], writes=[hbuf])
            if not last:
                self.post_ln(t)

    def store(self):
        kb = self.kb
        for t in range(NT):
            tok = kb.dma("sp", self.out[t * 128:(t + 1) * 128, :], self.h.t[:, t, :], reads=[self.h[t]])
            self.out_toks.append(tok)

    def attend(self, jobs):
        kb = self.kb
        ident = self.ident
        self.sbanks = self.banks[0:5] if self.attn_small else self.banks[0:6]
        self.obanks = self.banks[6:8]

        def bl(x):
            return list(x) if isinstance(x, (list, tuple)) else [x]
        groups = []
        for job in jobs:
            kts = job["kt"]
            n = len(kts)
            for g0 in range(0, n, 4):
                groups.append((job, kts[g0:g0 + 4], g0 == 0, g0 + 4 >= n))

        def emit_qk(grp):
            job, kts, first, last = grp
            sbk = self.sbank()
            for j, kt in enumerate(kts):
                reg = sbk.t[:, j * 128:(j + 1) * 128]
                nch = len(kt["k"])
                for ci in range(nch):
                    ka, kbuf = kt["k"][ci]
                    qa, qbuf = job["q"][ci]
                    lastmm = (ci == nch - 1) and kt.get("mask") is None
                    kb.op("pe", lambda e, reg=reg, ka=ka, qa=qa, s=(ci == 0), l=lastmm: e.matmul(reg, lhsT=ka, rhs=qa, start=s, stop=l),
                          reads=bl(kbuf) + bl(qbuf), writes=[sbk[0]])
                if kt.get("mask") is not None:
                    ma, mbuf = kt["mask"]
                    kb.op("pe", lambda e, reg=reg, ma=ma: e.matmul(reg, lhsT=ident.t[:], rhs=ma, start=False, stop=True),
                          reads=[ident[0]] + bl(mbuf), writes=[sbk[0]])
            return sbk

        def emit_exp(grp, sbk):
            job, kts, first, last = grp
            p = self.pbuf()
            w = len(kts) * 128
            kb.op("act", lambda e: e.activation(out=p.t[:, :w], in_=sbk.t[:, :w], func=AF.Exp, scale=job["scale"]),
                  reads=[sbk[0]], writes=[p[0]])
            return p

        def emit_pv(grp, p):
            job, kts, first, last = grp
            if first:
                job["_ob"] = self.obank()
            ob = job["_ob"]
            nv = job["nv"]
            for j, kt in enumerate(kts):
                va, vbuf = kt["v"]
                kb.op("pe", lambda e, j=j, va=va, s=(first and j == 0), l=(last and j == len(kts) - 1):
                      e.matmul(ob.t[:, :nv], lhsT=p.t[:, j * 128:(j + 1) * 128], rhs=va, start=s, stop=l),
                      reads=[p[0]] + bl(vbuf), writes=[ob[0]])
            if last:
                job["fin"](ob)

        pend = None
        for grp in groups:
            sbk = emit_qk(grp)
            p = emit_exp(grp, sbk)
            if pend is not None:
                emit_pv(*pend)
            pend = (grp, p)
        if pend is not None:
            emit_pv(*pend)

    def fin_plain(self, dst_ap, dst_buf, dv, extra=None):
        kb = self.kb

        def fin(ob):
            self._fi = (self._fi + 1) % len(self.fin_s)
            fs = self.fin_s[self._fi]
            if extra is None:
                kb.op("dve", lambda e: e.tensor_scalar(out=fs.t[:, 0:1], in0=ob.t[:, dv:dv + 1], scalar1=1e-30, scalar2=None, op0=ALU.max),
                      reads=[ob[0]], writes=[fs[0]])
            else:
                ea, ebuf = extra
                kb.op("dve", lambda e: e.tensor_scalar(out=fs.t[:, 0:1], in0=ob.t[:, dv:dv + 1], scalar1=ea, scalar2=None, op0=ALU.add),
                      reads=[ob[0], ebuf], writes=[fs[0]])
            kb.op("dve", lambda e: e.reciprocal(out=fs.t[:, 1:2], in_=fs.t[:, 0:1]), reads=[fs[0]], writes=[fs[1]])
            kb.op("dve", lambda e: e.tensor_scalar(out=dst_ap, in0=ob.t[:, 0:dv], scalar1=fs.t[:, 1:2], scalar2=None, op0=ALU.mult),
                  reads=[ob[0], fs[1]], writes=[dst_buf])
        return fin

    def rope(self, t, src3, srcbuf, dst3, dstbuf, nh, hd, ro, half, cs, sn):
        kb = self.kb
        x1 = src3[:, :, ro:ro + half]
        x2 = src3[:, :, ro + half:ro + 2 * half]
        cb = cs.t[:, t:t + 1, :].broadcast_to([128, nh, half])
        sb_ = sn.t[:, t:t + 1, :].broadcast_to([128, nh, half])
        tm = self.rtmp
        self._ri = (self._ri + 1) % 2
        base = self._ri * 4
        tv = [tm.t[:, base + i, 0:nh * half].rearrange("p (a b) -> p a b", a=nh) for i in range(4)]
        tb = [tm[base + i] for i in range(4)]
        kb.op("dve", lambda e: e.tensor_tensor(out=tv[0], in0=x1, in1=cb, op=ALU.mult), reads=[srcbuf, cs[0]], writes=[tb[0]])
        kb.op("dve", lambda e: e.tensor_tensor(out=tv[1], in0=x2, in1=sb_, op=ALU.mult), reads=[srcbuf, sn[0]], writes=[tb[1]])
        kb.op("dve", lambda e: e.tensor_tensor(out=tv[2], in0=x2, in1=cb, op=ALU.mult), reads=[srcbuf, cs[0]], writes=[tb[2]])
        kb.op("dve", lambda e: e.tensor_tensor(out=tv[3], in0=x1, in1=sb_, op=ALU.mult), reads=[srcbuf, sn[0]], writes=[tb[3]])
        kb.op("dve", lambda e: e.tensor_tensor(out=dst3[:, :, ro:ro + half], in0=tv[0], in1=tv[1], op=ALU.subtract), reads=[tb[0], tb[1]], writes=[dstbuf])
        kb.op("dve", lambda e: e.tensor_tensor(out=dst3[:, :, ro + half:ro + 2 * half], in0=tv[2], in1=tv[3], op=ALU.add), reads=[tb[2], tb[3]], writes=[dstbuf])
        if ro > 0:
            kb.op("dve", lambda e: e.tensor_copy(out=dst3[:, :, 0:ro], in_=src3[:, :, 0:ro]), reads=[srcbuf], writes=[dstbuf])
        if ro + 2 * half < hd:
            kb.op("dve", lambda e: e.tensor_copy(out=dst3[:, :, ro + 2 * half:hd], in_=src3[:, :, ro + 2 * half:hd]), reads=[srcbuf], writes=[dstbuf])

    def proj_tok(self, t, region, bank, wv, wbuf, ncols, kc=8, src=None):
        kb = self.kb
        for c in range(kc):
            if src is None:
                la, lbuf = self.hT.t[:, c, t * 128:(t + 1) * 128], self.hT[t]
            else:
                la, lbuf = src(c, t)
            kb.op("pe", lambda e, c=c, la=la: e.matmul(region, lhsT=la, rhs=wv[:, c, 0:ncols], start=(c == 0), stop=(c == kc - 1)),
                  reads=[lbuf, wbuf[0]], writes=[bank[0]])

    def pass_out(self, otok, ot, wo_tag, otok_is_f32=False):
        kb = self.kb
        for tb4 in range(4):
            bank = self.mbank()
            pT = bank.t[:].bitcast(BF16)
            for tl in range(4):
                t = tb4 * 4 + tl
                for c in range(2):
                    col = (tl * 2 + c) * 128
                    kb.op("pe", lambda e, t=t, c=c, col=col: e.transpose(out=pT[:, col:col + 128], in_=otok.t[:, t, c * 128:(c + 1) * 128], identity=self.ident.t[:]),
                          reads=[otok[t], self.ident[0]], writes=[bank[0]])
            pv = pT.rearrange("p (t c n) -> p t c n", t=4, c=2)
            for c in range(2):
                eng = "dve" if tb4 % 2 == 0 else "act"
                if eng == "dve":
                    kb.op("dve", lambda e, c=c: e.tensor_copy(out=ot.t[:, c, tb4 * 512:(tb4 + 1) * 512].rearrange("p (t n) -> p t n", t=4), in_=pv[:, :, c, :]),
                          reads=[bank[0]], writes=[ot[(c, tb4)]])
                else:
                    kb.op("act", lambda e, c=c: e.copy(out=ot.t[:, c, tb4 * 512:(tb4 + 1) * 512].rearrange("p (t n) -> p t n", t=4), in_=pv[:, :, c, :]),
                          reads=[bank[0]], writes=[ot[(c, tb4)]])
        wo = self.ws.next(wo_tag)
        wv = wo.t[:, 0:2048].rearrange("p (c n) -> p c n", c=2)
        import os
        PS = int(os.environ.get("PS", "9"))
        for t in (range(NT) if PS >= 2 else ()):
            for hf in range(2):
                bank = self.mbank()
                for c in range(2):
                    kb.op("pe", lambda e, c=c, t=t, hf=hf: e.matmul(bank.t[:, :], lhsT=ot.t[:, c, t * 128:(t + 1) * 128], rhs=wv[:, c, hf * 512:(hf + 1) * 512], start=(c == 0), stop=(c == 1)),
                          reads=[ot[(c, t // 4)], wo[0]], writes=[bank[0]])
                hs = self.h.t[:, t, hf * 512:(hf + 1) * 512]
                if PS >= 3:
                    kb.op("dve", lambda e, hs=hs, bank=bank: e.tensor_tensor(out=hs, in0=bank.t[:, :], in1=hs, op=ALU.add),
                          reads=[bank[0], self.h[t]], writes=[self.h[t]])

    def reg_xattn(self, li):
        for hd in range(4):
            self.reg_cols(f"x{li}k{hd}", f"l{li}_xkv", [(hd * 256, 256)])
            self.reg_cols(f"x{li}v{hd}", f"l{li}_xkv", [(1024 + hd * 256, 256)])
            self.reg_cols(f"x{li}q{hd}", f"l{li}_xq", [(hd * 256, 256)])
            self.reg_rows(f"x{li}o{hd}", self.dram[f"l{li}_xo"], hd * 256, 256)

    def xattn(self, li):
        kb = self.kb
        with ExitStack() as ls:
            qT = kb.sb([128, 2, S], BF16, "xqT", ls)
            kT = kb.sb([128, 2, 256], BF16, "xkT", ls)
            vx = kb.sb([128, 2, 257], BF16, "xvx", ls)
            otok = kb.sb([128, NT, 256], BF16, "xotok", ls)
            ot = kb.sb([128, 2, S], BF16, "xot", ls)
            kb.op("dve", lambda e: e.memset(vx.t[:, :, 256:257], 1.0), writes=[vx[0], vx[1]])
            for hd in range(4):
                wk = self.ws.next(f"x{li}k{hd}")
                wkv = wk.t[:, 0:2048].rearrange("p (c n) -> p c n", c=8)
                for c in range(2):
                    bank = self.mbank()
                    for dc in range(8):
                        kb.op("pe", lambda e, c=c, dc=dc, bank=bank: e.matmul(bank.t[:, 0:256], lhsT=wkv[:, dc, c * 128:(c + 1) * 128], rhs=self.memT.t[:, dc, :], start=(dc == 0), stop=(dc == 7)),
                              reads=[wk[0], self.memT[0], self.memT[1]], writes=[bank[0]])
                    kb.op("act", lambda e, c=c, bank=bank: e.copy(out=kT.t[:, c, :], in_=bank.t[:, 0:256]), reads=[bank[0]], writes=[kT[c]])
                wvb = self.ws.next(f"x{li}v{hd}")
                wvv = wvb.t[:, 0:2048].rearrange("p (c n) -> p c n", c=8)
                for mt in range(2):
                    bank = self.mbank()
                    for dc in range(8):
                        kb.op("pe", lambda e, mt=mt, dc=dc, bank=bank: e.matmul(bank.t[:, 0:256], lhsT=self.memT.t[:, dc, mt * 128:(mt + 1) * 128], rhs=wvv[:, dc, :], start=(dc == 0), stop=(dc == 7)),
                              reads=[wvb[0], self.memT[mt]], writes=[bank[0]])
                    kb.op("act", lambda e, mt=mt, bank=bank: e.copy(out=vx.t[:, mt, 0:256], in_=bank.t[:, 0:256]), reads=[bank[0]], writes=[vx[mt]])
                wq = self.ws.next(f"x{li}q{hd}")
                wqv = wq.t[:, 0:2048].rearrange("p (c n) -> p c n", c=8)
                for c in range(2):
                    for n in range(4):
                        bank = self.mbank()
                        for dc in range(8):
                            kb.op("pe", lambda e, c=c, n=n, dc=dc, bank=bank: e.matmul(bank.t[:, :], lhsT=wqv[:, dc, c * 128:(c + 1) * 128], rhs=self.hT.t[:, dc, n * 512:(n + 1) * 512], start=(dc == 0), stop=(dc == 7)),
                                  reads=[wq[0]] + self.hT_reads(4 * n, 4 * n + 4), writes=[bank[0]])
                        if (c + n) % 2 == 0:
                            kb.op("act", lambda e, c=c, n=n, bank=bank: e.copy(out=qT.t[:, c, n * 512:(n + 1) * 512], in_=bank.t[:, :]), reads=[bank[0]], writes=[qT[(c, n)]])
                        else:
                            kb.op("dve", lambda e, c=c, n=n, bank=bank: e.tensor_copy(out=qT.t[:, c, n * 512:(n + 1) * 512], in_=bank.t[:, :]), reads=[bank[0]], writes=[qT[(c, n)]])
                jobs = []
                for t in range(NT):
                    kts = []
                    for mt in range(2):
                        kts.append(dict(k=[(kT.t[:, c, mt * 128:(mt + 1) * 128], kT[c]) for c in range(2)],
                                        v=(vx.t[:, mt, :], vx[mt])))
                    jobs.append(dict(q=[(qT.t[:, c, t * 128:(t + 1) * 128], qT[(c, t // 4)]) for c in range(2)],
                                     kt=kts, nv=257, scale=1.0 / 16.0,
                                     fin=self.fin_plain(otok.t[:, t, :], otok[t], 256)))
                import os
                XS = int(os.environ.get("XSTOP", "9"))
                if XS >= 2:
                    self.attend(jobs)
                if XS >= 3:
                    self.pass_out(otok, ot, f"x{li}o{hd}")
                else:
                    self.ws.next(f"x{li}o{hd}")
        kb.barrier()

    def ffn_groups(self, li):
        moe = (li % 2 == 1)
        ne = NEXP if moe else 1
        if self.dbg_experts is not None and moe:
            ne = self.dbg_experts
        return [(ex, g) for ex in range(ne) for g in range(7)]

    def reg_ffn(self, li):
        moe = (li % 2 == 1)
        groups = self.ffn_groups(li)

        def wts(ex):
            if moe:
                return self.dram[f"l{li}_moe_w_in"][ex], self.dram[f"l{li}_moe_w_out"][ex]
            return self.dram[f"l{li}_ffn_w_in"], self.dram[f"l{li}_ffn_w_out"]

        def reg_gu(ex, g):
            Win, _ = wts(ex)
            for (nm, c0) in (("g", g * 512), ("u", DFF + g * 512)):
                def fn(buf, Win=Win, c0=c0):
                    v = buf.t[:, 0:4096].rearrange("p (c n) -> p c n", c=8)
                    self.wdma(buf, v, Win[:, c0:c0 + 512].rearrange("(c p) n -> p c n", p=128))
                self.ws.register(f"f{li}e{ex}{nm}{g}", fn)

        def reg_o(ex, g):
            _, Wout = wts(ex)
            self.reg_rows(f"f{li}e{ex}o{g}", Wout, g * 512, 512)

        for i, (ex, g) in enumerate(groups):
            reg_gu(ex, g)
            if i >= 1:
                reg_o(*groups[i - 1])
        reg_o(*groups[-1])

    def ffn(self, li):
        kb = self.kb
        moe = (li % 2 == 1)
        ne = NEXP if moe else 1
        if self.dbg_experts is not None and moe:
            ne = self.dbg_experts
        d = self.dram
        with ExitStack() as ls:
            comb = None
            if moe:
                comb = kb.sb([128, NT, 8], F32, "comb", ls)
                with ExitStack() as ls2:
                    rB = kb.sb([128, 8, D], F32, "rB", ls2)
                    junk2 = [kb.sb([128, D], F32, "junk", ls2) for _ in range(2)]
                    lg = kb.sb([128, NT, 8], F32, "lg", ls2)
                    bias = kb.sb([128, 8], F32, "rbias", ls2)
                    top = kb.sb([128, NT, 8], F32, "top", ls2)
                    gg = kb.sb([128, NT, 4], F32, "gg", ls2)
                    m12 = kb.sb([128, 2, 8], F32, "m12", ls2)
                    for ex in range(8):
                        kb.dma("sp", rB.t[:, ex, :], d[f"l{li}_moe_router"][ex:ex + 1, :].partition_broadcast(128), writes=[rB[ex]])
                    kb.dma("sp", bias.t[:], d[f"l{li}_moe_bias"].partition_broadcast(128), writes=[bias[0]])
                    for t in range(NT):
                        for ex in range(8):
                            jk = junk2[ex % 2]
                            kb.op("dve", lambda e, t=t, ex=ex, jk=jk: e.tensor_tensor(out=jk.t[:], in0=self.h.t[:, t, :], in1=rB.t[:, ex, :], op=ALU.mult),
                                  reads=[self.h[t], rB[ex]], writes=[jk[0]])
                            kb.op("dve", lambda e, t=t, ex=ex, jk=jk: e.reduce_sum(out=lg.t[:, t, ex:ex + 1], in_=jk.t[:], axis=AX.X),
                                  reads=[jk[0]], writes=[lg[(t, ex)]])
                        lgt = [lg[(t, ex)] for ex in range(8)]
                        kb.op("dve", lambda e, t=t: e.scalar_tensor_tensor(out=lg.t[:, t, :], in0=lg.t[:, t, :], scalar=1.0 / ALPHA, in1=bias.t[:], op0=ALU.mult, op1=ALU.add),
                              reads=lgt + [bias[0]], writes=lgt)
                        kb.op("dve", lambda e, t=t: e.max(out=top.t[:, t, :], in_=lg.t[:, t, :]), reads=lgt, writes=[top[t]])
                        kb.op("dve", lambda e, t=t: e.tensor_tensor(out=gg.t[:, t, 0:1], in0=top.t[:, t, 1:2], in1=top.t[:, t, 0:1], op=ALU.subtract),
                              reads=[top[t]], writes=[gg[(t, 0)]])
                        kb.op("act", lambda e, t=t: e.activation(out=gg.t[:, t, 1:2], in_=gg.t[:, t, 0:1], func=AF.Exp), reads=[gg[(t, 0)]], writes=[gg[(t, 1)]])
                        kb.op("dve", lambda e, t=t: e.tensor_scalar(out=gg.t[:, t, 2:3], in0=gg.t[:, t, 1:2], scalar1=1.0, scalar2=None, op0=ALU.add),
                              reads=[gg[(t, 1)]], writes=[gg[(t, 2)]])
                        kb.op("dve", lambda e, t=t: e.reciprocal(out=gg.t[:, t, 2:3], in_=gg.t[:, t, 2:3]), reads=[gg[(t, 2)]], writes=[gg[(t, 2)]])
                        kb.op("dve", lambda e, t=t: e.tensor_tensor(out=gg.t[:, t, 3:4], in0=gg.t[:, t, 1:2], in1=gg.t[:, t, 2:3], op=ALU.mult),
                              reads=[gg[(t, 1)], gg[(t, 2)]], writes=[gg[(t, 3)]])
                        kb.op("dve", lambda e, t=t: e.tensor_scalar(out=m12.t[:, 0, :], in0=lg.t[:, t, :], scalar1=top.t[:, t, 0:1], scalar2=gg.t[:, t, 2:3], op0=ALU.is_equal, op1=ALU.mult),
                              reads=lgt + [top[t], gg[(t, 2)]], writes=[m12[0]])
                        kb.op("dve", lambda e, t=t: e.tensor_scalar(out=m12.t[:, 1, :], in0=lg.t[:, t, :], scalar1=top.t[:, t, 1:2], scalar2=gg.t[:, t, 3:4], op0=ALU.is_equal, op1=ALU.mult),
                              reads=lgt + [top[t], gg[(t, 3)]], writes=[m12[1]])
                        kb.op("dve", lambda e, t=t: e.tensor_tensor(out=comb.t[:, t, :], in0=m12.t[:, 0, :], in1=m12.t[:, 1, :], op=ALU.add),
                              reads=[m12[0], m12[1]], writes=[comb[t]])
                kb.barrier()
            aT = [kb.sb([128, 4, S], BF16, "aT", ls) for _ in range(2)]
            sg = [kb.sb([128, 512], F32, "sg", ls) for _ in range(3)]
            sgi = 0
            pend = None

            def emit_out(ex, g, at, wo):
                wv = wo.t[:, 0:4096].rearrange("p (c n) -> p c n", c=4)
                for t in range(NT):
                    for hf in range(2):
                        self._fo = (getattr(self, '_fo', 0) + 1) % 4
                        bank = self.banks[4:8][self._fo]
                        for j in range(4):
                            kb.op("pe", lambda e, j=j, t=t, hf=hf, bank=bank: e.matmul(bank.t[:, :], lhsT=at.t[:, j, t * 128:(t + 1) * 128], rhs=wv[:, j, hf * 512:(hf + 1) * 512], start=(j == 0), stop=(j == 3)),
                                  reads=[at[(j, t // 4)], wo[0]], writes=[bank[0]])
                        hs = self.h.t[:, t, hf * 512:(hf + 1) * 512]
                        if moe:
                            kb.op("dve", lambda e, hs=hs, bank=bank, t=t: e.scalar_tensor_tensor(out=hs, in0=bank.t[:, :], scalar=comb.t[:, t, ex:ex + 1], in1=hs, op0=ALU.mult, op1=ALU.add),
                                  reads=[bank[0], self.h[t], comb[t]], writes=[self.h[t]])
                        else:
                            kb.op("dve", lambda e, hs=hs, bank=bank: e.tensor_tensor(out=hs, in0=bank.t[:, :], in1=hs, op=ALU.add),
                                  reads=[bank[0], self.h[t]], writes=[self.h[t]])

            gub = self.banks[0:4]
            gui = 0
            for gi, (ex, g) in enumerate(self.ffn_groups(li)):
                wg = self.ws.next(f"f{li}e{ex}g{g}")
                wu = self.ws.next(f"f{li}e{ex}u{g}")
                wgv = wg.t[:, 0:4096].rearrange("p (c n) -> p c n", c=8)
                wuv = wu.t[:, 0:4096].rearrange("p (c n) -> p c n", c=8)
                at = aT[gi % 2]
                for j in range(4):
                    for n in range(4):
                        bg = gub[gui % 4]
                        bu = gub[(gui + 1) % 4]
                        gui += 2
                        hr = self.hT_reads(4 * n, 4 * n + 4)
                        for dc in range(8):
                            kb.op("pe", lambda e, dc=dc, j=j, n=n, bg=bg: e.matmul(bg.t[:, :], lhsT=wgv[:, dc, j * 128:(j + 1) * 128], rhs=self.hT.t[:, dc, n * 512:(n + 1) * 512], start=(dc == 0), stop=(dc == 7)),
                                  reads=[wg[0]] + hr, writes=[bg[0]])
                        for dc in range(8):
                            kb.op("pe", lambda e, dc=dc, j=j, n=n, bu=bu: e.matmul(bu.t[:, :], lhsT=wuv[:, dc, j * 128:(j + 1) * 128], rhs=self.hT.t[:, dc, n * 512:(n + 1) * 512], start=(dc == 0), stop=(dc == 7)),
                                  reads=[wu[0]] + hr, writes=[bu[0]])
                        s_ = sg[sgi % 3]
                        sgi += 1
                        kb.op("act", lambda e, bg=bg, s_=s_: e.activation(out=s_.t[:], in_=bg.t[:, :], func=AF.Silu), reads=[bg[0]], writes=[s_[0]])
                        kb.op("dve", lambda e, bu=bu, s_=s_, j=j, n=n, at=at: e.tensor_tensor(out=at.t[:, j, n * 512:(n + 1) * 512], in0=bu.t[:, :], in1=s_.t[:], op=ALU.mult),
                              reads=[bu[0], s_[0]], writes=[at[(j, n)]])
                if pend is not None:
                    wo = self.ws.next(f"f{li}e{pend[0]}o{pend[1]}")
                    emit_out(pend[0], pend[1], pend[2], wo)
                pend = (ex, g, at)
            wo = self.ws.next(f"f{li}e{pend[0]}o{pend[1]}")
            emit_out(pend[0], pend[1], pend[2], wo)
        kb.barrier()

    def transposes(self, srcs, rows=128):
        kb = self.kb
        bank = self.mbank()
        pT = bank.t[:].bitcast(BF16)
        for i, (ap, buf) in enumerate(srcs):
            nc_ = ap.shape[-1]
            kb.op("pe", lambda e, i=i, ap=ap, nc_=nc_: e.transpose(out=pT[0:nc_, i * 128:(i + 1) * 128], in_=ap, identity=self.ident.t[:]),
                  reads=(list(buf) if isinstance(buf, (list, tuple)) else [buf]) + [self.ident[0]], writes=[bank[0]])
        return bank, pT

    def skewed(self, n, pe_part, mid_part, fin_part):
        a = pe_part(0)
        prev = None
        for t in range(n):
            nxt = pe_part(t + 1) if t + 1 < n else None
            m = mid_part(t, a)
            if prev is not None:
                fin_part(t - 1, prev)
            prev = m
            a = nxt
        fin_part(n - 1, prev)

    def causal_kts(self, t, kfn, vfn, lo=0, far_at=None):
        kts = []
        for kt in range(lo, t + 1):
            d = dict(k=kfn(kt), v=vfn(kt))
            if kt == t:
                d["mask"] = (self.causal.t[:], self.causal[0])
            elif far_at is not None and kt == far_at:
                d["mask"] = (self.far.t[:], self.far[0])
            kts.append(d)
        return kts

    def reg_moba(self, li):
        for p in range(4):
            self.reg_cols(f"m{li}qk{p}", "l2_moba_w_in", [(p * 256, 256), (1024 + p * 256, 256)])
            self.reg_cols(f"m{li}v{p}", "l2_moba_w_in", [(2048 + p * 256, 256)])
            self.reg_rows(f"m{li}o{p}", self.dram["l2_moba_w_out"], p * 256, 256)

    def moba(self, li):
        kb = self.kb
        d = self.dram
        with ExitStack() as ls:
            qaT = kb.sb([128, 4, S], BF16, "qaT", ls)
            kaT = kb.sb([128, 4, S], BF16, "kaT", ls)
            vx = kb.sb([128, NT, 4, 65], BF16, "vx", ls)
            stg = [kb.sb([128, 512], BF16, "stg", ls) for _ in range(2)]
            otok = kb.sb([128, NT, 256], BF16, "otok", ls)
            ot = kb.sb([128, 2, S], BF16, "ot", ls)
            nm = kb.sb([128, NT, 4, 32], BF16, "nm", ls)
            ksf = kb.sb([128, 4, 8], F32, "ksf", ls)
            ksb = kb.sb([128, 4, 8], BF16, "ksb", ls)
            tB = kb.sb([128, NT, 8], F32, "tB", ls)
            tV = kb.sb([128, NT, 8], F32, "tV", ls)
            tO = kb.sb([128, NT, 8], F32, "tO", ls)
            gm = [kb.sb([128, 4, 8], F32, "gm", ls) for _ in range(2)]
            sel = [kb.sb([128, 4, 8], F32, "sel", ls) for _ in range(2)]
            top8 = [kb.sb([128, 8], F32, "top8", ls) for _ in range(4)]
            self.rtmp = kb.sb([128, 8, 64], F32, "rtmp", ls)
            self._ri = 0
            kb.dma("sp", tB.t[:], d["c_mobaB"][:, :, :], writes=[tB[0]])
            kb.dma("sp", tV.t[:], d["c_mobaV"][:, :, :], writes=[tV[0]])
            kb.dma("sp", tO.t[:], d["c_mobaO"][:, :, :], writes=[tO[0]])
            for r in range(4):
                kb.dma("pool", kaT.t[64:96, r, :], d["c_e8"][:, :], writes=[kaT[("e", r)]])
            kb.op("dve", lambda e: e.memset(vx.t[:, :, :, 64:65], 1.0), writes=[vx[t] for t in range(NT)])
            kb.op("dve", lambda e: e.memset(nm.t[:], 0.0), writes=[nm[t] for t in range(NT)])
            for p in range(4):
                wqk = self.ws.next(f"m{li}qk{p}")
                wv_ = self.ws.next(f"m{li}v{p}")
                wqkv = wqk.t[:, 0:4096].rearrange("p (c n) -> p c n", c=8)
                wvv = wv_.t[:, 0:2048].rearrange("p (c n) -> p c n", c=8)
                def pe_part(t):
                    bA = self.mbank()
                    self.proj_tok(t, bA.t[:, 0:256], bA, wqkv[:, :, 0:256], wqk, 256)
                    self.proj_tok(t, bA.t[:, 256:512], bA, wqkv[:, :, 256:512], wqk, 256)
                    bB = self.mbank()
                    self.proj_tok(t, bB.t[:, 0:256], bB, wvv, wv_, 256)
                    return bA, bB

                def mid_part(t, ab):
                    bA, bB = ab
                    sg_ = stg[t % 2]
                    self.rope(t, bA.t[:, 0:512].rearrange("p (a b) -> p a b", a=8), bA[0],
                              sg_.t[:, :].rearrange("p (a b) -> p a b", a=8), sg_[0], 8, 64, 0, 8, self.cosp, self.sinp)
                    kb.op("act", lambda e, t=t, bB=bB: e.activation(out=vx.t[:, t, :, 0:64], in_=bB.t[:, 0:256].rearrange("p (a b) -> p a b", a=4), func=AF.Identity),
                          reads=[bB[0]], writes=[vx[t]])
                    return self.transposes([(sg_.t[:, i * 128:(i + 1) * 128], sg_[0]) for i in range(4)])

                def fin_part(t, bp):
                    bT, pT = bp
                    tsl = slice(t * 128, (t + 1) * 128)
                    for (dstT, c0) in ((qaT, 0), (kaT, 256)):
                        for par in range(2):
                            kb.op("dve", lambda e, dstT=dstT, c0=c0, par=par, pT=pT, tsl=tsl: e.tensor_copy(
                                out=dstT.t[0:64, par:4:2, tsl], in_=pT[par * 64:par * 64 + 64, c0:c0 + 256].rearrange("p (a n) -> p a n", a=2)),
                                reads=[bT[0]], writes=[dstT[(par, t)], dstT[(par + 2, t)]])
                self.skewed(NT, pe_part, mid_part, fin_part)
                kb.op("dve", lambda e: e.tensor_reduce(out=ksf.t[0:64, :, :], in_=kaT.t[0:64, :, :].rearrange("p r (j n) -> p r j n", n=256), axis=AX.X, op=ALU.add),
                      reads=[kaT[(r, t)] for r in range(4) for t in range(NT)], writes=[ksf[0]])
                kb.op("dve", lambda e: e.tensor_copy(out=ksb.t[0:64, :, :], in_=ksf.t[0:64, :, :]), reads=[ksf[0]], writes=[ksb[0]])
                for t in range(NT):
                    tsl = slice(t * 128, (t + 1) * 128)
                    bG = self.mbank()
                    for r in range(4):
                        kb.op("pe", lambda e, r=r, bG=bG, tsl=tsl: e.matmul(bG.t[:, r * 8:(r + 1) * 8], lhsT=qaT.t[0:64, r, tsl], rhs=ksb.t[0:64, r, :], start=True, stop=True),
                              reads=[qaT[(r, t)], ksb[0]], writes=[bG[0]])
                    g_ = gm[t % 2]
                    s_ = sel[t % 2]
                    kb.op("dve", lambda e, bG=bG, g_=g_, t=t: e.tensor_tensor(out=g_.t[:], in0=bG.t[:, 0:32].rearrange("p (a b) -> p a b", a=4),
                                                                          in1=tB.t[:, t:t + 1, :].broadcast_to([128, 4, 8]), op=ALU.add),
                          reads=[bG[0], tB[0]], writes=[g_[0]])
                    for r in range(4):
                        tp = top8[r]
                        kb.op("dve", lambda e, r=r, tp=tp, g_=g_: e.max(out=tp.t[:], in_=g_.t[:, r, :]), reads=[g_[0]], writes=[tp[0]])
                        kb.op("dve", lambda e, r=r, tp=tp, g_=g_, s_=s_: e.tensor_scalar(out=s_.t[:, r, :], in0=g_.t[:, r, :], scalar1=tp.t[:, 2:3], scalar2=None, op0=ALU.is_ge),
                              reads=[g_[0], tp[0]], writes=[s_[r]])
                    sr = [s_[r] for r in range(4)]
                    kb.op("dve", lambda e, s_=s_, t=t: e.tensor_tensor(out=s_.t[:], in0=s_.t[:], in1=tV.t[:, t:t + 1, :].broadcast_to([128, 4, 8]), op=ALU.mult),
                          reads=sr + [tV[0]], writes=sr)
                    kb.op("dve", lambda e, s_=s_, t=t: e.tensor_tensor(out=s_.t[:], in0=s_.t[:], in1=tO.t[:, t:t + 1, :].broadcast_to([128, 4, 8]), op=ALU.add),
                          reads=sr + [tO[0]], writes=sr)
                    kb.op("dve", lambda e, s_=s_, t=t: e.tensor_scalar(out=nm.t[:, t, :, 0:8], in0=s_.t[:], scalar1=-1.0, scalar2=-NEG, op0=ALU.add, op1=ALU.mult),
                          reads=sr, writes=[nm[t]])
                for r in range(4):
                    for tb in range(2):
                        bT, pT = self.transposes([(nm.t[:, tb * 8 + j, r, :], nm[tb * 8 + j]) for j in range(8)])
                        kb.op("dve", lambda e, r=r, tb=tb, pT=pT: e.tensor_copy(out=qaT.t[64:96, r, tb * 1024:(tb + 1) * 1024], in_=pT[0:32, 0:1024]),
                              reads=[bT[0]], writes=[qaT[("m", r, tb)]])
                jobs = []
                for r in range(4):
                    for t in range(NT):
                        tsl = slice(t * 128, (t + 1) * 128)
                        kts = self.causal_kts(
                            t,
                            lambda kt, r=r: [(kaT.t[0:96, r, kt * 128:(kt + 1) * 128], [kaT[(r, kt)], kaT[("e", r)]])],
                            lambda kt, r=r: (vx.t[:, kt, r, :], vx[kt]))
                        jobs.append(dict(q=[(qaT.t[0:96, r, tsl], [qaT[(r, t)], qaT[("m", r, t // 8)]])], kt=kts, nv=65, scale=0.125,
                                         fin=self.fin_plain(otok.t[:, t, r * 64:(r + 1) * 64], otok[t], 64)))
                self.attend(jobs)
                self.pass_out(otok, ot, f"m{li}o{p}")
        kb.barrier()

    def reg_swa(self, li):
        for p in range(4):
            g = p // 2
            self.reg_cols(f"s{li}qkv{p}", "l3_swa_w_in", [(p * 256, 256), (1024 + g * 64, 64), (1024 + 128 + g * 64, 64)])
            self.reg_rows(f"s{li}o{p}", self.dram["l3_swa_w_out"], p * 256, 256)

    def swa(self, li):
        kb = self.kb
        d = self.dram
        with ExitStack() as ls:
            qT = kb.sb([128, 4, S], BF16, "qT", ls)
            kT = kb.sb([128, S], BF16, "kT", ls)
            vx = kb.sb([128, NT, 65], BF16, "vx", ls)
            stg = [kb.sb([128, 320], BF16, "stg", ls) for _ in range(2)]
            otok = kb.sb([128, NT, 256], BF16, "otok", ls)
            ot = kb.sb([128, 2, S], BF16, "ot", ls)
            snk = kb.sb([128, 16], F32, "snk", ls)
            esink = kb.sb([128, 16], F32, "esink", ls)
            self.rtmp = kb.sb([128, 8, 64], F32, "rtmp", ls)
            self._ri = 0
            kb.dma("sp", snk.t[:], d["l3_swa_sinks"].partition_broadcast(128), writes=[snk[0]])
            kb.op("act", lambda e: e.activation(out=esink.t[:], in_=snk.t[:], func=AF.Exp), reads=[snk[0]], writes=[esink[0]])
            kb.op("dve", lambda e: e.memset(vx.t[:, :, 64:65], 1.0), writes=[vx[t] for t in range(NT)])
            for p in range(4):
                w = self.ws.next(f"s{li}qkv{p}")
                wv = w.t[:, 0:8 * 384].rearrange("p (c n) -> p c n", c=8)
                def pe_part(t):
                    bA = self.mbank()
                    self.proj_tok(t, bA.t[:, 0:256], bA, wv[:, :, 0:256], w, 256)
                    self.proj_tok(t, bA.t[:, 256:384], bA, wv[:, :, 256:384], w, 128)
                    return bA

                def mid_part(t, bA):
                    sg_ = stg[t % 2]
                    self.rope(t, bA.t[:, 0:320].rearrange("p (a b) -> p a b", a=5), bA[0],
                              sg_.t[:, :].rearrange("p (a b) -> p a b", a=5), sg_[0], 5, 64, 0, 8, self.cosp, self.sinp)
                    kb.op("dve", lambda e, t=t, bA=bA: e.tensor_copy(out=vx.t[:, t, 0:64], in_=bA.t[:, 320:384]), reads=[bA[0]], writes=[vx[t]])
                    return self.transposes([(sg_.t[:, 0:128], sg_[0]), (sg_.t[:, 128:256], sg_[0]), (sg_.t[:, 256:320], sg_[0])])

                def fin_part(t, bp):
                    bT, pT = bp
                    tsl = slice(t * 128, (t + 1) * 128)
                    for par in range(2):
                        kb.op("dve", lambda e, par=par, pT=pT, tsl=tsl: e.tensor_copy(
                            out=qT.t[0:64, par:4:2, tsl], in_=pT[par * 64:par * 64 + 64, 0:256].rearrange("p (a n) -> p a n", a=2)),
                            reads=[bT[0]], writes=[qT[(par, t)], qT[(par + 2, t)]])
                    kb.op("dve", lambda e, pT=pT, tsl=tsl: e.tensor_copy(out=kT.t[0:64, tsl], in_=pT[0:64, 256:384]), reads=[bT[0]], writes=[kT[t]])
                self.skewed(NT, pe_part, mid_part, fin_part)
                jobs = []
                for r in range(4):
                    hh = p * 4 + r
                    for t in range(NT):
                        tsl = slice(t * 128, (t + 1) * 128)
                        kts = self.causal_kts(
                            t,
                            lambda kt: [(kT.t[0:64, kt * 128:(kt + 1) * 128], kT[kt])],
                            lambda kt: (vx.t[:, kt, :], vx[kt]),
                            lo=max(0, t - 1), far_at=t - 1)
                        jobs.append(dict(q=[(qT.t[0:64, r, tsl], qT[(r, t)])], kt=kts, nv=65, scale=0.125,
                                         fin=self.fin_plain(otok.t[:, t, r * 64:(r + 1) * 64], otok[t], 64, extra=(esink.t[:, hh:hh + 1], esink[0]))))
                self.attend(jobs)
                self.pass_out(otok, ot, f"s{li}o{p}")
        kb.barrier()

    def reg_mla(self, li):
        self.reg_cols(f"a{li}d", "l1_mla_w_down", [(0, 416)])
        for sp in range(8):
            self.reg_cols(f"a{li}uq{sp}", "l1_mla_w_uq", [(sp * 192, 192)], kc=2)
            self.reg_cols(f"a{li}ukv{sp}", "l1_mla_w_ukv", [(sp * 256, 256)], kc=1)
            if sp % 2 == 1:
                self.reg_rows(f"a{li}o{sp // 2}", self.dram["l1_mla_w_out"], (sp // 2) * 256, 256)

    def mla(self, li):
        kb = self.kb
        d = self.dram
        with ExitStack() as ls:
            cqnT = kb.sb([128, 2, S], BF16, "cqnT", ls)
            ckvnT = kb.sb([128, S], BF16, "ckvnT", ls)
            krope = kb.sb([128, NT, 32], BF16, "krope", ls)
            qaT = kb.sb([128, 2, S], BF16, "qaT", ls)
            kaT = kb.sb([128, 2, S], BF16, "kaT", ls)
            vx = kb.sb([128, NT, 2, 65], BF16, "vx", ls)
            qtok = [kb.sb([128, 2, 96], BF16, "qtok", ls) for _ in range(2)]
            ktok = [kb.sb([128, 2, 96], BF16, "ktok", ls) for _ in range(2)]
            otok = kb.sb([128, NT, 256], BF16, "otok", ls)
            ot = kb.sb([128, 2, S], BF16, "ot", ls)
            dn = [kb.sb([128, 416], F32, "dn", ls) for _ in range(2)]
            junk = kb.sb([128, 256], F32, "junk", ls)
            gq = kb.sb([128, 384], F32, "gq", ls)
            nb = [kb.sb([128, 384], BF16, "nb", ls) for _ in range(2)]
            ss = [kb.sb([128, 6], F32, "ss", ls) for _ in range(2)]
            self.rtmp = kb.sb([128, 8, 64], F32, "rtmp", ls)
            self._ri = 0
            kb.dma("sp", gq.t[:, 0:256], d["l1_mla_q_norm"].partition_broadcast(128), writes=[gq[0]])
            kb.dma("sp", gq.t[:, 256:384], d["l1_mla_kv_norm"].partition_broadcast(128), writes=[gq[1]])
            kb.op("dve", lambda e: e.memset(vx.t[:, :, :, 64:65], 1.0), writes=[vx[t] for t in range(NT)])
            wd = self.ws.next(f"a{li}d")
            wdv = wd.t[:, 0:8 * 416].rearrange("p (c n) -> p c n", c=8)
            def pe_part0(t):
                bA = self.mbank()
                self.proj_tok(t, bA.t[:, 0:416], bA, wdv, wd, 416)
                return bA

            def mid_part0(t, bA):
                dn_ = dn[t % 2]
                ss_ = ss[t % 2]
                nb_ = nb[t % 2]
                kb.op("act", lambda e, bA=bA, dn_=dn_: e.copy(out=dn_.t[:], in_=bA.t[:, 0:416]), reads=[bA[0]], writes=[dn_[0]])
                for (i, c0, n) in ((0, 0, 256), (1, 256, 128)):
                    kb.op("dve", lambda e, c0=c0, n=n, dn_=dn_: e.tensor_tensor(out=junk.t[:, 0:n], in0=dn_.t[:, c0:c0 + n], in1=dn_.t[:, c0:c0 + n], op=ALU.mult),
                          reads=[dn_[0]], writes=[junk[0]])
                    kb.op("dve", lambda e, i=i, n=n, ss_=ss_: e.reduce_sum(out=ss_.t[:, i:i + 1], in_=junk.t[:, 0:n], axis=AX.X), reads=[junk[0]], writes=[ss_[i]])
                kb.op("dve", lambda e, ss_=ss_: e.tensor_scalar(out=ss_.t[:, 0:1], in0=ss_.t[:, 0:1], scalar1=1.0 / 256, scalar2=RMS_EPS, op0=ALU.mult, op1=ALU.add),
                      reads=[ss_[0]], writes=[ss_[0]])
                kb.op("dve", lambda e, ss_=ss_: e.tensor_scalar(out=ss_.t[:, 1:2], in0=ss_.t[:, 1:2], scalar1=1.0 / 128, scalar2=RMS_EPS, op0=ALU.mult, op1=ALU.add),
                      reads=[ss_[1]], writes=[ss_[1]])
                kb.op("act", lambda e, ss_=ss_: e.activation(out=ss_.t[:, 2:4], in_=ss_.t[:, 0:2], func=AF.Sqrt), reads=[ss_[0], ss_[1]], writes=[ss_[2]])
                kb.op("dve", lambda e, ss_=ss_: e.reciprocal(out=ss_.t[:, 4:6], in_=ss_.t[:, 2:4]), reads=[ss_[2]], writes=[ss_[3]])
                for (i, c0, n) in ((0, 0, 256), (1, 256, 128)):
                    kb.op("dve", lambda e, i=i, c0=c0, n=n, dn_=dn_, ss_=ss_, nb_=nb_: e.scalar_tensor_tensor(
                        out=nb_.t[:, c0:c0 + n], in0=dn_.t[:, c0:c0 + n], scalar=ss_.t[:, 4 + i:5 + i], in1=gq.t[:, c0:c0 + n], op0=ALU.mult, op1=ALU.mult),
                        reads=[dn_[0], ss_[3], gq[0], gq[1]], writes=[nb_[0]])
                self.rope(t, dn_.t[:, 384:416].rearrange("p (a b) -> p a b", a=1), dn_[0],
                          krope.t[:, t, :].rearrange("p (a b) -> p a b", a=1), krope[t], 1, 32, 0, 16, self.cosm, self.sinm)
                return self.transposes([(nb_.t[:, i * 128:(i + 1) * 128], nb_[0]) for i in range(3)])

            def fin_part0(t, bp):
                bT, pT = bp
                tsl = slice(t * 128, (t + 1) * 128)
                kb.op("dve", lambda e, pT=pT, tsl=tsl: e.tensor_copy(out=cqnT.t[:, :, tsl], in_=pT[:, 0:256].rearrange("p (a n) -> p a n", a=2)),
                      reads=[bT[0]], writes=[cqnT[t]])
                kb.op("dve", lambda e, pT=pT, tsl=tsl: e.tensor_copy(out=ckvnT.t[:, tsl], in_=pT[:, 256:384]), reads=[bT[0]], writes=[ckvnT[t]])
            self.skewed(NT, pe_part0, mid_part0, fin_part0)
            for sp in range(8):
                p = sp // 2
                hf2 = sp % 2
                wq = self.ws.next(f"a{li}uq{sp}")
                wkv = self.ws.next(f"a{li}ukv{sp}")
                wqv = wq.t[:, 0:384].rearrange("p (c n) -> p c n", c=2)
                wkvv = wkv.t[:, 0:256].rearrange("p (c n) -> p c n", c=1)
                def pe_part(t):
                    bA = self.mbank()
                    self.proj_tok(t, bA.t[:, 0:192], bA, wqv, wq, 192, kc=2, src=lambda c, t: (cqnT.t[:, c, t * 128:(t + 1) * 128], cqnT[t]))
                    bB = self.mbank()
                    self.proj_tok(t, bB.t[:, 0:256], bB, wkvv, wkv, 256, kc=1, src=lambda c, t: (ckvnT.t[:, t * 128:(t + 1) * 128], ckvnT[t]))
                    return bA, bB

                def mid_part(t, ab):
                    bA, bB = ab
                    q_ = qtok[t % 2]
                    k_ = ktok[t % 2]
                    self.rope(t, bA.t[:, 0:192].rearrange("p (a b) -> p a b", a=2), bA[0], q_.t[:], q_[0], 2, 96, 64, 16, self.cosm, self.sinm)
                    bB3 = bB.t[:, 0:256].rearrange("p (a b) -> p a b", a=2)
                    kb.op("act", lambda e, k_=k_, bB3=bB3: e.activation(out=k_.t[:, :, 0:64], in_=bB3[:, :, 0:64], func=AF.Identity), reads=[bB[0]], writes=[k_[0]])
                    kb.op("act", lambda e, t=t, bB3=bB3: e.activation(out=vx.t[:, t, :, 0:64], in_=bB3[:, :, 64:128], func=AF.Identity), reads=[bB[0]], writes=[vx[t]])
                    kb.op("dve", lambda e, t=t, k_=k_: e.tensor_copy(out=k_.t[:, :, 64:96], in_=krope.t[:, t:t + 1, :].broadcast_to([128, 2, 32])),
                          reads=[krope[t]], writes=[k_[1]])
                    return self.transposes([(q_.t[:, r, :], q_[0]) for r in range(2)] + [(k_.t[:, r, :], [k_[0], k_[1]]) for r in range(2)])

                def fin_part(t, bp):
                    bT, pT = bp
                    tsl = slice(t * 128, (t + 1) * 128)
                    kb.op("dve", lambda e, pT=pT, tsl=tsl: e.tensor_copy(out=qaT.t[0:96, :, tsl], in_=pT[0:96, 0:256].rearrange("p (a n) -> p a n", a=2)),
                          reads=[bT[0]], writes=[qaT[t]])
                    kb.op("dve", lambda e, pT=pT, tsl=tsl: e.tensor_copy(out=kaT.t[0:96, :, tsl], in_=pT[0:96, 256:512].rearrange("p (a n) -> p a n", a=2)),
                          reads=[bT[0]], writes=[kaT[t]])
                self.skewed(NT, pe_part, mid_part, fin_part)
                jobs = []
                for r in range(2):
                    for t in range(NT):
                        tsl = slice(t * 128, (t + 1) * 128)
                        kts = self.causal_kts(
                            t,
                            lambda kt, r=r: [(kaT.t[0:96, r, kt * 128:(kt + 1) * 128], kaT[kt])],
                            lambda kt, r=r: (vx.t[:, kt, r, :], vx[kt]))
                        oc = (hf2 * 2 + r) * 64
                        jobs.append(dict(q=[(qaT.t[0:96, r, tsl], qaT[t])], kt=kts, nv=65, scale=96.0 ** -0.5,
                                         fin=self.fin_plain(otok.t[:, t, oc:oc + 64], otok[t], 64)))
                self.attend(jobs)
                if hf2 == 1:
                    self.pass_out(otok, ot, f"a{li}o{p}")
        kb.barrier()

    def reg_nsa(self, li):
        W = "l0_nsa_w_in"
        w1 = self.dram["l0_nsa_cmp_w1"]
        for g in range(4):
            self.reg_cols(f"n_q{g}", W, [(g * 256, 256)])
            self.reg_cols(f"n_kv{g}", W, [(1024 + j * 256 + g * 64, 64) for j in range(6)] + [(2560 + j * 16 + g * 4, 4) for j in range(3)])

            def fn(buf):
                for j in range(2):
                    self.wdma(buf, buf.t[j * 64:(j + 1) * 64, 0:4096].rearrange("p (l h) -> p l h", l=32),
                              w1[j].rearrange("(l d) h -> d l h", d=64))
            self.ws.register(f"n_w1_{g}", fn)
            self.reg_rows(f"n_o{g}", self.dram["l0_nsa_w_out"], g * 256, 256)

    def nsa(self, li):
        kb = self.kb
        d = self.dram
        with ExitStack() as ls:
            qaT = kb.sb([128, 4, S], BF16, "qaT", ls)
            cvT = kb.sb([128, S], BF16, "cvT", ls)
            ksaT = kb.sb([128, S], BF16, "ksaT", ls)
            kwT = kb.sb([128, S], BF16, "kwT", ls)
            vsx = kb.sb([128, NT, 65], BF16, "vsx", ls)
            vwx = kb.sb([128, NT, 65], BF16, "vwx", ls)
            w2sb = kb.sb([128, 2, 64], BF16, "w2sb", ls)
            peT = kb.sb([128, 32], BF16, "peT", ls)
            cbias = kb.sb([128, 2], F32, "cbias", ls)
            hid = kb.sb([128, 2, 128], BF16, "hid", ls)
            kcT = kb.sb([128, 128], BF16, "kcT", ls)
            vcx = kb.sb([128, 97], BF16, "vcx", ls)
            gx = [kb.sb([128, 128], F32, "gx", ls) for _ in range(2)]
            cmpmask = kb.sb([128, S], BF16, "cmpmask", ls)
            imp = kb.sb([128, NT, 32], F32, "imp", ls)
            tA = kb.sb([128, NT, 32], F32, "tA", ls)
            tB = kb.sb([128, NT, 32], F32, "tB", ls)
            acc = [kb.sb([128, 4, 64], F32, "acc", ls) for _ in range(1)]
            otok = kb.sb([128, NT, 256], BF16, "otok", ls)
            ot = kb.sb([128, 2, S], BF16, "ot", ls)
            stgs = [kb.sb([128, 640], BF16, "stg", ls) for _ in range(2)]
            nmt = kb.sb([128, NT, 32], BF16, "nmt", ls)
            graw = kb.sb([128, NT, 12], F32, "graw", ls)
            gates = kb.sb([128, NT, 12], F32, "gates", ls)
            sco = [kb.sb([128, 32], F32, "sco", ls) for _ in range(2)]
            selm = [kb.sb([128, 32], F32, "selm", ls) for _ in range(2)]
            top8 = [kb.sb([128, 8], F32, "top8", ls) for _ in range(2)]
            self.rtmp = kb.sb([128, 8, 32], F32, "rtmp", ls)
            self._ri = 0
            kb.dma("pool", peT.t[:], d["l0_nsa_cmp_pe"][:, :], writes=[peT[0]])
            for j in range(2):
                kb.dma("pool", w2sb.t[:, j, :], d["l0_nsa_cmp_w2"][j], writes=[w2sb[j]])
            kb.dma("pool", cmpmask.t[:], d["c_cmpmask"][:, :], writes=[cmpmask[0]])
            kb.dma("pool", ksaT.t[64:96, :], d["c_e32"][:, :], writes=[ksaT["e"]])
            kb.dma("sp", tA.t[:], d["c_nsaA"][:, :, :], writes=[tA[0]])
            kb.dma("sp", tB.t[:], d["c_nsaB"][:, :, :], writes=[tB[0]])
            kb.op("dve", lambda e: e.memset(vsx.t[:, :, 64:65], 1.0), writes=[vsx[t] for t in range(NT)])
            kb.op("dve", lambda e: e.memset(vwx.t[:, :, 64:65], 1.0), writes=[vwx[t] for t in range(NT)])
            kb.op("dve", lambda e: e.memset(vcx.t[:], 0.0), writes=[vcx[0]])
            kb.dma("pool", vcx.t[:, 64:97], d["c_ovx"][:, :], reads=[], writes=[vcx[0]])
            kb.op("dve", lambda e: e.memset(kcT.t[:], 0.0), writes=[kcT[0]])
            kb.op("dve", lambda e: e.memset(hid.t[:], 0.0), writes=[hid[0], hid[1]])

            def fin_nsa(branch, r, t, g):
                acc_ = acc[0]

                def fin(ob):
                    self._fi = (self._fi + 1) % len(self.fin_s)
                    fs = self.fin_s[self._fi]
                    kb.op("dve", lambda e: e.tensor_scalar(out=fs.t[:, 0:1], in0=ob.t[:, 64:65], scalar1=1e-30, scalar2=None, op0=ALU.max),
                          reads=[ob[0]], writes=[fs[0]])
                    kb.op("dve", lambda e: e.reciprocal(out=fs.t[:, 1:2], in_=fs.t[:, 0:1]), reads=[fs[0]], writes=[fs[1]])
                    gc = branch * 4 + r
                    kb.op("dve", lambda e: e.tensor_tensor(out=fs.t[:, 2:3], in0=fs.t[:, 1:2], in1=gates.t[:, t, gc:gc + 1], op=ALU.mult),
                          reads=[fs[1], gates[0]], writes=[fs[2]])
                    if branch == 0:
                        kb.op("dve", lambda e: e.tensor_scalar(out=acc_.t[:, r, :], in0=ob.t[:, 0:64], scalar1=fs.t[:, 2:3], scalar2=None, op0=ALU.mult),
                              reads=[ob[0], fs[2]], writes=[acc_[r]])
                        if r == 0:
                            kb.op("dve", lambda e: e.tensor_scalar(out=imp.t[:, t, :], in0=ob.t[:, 65:97], scalar1=fs.t[:, 1:2], scalar2=None, op0=ALU.mult),
                                  reads=[ob[0], fs[1]], writes=[imp[t]])
                        else:
                            kb.op("dve", lambda e: e.scalar_tensor_tensor(out=imp.t[:, t, :], in0=ob.t[:, 65:97], scalar=fs.t[:, 1:2], in1=imp.t[:, t, :], op0=ALU.mult, op1=ALU.add),
                                  reads=[ob[0], fs[1], imp[t]], writes=[imp[t]])
                    elif branch == 2:
                        kb.op("dve", lambda e: e.scalar_tensor_tensor(out=acc_.t[:, r, :], in0=ob.t[:, 0:64], scalar=fs.t[:, 2:3], in1=acc_.t[:, r, :], op0=ALU.mult, op1=ALU.add),
                              reads=[ob[0], fs[2], acc_[r]], writes=[acc_[r]])
                    else:
                        kb.op("dve", lambda e: e.scalar_tensor_tensor(out=otok.t[:, t, r * 64:(r + 1) * 64], in0=ob.t[:, 0:64], scalar=fs.t[:, 2:3], in1=acc_.t[:, r, :], op0=ALU.mult, op1=ALU.add),
                              reads=[ob[0], fs[2], acc_[r]], writes=[otok[t]])
                return fin

            for g in range(4):
                wq = self.ws.next(f"n_q{g}")
                wkv = self.ws.next(f"n_kv{g}")
                wqv = wq.t[:, 0:2048].rearrange("p (c n) -> p c n", c=8)
                wkvv = wkv.t[:, 0:8 * 396].rearrange("p (c n) -> p c n", c=8)
                def pe_part(t):
                    bA = self.mbank()
                    self.proj_tok(t, bA.t[:, 0:256], bA, wqv, wq, 256)
                    bB = self.mbank()
                    self.proj_tok(t, bB.t[:, 0:396], bB, wkvv, wkv, 396)
                    return bA, bB

                def mid_part(t, ab):
                    bA, bB = ab
                    stg = stgs[t % 2]
                    self.rope(t, bA.t[:, 0:256].rearrange("p (a b) -> p a b", a=4), bA[0],
                              stg.t[:, 0:256].rearrange("p (a b) -> p a b", a=4), stg[0], 4, 64, 0, 8, self.cosp, self.sinp)
                    self.rope(t, bB.t[:, 0:384].rearrange("p (a b) -> p a b", a=3)[:, :, 0:64], bB[0],
                              stg.t[:, 256:640].rearrange("p (a b) -> p a b", a=3)[:, :, 0:64], stg[0], 3, 64, 0, 8, self.cosp, self.sinp)
                    kb.op("dve", lambda e, bB=bB: e.tensor_copy(out=stg.t[:, 320:384], in_=bB.t[:, 64:128]), reads=[bB[0]], writes=[stg[0]])
                    kb.op("dve", lambda e, bB=bB, t=t: e.tensor_copy(out=vsx.t[:, t, 0:64], in_=bB.t[:, 192:256]), reads=[bB[0]], writes=[vsx[t]])
                    kb.op("dve", lambda e, bB=bB, t=t: e.tensor_copy(out=vwx.t[:, t, 0:64], in_=bB.t[:, 320:384]), reads=[bB[0]], writes=[vwx[t]])
                    kb.op("dve", lambda e, bB=bB, t=t: e.tensor_copy(out=graw.t[:, t, :], in_=bB.t[:, 384:396]), reads=[bB[0]], writes=[graw[0]])
                    return self.transposes([(stg.t[:, 0:128], stg[0]), (stg.t[:, 128:256], stg[0]), (stg.t[:, 256:384], stg[0]),
                                            (stg.t[:, 384:448], stg[0]), (stg.t[:, 512:576], stg[0])])

                def fin_part(t, bp):
                    bT, pT = bp
                    tsl = slice(t * 128, (t + 1) * 128)
                    for par in range(2):
                        kb.op("dve", lambda e, par=par, pT=pT, tsl=tsl: e.tensor_copy(
                            out=qaT.t[0:64, par:4:2, tsl], in_=pT[par * 64:par * 64 + 64, 0:256].rearrange("p (a n) -> p a n", a=2)),
                            reads=[bT[0]], writes=[qaT[(par, t)], qaT[(par + 2, t)]])
                    kb.op("dve", lambda e, pT=pT, tsl=tsl: e.tensor_copy(out=cvT.t[:, tsl], in_=pT[:, 256:384]), reads=[bT[0]], writes=[cvT[t]])
                    kb.op("dve", lambda e, pT=pT, tsl=tsl: e.tensor_copy(out=ksaT.t[0:64, tsl], in_=pT[0:64, 384:512]), reads=[bT[0]], writes=[ksaT[t]])
                    kb.op("dve", lambda e, pT=pT, tsl=tsl: e.tensor_copy(out=kwT.t[0:64, tsl], in_=pT[0:64, 512:640]), reads=[bT[0]], writes=[kwT[t]])
                self.skewed(NT, pe_part, mid_part, fin_part)
                kb.op("act", lambda e: e.activation(out=gates.t[:], in_=graw.t[:], func=AF.Exp, scale=-1.0), reads=[graw[0]], writes=[gates[0]])
                kb.op("dve", lambda e: e.tensor_scalar(out=gates.t[:], in0=gates.t[:], scalar1=1.0, scalar2=None, op0=ALU.add), reads=[gates[0]], writes=[gates[0]])
                kb.op("dve", lambda e: e.reciprocal(out=gates.t[:], in_=gates.t[:]), reads=[gates[0]], writes=[gates[0]])
                w1 = self.ws.next(f"n_w1_{g}")
                w1v = w1.t[:, 0:4096].rearrange("p (l h) -> p l h", l=32)
                cv_all = [cvT[t] for t in range(NT)]
                if g == 0:
                    bank = self.mbank()
                    for j in range(2):
                        for l in range(32):
                            kb.op("pe", lambda e, j=j, l=l, bank=bank: e.matmul(bank.t[:, j:j + 1], lhsT=w1v[j * 64:(j + 1) * 64, l, :], rhs=peT.t[j * 64:(j + 1) * 64, l:l + 1], start=(l == 0), stop=(l == 31)),
                                  reads=[w1[0], peT[0]], writes=[bank[0]])
                    kb.op("dve", lambda e, bank=bank: e.tensor_copy(out=cbias.t[:], in_=bank.t[:, 0:2]), reads=[bank[0]], writes=[cbias[0]])
                for j in range(2):
                    bank = self.mbank()
                    for l in range(32):
                        kb.op("pe", lambda e, j=j, l=l, bank=bank: e.matmul(bank.t[:, 0:127], lhsT=w1v[j * 64:(j + 1) * 64, l, :], rhs=cvT.t[j * 64:(j + 1) * 64, l:l + 2017:16], start=(l == 0), stop=(l == 31)),
                              reads=[w1[0]] + cv_all, writes=[bank[0]])
                    xf, u_ = gx
                    e_ = u_
                    kb.op("dve", lambda e, j=j, bank=bank: e.tensor_scalar(out=xf.t[:, 0:127], in0=bank.t[:, 0:127], scalar1=cbias.t[:, j:j + 1], scalar2=None, op0=ALU.add),
                          reads=[bank[0], cbias[0]], writes=[xf[0]])
                    kb.op("dve", lambda e: e.tensor_tensor(out=u_.t[:, 0:127], in0=xf.t[:, 0:127], in1=xf.t[:, 0:127], op=ALU.mult), reads=[xf[0]], writes=[u_[0]])
                    kb.op("dve", lambda e: e.tensor_scalar(out=u_.t[:, 0:127], in0=u_.t[:, 0:127], scalar1=0.044715, scalar2=1.0, op0=ALU.mult, op1=ALU.add), reads=[u_[0]], writes=[u_[0]])
                    kb.op("dve", lambda e: e.tensor_tensor(out=u_.t[:, 0:127], in0=u_.t[:, 0:127], in1=xf.t[:, 0:127], op=ALU.mult), reads=[u_[0], xf[0]], writes=[u_[0]])
                    kb.op("act", lambda e: e.activation(out=e_.t[:, 0:127], in_=u_.t[:, 0:127], func=AF.Exp, scale=-1.5957691216), reads=[u_[0]], writes=[e_[0]])
                    kb.op("dve", lambda e: e.tensor_scalar(out=e_.t[:, 0:127], in0=e_.t[:, 0:127], scalar1=1.0, scalar2=None, op0=ALU.add), reads=[e_[0]], writes=[e_[0]])
                    kb.op("dve", lambda e: e.reciprocal(out=e_.t[:, 0:127], in_=e_.t[:, 0:127]), reads=[e_[0]], writes=[e_[0]])
                    kb.op("dve", lambda e, j=j: e.tensor_tensor(out=hid.t[:, j, 0:127], in0=xf.t[:, 0:127], in1=e_.t[:, 0:127], op=ALU.mult), reads=[xf[0], e_[0]], writes=[hid[j]])
                bank = self.mbank()
                kb.op("pe", lambda e, bank=bank: e.matmul(bank.t[0:64, 0:127], lhsT=w2sb.t[:, 0, :], rhs=hid.t[:, 0, 0:127], start=True, stop=True),
                      reads=[w2sb[0], hid[0]], writes=[bank[0]])
                kb.op("dve", lambda e, bank=bank: e.tensor_copy(out=kcT.t[0:64, 0:127], in_=bank.t[0:64, 0:127]), reads=[bank[0]], writes=[kcT[0]])
                bank = self.mbank()
                kb.op("pe", lambda e, bank=bank: e.matmul(bank.t[0:127, 0:64], lhsT=hid.t[:, 1, 0:127], rhs=w2sb.t[:, 1, :], start=True, stop=True),
                      reads=[w2sb[1], hid[1]], writes=[bank[0]])
                kb.op("dve", lambda e, bank=bank: e.tensor_copy(out=vcx.t[0:127, 0:64], in_=bank.t[0:127, 0:64]), reads=[bank[0]], writes=[vcx[0]])
                self.attn_small = True
                self.mbanks = [self.banks[5]]
                self._mi = 0
                for t in range(NT):
                    tsl = slice(t * 128, (t + 1) * 128)
                    jobs = []
                    for r in range(4):
                        jobs.append(dict(q=[(qaT.t[0:64, r, tsl], qaT[(r, t)])],
                                         kt=[dict(k=[(kcT.t[0:64, :], kcT[0])], v=(vcx.t[:, :], vcx[0]), mask=(cmpmask.t[:, tsl], cmpmask[0]))],
                                         nv=97, scale=0.125, fin=fin_nsa(0, r, t, g)))
                    self.attend(jobs)
                    sc_ = sco[t % 2]
                    sm_ = selm[t % 2]
                    tp_ = top8[t % 2]
                    kb.op("dve", lambda e, t=t, sc_=sc_: e.tensor_tensor(out=sc_.t[:], in0=imp.t[:, t, :], in1=tA.t[:, t, :], op=ALU.mult), reads=[imp[t], tA[0]], writes=[sc_[0]])
                    kb.op("dve", lambda e, t=t, sc_=sc_: e.tensor_tensor(out=sc_.t[:], in0=sc_.t[:], in1=tB.t[:, t, :], op=ALU.add), reads=[sc_[0], tB[0]], writes=[sc_[0]])
                    kb.op("dve", lambda e, sc_=sc_, tp_=tp_: e.max(out=tp_.t[:], in_=sc_.t[:]), reads=[sc_[0]], writes=[tp_[0]])
                    kb.op("dve", lambda e, sc_=sc_, tp_=tp_, sm_=sm_: e.tensor_scalar(out=sm_.t[:], in0=sc_.t[:], scalar1=tp_.t[:, 7:8], scalar2=None, op0=ALU.is_ge),
                          reads=[sc_[0], tp_[0]], writes=[sm_[0]])
                    kb.op("dve", lambda e, t=t, sm_=sm_: e.tensor_scalar(out=nmt.t[:, t, :], in0=sm_.t[:], scalar1=-1.0, scalar2=-NEG, op0=ALU.add, op1=ALU.mult),
                          reads=[sm_[0]], writes=[nmt[t]])
                    jobs = []
                    for r in range(4):
                        kts = self.causal_kts(t, lambda kt: [(kwT.t[0:64, kt * 128:(kt + 1) * 128], kwT[kt])], lambda kt: (vwx.t[:, kt, :], vwx[kt]),
                                              lo=max(0, t - 4), far_at=t - 4)
                        jobs.append(dict(q=[(qaT.t[0:64, r, tsl], qaT[(r, t)])], kt=kts, nv=65, scale=0.125, fin=fin_nsa(2, r, t, g)))
                    self.attend(jobs)
                    bT, pT = self.transposes([(nmt.t[:, t, :], nmt[t])])
                    kb.op("dve", lambda e, pT=pT, tsl=tsl: e.tensor_copy(out=qaT.t[64:96, :, tsl], in_=pT[0:32, 0:128].unsqueeze(1).broadcast_to([32, 4, 128])),
                          reads=[bT[0]], writes=[qaT[("m", t)]])
                    jobs = []
                    for r in range(4):
                        kts = self.causal_kts(t, lambda kt: [(ksaT.t[0:96, kt * 128:(kt + 1) * 128], [ksaT[kt], ksaT["e"]])], lambda kt: (vsx.t[:, kt, :], vsx[kt]))
                        jobs.append(dict(q=[(qaT.t[0:96, r, tsl], [qaT[(r, t)], qaT[("m", t)]])], kt=kts, nv=65, scale=0.125, fin=fin_nsa(1, r, t, g)))
                    self.attend(jobs)
                self.attn_small = False
                self.mbanks = self.banks[0:8]
                self.pass_out(otok, ot, f"n_o{g}")
        kb.barrier()


def _layout_weight(name, arr):
    a = np.asarray(arr)
    if name == "l0_nsa_cmp_pe":
        return np.ascontiguousarray(np.concatenate([a[0].T, a[1].T], axis=0)).astype(np.float32)
    if name.endswith("_moe_router"):
        return np.ascontiguousarray(a.T)
    shp = WSHAPES[name]
    return np.ascontiguousarray(a.reshape(shp))


_PROG_CACHE = {}


def _get_prog(plan_key, plan, dbg=None):
    p = _PROG_CACHE.get(plan_key)
    if p is None:
        p = Prog(plan, dbg)
        p.build()
        _PROG_CACHE[plan_key] = p
    return p


def run_plan(inputs, plan=None, cores=8, dbg=None, trace=False):
    key = (None if plan is None else tuple(plan), dbg)
    prog = _get_prog(key, plan, dbg)
    shared = dict(prog.consts)
    for k in prog.used:
        shared[k] = _layout_weight(k, inputs[k])
    x = np.asarray(inputs["x"], dtype=np.float32)
    mem = np.asarray(inputs["mem"], dtype=np.float32)
    pos = np.asarray(inputs["positions"]).astype(np.int32)
    in_maps = []
    for b in range(cores):
        m = dict(shared)
        m["x"] = np.ascontiguousarray(x[b])
        m["mem"] = np.ascontiguousarray(mem[b])
        m["posT"] = np.ascontiguousarray(pos[b].reshape(NT, 128).T)
        in_maps.append(m)
    res = run_bass_kernel_spmd(prog.nc, in_maps, core_ids=list(range(cores)), trace=trace)
    out = np.stack([np.asarray(r["out"]) for r in res.results], axis=0)
    return out, res


def kernel(**inputs):
    out, _ = run_plan(inputs, None, cores=8)
    return out.astype(np.float32)
```

```python
import numpy as np
import concourse.bass as bass
import concourse.mybir as mybir
from concourse.bass_utils import run_bass_kernel_spmd
from contextlib import ExitStack

F32 = mybir.dt.float32
BF16 = mybir.dt.bfloat16
I32 = mybir.dt.int32
AF = mybir.ActivationFunctionType
ALU = mybir.AluOpType
AX = mybir.AxisListType

S = 2048
D = 1024
NT = 16
DFF = 3584
NEXP = 8
ALPHA = 8.0 ** 0.25
LN_EPS = 1e-5
RMS_EPS = 1e-6
NEG = -30000.0
PI = float(np.pi)

EPOCH = 30000
NDS = 8


class Buf:
    __slots__ = ("w", "r", "psum")

    def __init__(self, psum=False):
        self.w = None
        self.r = {}
        self.psum = psum


class Tile:
    def __init__(self, t, psum=False):
        self.t = t
        self.parts = {}
        self.psum = psum

    def __getitem__(self, key):
        if self.psum:
            key = 0
        b = self.parts.get(key)
        if b is None:
            b = Buf(self.psum)
            self.parts[key] = b
        return b

    def all(self):
        return list(self.parts.values())


class KB:
    def __init__(self, nc, st):
        self.nc = nc
        self.st = st
        self.eng = {"pe": nc.tensor, "act": nc.scalar, "dve": nc.vector, "pool": nc.gpsimd, "sp": nc.sync}
        self.cnt = {e: 0 for e in ("pe", "act", "dve", "pool")}
        self.csems = {}
        self.seen = {e: {} for e in self.eng}
        self.dsems = {}
        self.dval = {}
        self.drr = {q: 0 for q in ("sp", "act", "pool")}
        self.nwait = 0
        self.nins = 0
        self.uid = 0

    def sb(self, shape, dt, name=None, st=None):
        self.uid += 1
        t = (st or self.st).enter_context(self.nc.sbuf_tensor(f"{name or 'sb'}_{self.uid}", list(shape), dt))
        return Tile(t)

    def ps(self, shape, dt, name=None):
        self.uid += 1
        t = self.st.enter_context(self.nc.psum_tensor(f"{name or 'ps'}_{self.uid}", list(shape), dt))
        return Tile(t, psum=True)

    def _csem(self, e, c):
        ep = (c - 1) // EPOCH
        k = (e, ep)
        s = self.csems.get(k)
        if s is None:
            s = self.st.enter_context(self.nc.semaphore(f"s_{e}_{ep}"))
            self.csems[k] = s
        return s, (c - 1) % EPOCH + 1

    def _dsem(self, key):
        s = self.dsems.get(key)
        if s is None:
            s = self.st.enter_context(self.nc.semaphore(f"d_{key[1]}_{key[2]}"))
            self.dsems[key] = s
            self.dval[key] = 0
        return s

    def _wait(self, e, k, v):
        eng = self.eng[e]
        if isinstance(k, tuple):
            eng.wait_ge(self._dsem(k), v)
        else:
            s, val = self._csem(k, v)
            eng.wait_ge(s, val)
        self.nwait += 1

    def _sync(self, e, reads, writes, is_dma=False):
        needs = {}

        def need(tok):
            if tok is None:
                return
            k, v = tok
            if needs.get(k, 0) < v:
                needs[k] = v

        for b in reads:
            need(b.w)
            if b.psum:
                for k, v in b.r.items():
                    if k != e:
                        need((k, v))
        for b in writes:
            if not (e == "pe" and not is_dma and b.w is not None and b.w[0] == "pe"):
                need(b.w)
            for k, v in b.r.items():
                if k != e or is_dma:
                    need((k, v))
        for k, v in needs.items():
            if self.seen[e].get(k, 0) >= v:
                continue
            self._wait(e, k, v)
            self.seen[e][k] = v

    def op(self, e, fn, reads=(), writes=()):
        reads = list(reads)
        writes = list(writes)
        self._sync(e, reads, writes)
        ins = fn(self.eng[e])
        self.cnt[e] += 1
        c = self.cnt[e]
        s, val = self._csem(e, c)
        ins.then_inc(s, 1)
        self.nins += 1
        for b in reads:
            b.r[e] = c
        for b in writes:
            b.w = (e, c)
            b.r = {}
        return ins

    def dma(self, q, out, in_, reads=(), writes=(), **kw):
        reads = list(reads)
        writes = list(writes)
        self._sync(q, reads, writes, is_dma=True)
        j = self.drr[q]
        self.drr[q] = (j + 1) % NDS
        key = ("d", q, j)
        s = self._dsem(key)
        prev = self.dval[key]
        if prev > 0 and self.seen[q].get(key, 0) < prev:
            self._wait(q, key, prev)
            self.seen[q][key] = prev
        ins = self.eng[q].dma_start(out=out, in_=in_, **kw)
        ins.then_inc(s, 16)
        self.nins += 1
        v = prev + 16
        self.dval[key] = v
        for b in reads:
            b.r[key] = v
        for b in writes:
            b.w = (key, v)
            b.r = {}
        return (key, v)

    def barrier(self):
        toks = [(e, c) for e, c in self.cnt.items() if c > 0]
        toks += [(k, v) for k, v in self.dval.items() if v > 0]
        for e in ("pe", "act", "dve", "pool", "sp"):
            for k, v in toks:
                if k == e:
                    continue
                if self.seen[e].get(k, 0) < v:
                    self._wait(e, k, v)
                    self.seen[e][k] = v

    def finish(self, toks, e="sp"):
        for k, v in toks:
            if self.seen[e].get(k, 0) < v:
                self._wait(e, k, v)
                self.seen[e][k] = v


class WStream:
    def __init__(self, kb, nbuf):
        self.kb = kb
        self.bufs = [kb.sb([128, 4096], BF16, f"wb{i}") for i in range(nbuf)]
        self.descs = []
        self.issued = 0
        self.cur = 0

    def register(self, tag, fn):
        self.descs.append((tag, fn))

    def next(self, tag):
        i = self.cur
        assert self.descs[i][0] == tag, (i, self.descs[i][0], tag)
        n = len(self.bufs)
        while self.issued < len(self.descs) and self.issued <= i + n - 2:
            j = self.issued
            self.descs[j][1](self.bufs[j % n])
            self.issued += 1
        self.cur += 1
        return self.bufs[i % n]


def _consts():
    c = {}
    c["c_ident"] = np.eye(128, dtype=np.float32)
    k = np.arange(128)[:, None]
    q = np.arange(128)[None, :]
    c["c_causal"] = np.where(k <= q, 0.0, NEG).astype(np.float32)
    c["c_far"] = np.where(k > q, 0.0, NEG).astype(np.float32)
    cc = np.arange(128)[:, None]
    qq = np.arange(S)[None, :]
    c["c_cmpmask"] = np.where((cc <= 126) & (cc * 16 + 31 <= qq), 0.0, NEG).astype(np.float32)
    kk = np.arange(S)[None, :]
    j32 = np.arange(32)[:, None]
    c["c_e32"] = (kk // 64 == j32).astype(np.float32)
    c["c_e8"] = ((kk // 256 == j32) & (j32 < 8)).astype(np.float32)
    c_start = np.arange(127) * 16
    b_start = np.arange(32) * 64
    ov = np.clip(np.minimum(c_start[:, None] + 32, b_start[None, :] + 64) - np.maximum(c_start[:, None], b_start[None, :]), 0, None) / 32.0
    ovx = np.zeros((128, 33), np.float32)
    ovx[:127, 0] = 1.0
    ovx[:127, 1:] = ov
    c["c_ovx"] = ovx
    tpos = np.arange(S)
    cur = (tpos // 64)[:, None]
    blk = np.arange(32)[None, :]
    forced = (blk == 0) | (blk == cur) | (blk == cur - 1)
    valid = blk <= cur
    A = (valid & ~forced).astype(np.float32)
    Bt = np.where(valid, np.where(forced, 1e4, 0.0), -1e30).astype(np.float32)
    c["c_nsaA"] = np.ascontiguousarray(A.reshape(16, 128, 32).transpose(1, 0, 2))
    c["c_nsaB"] = np.ascontiguousarray(Bt.reshape(16, 128, 32).transpose(1, 0, 2))
    own = (tpos // 256)[:, None]
    j8 = np.arange(8)[None, :]
    mv = (j8 < own).astype(np.float32)
    c["c_mobaB"] = np.ascontiguousarray(np.where(j8 < own, 0.0, -1e30).astype(np.float32).reshape(16, 128, 8).transpose(1, 0, 2))
    c["c_mobaV"] = np.ascontiguousarray(mv.reshape(16, 128, 8).transpose(1, 0, 2))
    c["c_mobaO"] = np.ascontiguousarray((j8 == own).astype(np.float32).reshape(16, 128, 8).transpose(1, 0, 2))
    c["c_invp"] = (500000.0 ** (-np.arange(0, 16, 2, dtype=np.float32) / 16)).astype(np.float32).reshape(1, 8)
    c["c_invm"] = (500000.0 ** (-np.arange(0, 32, 2, dtype=np.float32) / 32)).astype(np.float32).reshape(1, 16)
    return c


WSHAPES = {
    "l0_nsa_w_in": (1024, 2608), "l0_nsa_cmp_pe": (128, 32), "l0_nsa_cmp_w1": (2, 2048, 128),
    "l0_nsa_cmp_w2": (2, 128, 64), "l0_nsa_w_out": (1024, 1024),
    "l1_mla_w_down": (1024, 416), "l1_mla_q_norm": (1, 256), "l1_mla_kv_norm": (1, 128),
    "l1_mla_w_uq": (256, 1536), "l1_mla_w_ukv": (128, 2048), "l1_mla_w_out": (1024, 1024),
    "l2_moba_w_in": (1024, 3072), "l2_moba_w_out": (1024, 1024),
    "l3_swa_w_in": (1024, 1280), "l3_swa_sinks": (1, 16), "l3_swa_w_out": (1024, 1024),
}
for _i in range(4):
    for _k in (1, 2, 3):
        WSHAPES[f"l{_i}_ln{_k}_g"] = (1, 1024)
        WSHAPES[f"l{_i}_ln{_k}_b"] = (1, 1024)
    WSHAPES[f"l{_i}_xq"] = (1024, 1024)
    WSHAPES[f"l{_i}_xkv"] = (1024, 2048)
    WSHAPES[f"l{_i}_xo"] = (1024, 1024)
    if _i % 2 == 0:
        WSHAPES[f"l{_i}_ffn_w_in"] = (1024, 7168)
        WSHAPES[f"l{_i}_ffn_w_out"] = (3584, 1024)
    else:
        WSHAPES[f"l{_i}_moe_router"] = (8, 1024)
        WSHAPES[f"l{_i}_moe_bias"] = (1, 8)
        WSHAPES[f"l{_i}_moe_w_in"] = (8, 1024, 7168)
        WSHAPES[f"l{_i}_moe_w_out"] = (8, 3584, 1024)


class Prog:
    def __init__(self, plan=None, debug_ffn_experts=None):
        if plan is None:
            plan = []
            for li in range(4):
                plan += [("mixer", li), ("ln", li, 1), ("xattn", li), ("ln", li, 2), ("ffn", li), ("ln", li, 3)]
        self.plan = list(plan)
        self.dbg_experts = debug_ffn_experts
        self.nc = bass.Bass("TRN2", target_bir_lowering=False)
        nc = self.nc
        self.dram = {}

        def din(name, shape, dt=F32):
            self.dram[name] = nc.dram_tensor(name, list(shape), dt, kind="ExternalInput").ap()

        din("x", [S, D])
        din("mem", [256, D])
        din("posT", [128, NT], I32)
        self.consts = _consts()
        for k, v in self.consts.items():
            din(k, v.shape)
        self.used = []
        for k, shp in WSHAPES.items():
            li = int(k[1])
            need = False
            for stp in self.plan:
                if stp[1] != li:
                    continue
                if stp[0] == "mixer" and any(s in k for s in ("nsa", "mla", "moba", "swa")):
                    need = True
                if stp[0] == "ln" and f"_ln{stp[2]}_" in k:
                    need = True
                if stp[0] == "xattn" and any(k.endswith(s) for s in ("_xq", "_xkv", "_xo")):
                    need = True
                if stp[0] == "ffn" and ("ffn" in k or "moe" in k):
                    need = True
            if need:
                din(k, shp)
                self.used.append(k)
        self.out = nc.dram_tensor("out", [S, D], F32, kind="ExternalOutput").ap()

    def sbank(self):
        self._si = (self._si + 1) % len(self.sbanks)
        return self.sbanks[self._si]

    def obank(self):
        self._oi = (self._oi + 1) % len(self.obanks)
        return self.obanks[self._oi]

    def mbank(self):
        self._mi = (self._mi + 1) % len(self.mbanks)
        return self.mbanks[self._mi]

    def pbuf(self):
        self._pi = (self._pi + 1) % len(self.pbufs)
        return self.pbufs[self._pi]

    def hT_reads(self, t0, t1):
        return [self.hT[t] for t in range(t0, t1)]

    def build(self):
        nc = self.nc
        with ExitStack() as st:
            self.st = st
            kb = self.kb = KB(nc, st)
            self.h = kb.sb([128, NT, D], F32, "h")
            self.hT = kb.sb([128, 8, S], BF16, "hT")
            self.ws = WStream(kb, 4)
            self.ident = kb.sb([128, 128], BF16, "ident")
            self.causal = kb.sb([128, 128], BF16, "causal")
            self.far = kb.sb([128, 128], BF16, "far")
            self.memT = kb.sb([128, 8, 256], BF16, "memT")
            self.cosp = kb.sb([128, NT, 8], F32, "cosp")
            self.sinp = kb.sb([128, NT, 8], F32, "sinp")
            self.cosm = kb.sb([128, NT, 16], F32, "cosm")
            self.sinm = kb.sb([128, NT, 16], F32, "sinm")
            self.pbufs = [kb.sb([128, 512], BF16, "pb") for _ in range(3)]
            self.fin_s = [kb.sb([128, 4], F32, "fs") for _ in range(4)]
            self._fi = 0
            banks = [kb.ps([128, 512], F32, f"bank{i}") for i in range(8)]
            self.banks = banks
            self.attn_small = False
            self.sbanks = banks[0:6]
            self.obanks = banks[6:8]
            self.mbanks = banks[0:8]
            self._si = self._oi = self._mi = self._pi = 0
            self.out_toks = []
            for stp in self.plan:
                if stp[0] == "mixer":
                    [self.reg_nsa, self.reg_mla, self.reg_moba, self.reg_swa][stp[1]](stp[1])
                elif stp[0] == "xattn":
                    self.reg_xattn(stp[1])
                elif stp[0] == "ffn":
                    self.reg_ffn(stp[1])
            self.setup()
            for i, stp in enumerate(self.plan):
                last = (i == len(self.plan) - 1)
                if stp[0] == "mixer":
                    [self.nsa, self.mla, self.moba, self.swa][stp[1]](stp[1])
                elif stp[0] == "xattn":
                    self.xattn(stp[1])
                elif stp[0] == "ffn":
                    self.ffn(stp[1])
                else:
                    self.ln(stp[1], stp[2], last=last)
            self.store()
            kb.finish(self.out_toks)
            self.stats = (kb.nins, kb.nwait, dict(kb.cnt))
        return nc

    def wdma(self, buf, view, src):
        self.kb.dma("pool", view, src, writes=[buf[0]])

    def reg_cols(self, tag, wname, col_segs, kc=8, rows=None):
        ncols = sum(n for _, n in col_segs)
        W = self.dram[wname]

        def fn(buf):
            v = buf.t[:, 0:kc * ncols].rearrange("p (c n) -> p c n", c=kc)
            o = 0
            for (c0, n) in col_segs:
                src = W[:, c0:c0 + n] if rows is None else W[rows[0]:rows[1], c0:c0 + n]
                self.wdma(buf, v[:, :, o:o + n], src.rearrange("(c p) n -> p c n", p=128))
                o += n
        self.ws.register(tag, fn)

    def reg_rows(self, tag, W, r0, nrows, ncols=1024):
        kc = nrows // 128

        def fn(buf):
            v = buf.t[:, 0:kc * ncols].rearrange("p (c n) -> p c n", c=kc)
            self.wdma(buf, v, W[r0:r0 + nrows, :].rearrange("(c p) n -> p c n", p=128))
        self.ws.register(tag, fn)

    def setup(self):
        kb = self.kb
        d = self.dram
        kb.dma("pool", self.ident.t[:], d["c_ident"][:, :], writes=[self.ident[0]])
        kb.dma("pool", self.causal.t[:], d["c_causal"][:, :], writes=[self.causal[0]])
        kb.dma("pool", self.far.t[:], d["c_far"][:, :], writes=[self.far[0]])
        with ExitStack() as ls:
            self.hb = [kb.sb([128, D], BF16, "hb", ls) for _ in range(2)]
            posi = kb.sb([128, NT], I32, "posi", ls)
            posf = kb.sb([128, NT], F32, "posf", ls)
            inv = kb.sb([128, 24], F32, "inv", ls)
            ang = kb.sb([128, NT, 16], F32, "ang", ls)
            tmp = kb.sb([128, NT, 16], F32, "angt", ls)
            tmp2 = kb.sb([128, NT, 16], F32, "angt2", ls)
            ki = kb.sb([128, NT, 16], I32, "angk", ls)
            memb = kb.sb([128, 2, D], BF16, "memb", ls)
            negpi = kb.sb([128, 1], F32, "negpi", ls)
            kb.op("dve", lambda e: e.memset(negpi.t[:], -PI), writes=[negpi[0]])
            kb.dma("sp", posi.t[:], d["posT"][:, :], writes=[posi[0]])
            kb.dma("sp", inv.t[:, 0:8], d["c_invp"].partition_broadcast(128), writes=[inv[0]])
            kb.dma("sp", inv.t[:, 8:24], d["c_invm"].partition_broadcast(128), writes=[inv[1]])
            kb.op("dve", lambda e: e.tensor_copy(out=posf.t[:], in_=posi.t[:]), reads=[posi[0]], writes=[posf[0]])
            import os
            SK = os.environ.get("SKIP", "")
            for (n, o, cs, sn) in (() if "rope" in SK else ((8, 0, self.cosp, self.sinp), (16, 8, self.cosm, self.sinm))):
                a3 = ang.t[:, :, 0:n]
                t3 = tmp.t[:, :, 0:n]
                kb.op("dve", lambda e, a3=a3, n=n, o=o: e.tensor_tensor(
                    out=a3, in0=posf.t[:].unsqueeze(2).broadcast_to([128, NT, n]),
                    in1=inv.t[:, o:o + n].unsqueeze(1).broadcast_to([128, NT, n]), op=ALU.mult),
                    reads=[posf[0], inv[0], inv[1]], writes=[ang[0]])
                for (shift, dst) in ((0.0, sn), (0.5 * PI, cs)):
                    k3 = ki.t[:, :, 0:n]
                    m3 = tmp2.t[:, :, 0:n]
                    kb.op("dve", lambda e, t3=t3, a3=a3, shift=shift: e.tensor_scalar(
                        out=t3, in0=a3, scalar1=shift, scalar2=None, op0=ALU.add), reads=[ang[0]], writes=[tmp[0]])
                    kb.op("dve", lambda e, t3=t3, m3=m3: e.tensor_scalar(
                        out=m3, in0=t3, scalar1=1.0 / (2 * PI), scalar2=None, op0=ALU.mult), reads=[tmp[0]], writes=[tmp2[0]])
                    kb.op("dve", lambda e, k3=k3, m3=m3: e.tensor_copy(out=k3, in_=m3), reads=[tmp2[0]], writes=[ki[0]])
                    kb.op("dve", lambda e, k3=k3, m3=m3: e.tensor_copy(out=m3, in_=k3), reads=[ki[0]], writes=[tmp2[0]])
                    kb.op("dve", lambda e, t3=t3, m3=m3: e.scalar_tensor_tensor(
                        out=t3, in0=m3, scalar=-2 * PI, in1=t3, op0=ALU.mult, op1=ALU.add), reads=[tmp2[0], tmp[0]], writes=[tmp[0]])
                    kb.op("dve", lambda e, t3=t3, m3=m3: e.tensor_scalar(
                        out=m3, in0=t3, scalar1=PI, scalar2=-2 * PI, op0=ALU.is_gt, op1=ALU.mult), reads=[tmp[0]], writes=[tmp2[0]])
                    kb.op("dve", lambda e, t3=t3, m3=m3: e.tensor_tensor(out=t3, in0=t3, in1=m3, op=ALU.add), reads=[tmp[0], tmp2[0]], writes=[tmp[0]])
                    kb.op("dve", lambda e, t3=t3, m3=m3: e.tensor_scalar(
                        out=m3, in0=t3, scalar1=-PI, scalar2=2 * PI, op0=ALU.is_lt, op1=ALU.mult), reads=[tmp[0]], writes=[tmp2[0]])
                    kb.op("dve", lambda e, t3=t3, m3=m3: e.tensor_tensor(out=t3, in0=t3, in1=m3, op=ALU.add), reads=[tmp[0], tmp2[0]], writes=[tmp[0]])
                    kb.op("act", lambda e, t3=t3, dst=dst: e.activation(out=dst.t[:], in_=t3, func=AF.Sin), reads=[tmp[0]], writes=[dst[0]])
            for i in range(2):
                kb.dma("pool", memb.t[:, i, :], d["mem"][i * 128:(i + 1) * 128, :], writes=[memb[i]])
            for i in (() if "mem" in SK else range(2)):
                bank = self.mbank()
                pT = bank.t[:].bitcast(BF16)
                for c in range(8):
                    kb.op("pe", lambda e, c=c, i=i, pT=pT: e.transpose(out=pT[:, c * 128:(c + 1) * 128], in_=memb.t[:, i, c * 128:(c + 1) * 128], identity=self.ident.t[:]),
                          reads=[memb[i], self.ident[0]], writes=[bank[0]])
                kb.op("dve", lambda e, i=i, pT=pT: e.tensor_copy(out=self.memT.t[:, :, i * 128:(i + 1) * 128], in_=pT.rearrange("p (c n) -> p c n", c=8)),
                      reads=[bank[0]], writes=[self.memT[i]])
            for t in range(NT):
                kb.dma("sp", self.h.t[:, t, :], d["x"][t * 128:(t + 1) * 128, :], writes=[self.h[t]])
            for t in (() if "post" in SK else range(NT)):
                self.post_ln(t)
            if "bar" not in SK:
                kb.barrier()

    def post_ln(self, t):
        kb = self.kb
        ht = self.h.t[:, t, :]
        hb = self.hb[t % 2]
        kb.op("act", lambda e: e.copy(out=hb.t[:], in_=ht), reads=[self.h[t]], writes=[hb[0]])
        kb.op("act", lambda e: e.mul(out=ht, in_=ht, mul=ALPHA), reads=[self.h[t]], writes=[self.h[t]])
        bank = self.mbank()
        pT = bank.t[:].bitcast(BF16)
        for c in range(8):
            kb.op("pe", lambda e, c=c: e.transpose(out=pT[:, c * 128:(c + 1) * 128], in_=hb.t[:, c * 128:(c + 1) * 128], identity=self.ident.t[:]),
                  reads=[hb[0], self.ident[0]], writes=[bank[0]])
        kb.op("dve", lambda e: e.tensor_copy(out=self.hT.t[:, :, t * 128:(t + 1) * 128], in_=pT.rearrange("p (c n) -> p c n", c=8)),
              reads=[bank[0]], writes=[self.hT[t]])

    def ln(self, li, k, last=False):
        with ExitStack() as ls:
            kb = self.kb
            self.lnp = kb.sb([128, 2, D], F32, "lnp", ls)
            self.lnst = [kb.sb([128, 2, 6], F32, "lnst", ls) for _ in range(4)]
            self.lnmv = [kb.sb([128, 8], F32, "lnmv", ls) for _ in range(4)]
            self.hb = [kb.sb([128, D], BF16, "hb", ls) for _ in range(2)]
            self._ln(li, k, last)
            self.kb.barrier()

    def _ln(self, li, k, last=False):
        kb = self.kb
        d = self.dram
        lnp = self.lnp
        kb.dma("sp", lnp.t[:, 0, :], d[f"l{li}_ln{k}_g"].partition_broadcast(128), writes=[lnp[0]])
        kb.dma("sp", lnp.t[:, 1, :], d[f"l{li}_ln{k}_b"].partition_broadcast(128), writes=[lnp[1]])
        if not last:
            for i in range(2):
                kb.op("dve", lambda e, i=i: e.tensor_scalar(out=lnp.t[:, i, :], in0=lnp.t[:, i, :], scalar1=ALPHA, scalar2=None, op0=ALU.mult),
                      reads=[lnp[i]], writes=[lnp[i]])
        NB = len(self.lnst)
        banks = {}

        def A1(t):
            ht, hbuf, st_, mv = self.h.t[:, t, :], self.h[t], self.lnst[t % NB], self.lnmv[t % NB]
            for i in range(2):
                kb.op("dve", lambda e, i=i: e.bn_stats(out=st_.t[:, i, :], in_=ht[:, i * 512:(i + 1) * 512]), reads=[hbuf], writes=[st_[i]])
            kb.op("dve", lambda e: e.bn_aggr(out=mv.t[:, 0:2], in_=st_.t[:]), reads=[st_[0], st_[1]], writes=[mv[0]])
            kb.op("dve", lambda e: e.tensor_scalar(out=mv.t[:, 3:4], in0=mv.t[:, 1:2], scalar1=LN_EPS, scalar2=None, op0=ALU.add),
                  reads=[mv[0]], writes=[mv[2]])
            kb.op("act", lambda e: e.activation(out=mv.t[:, 3:4], in_=mv.t[:, 3:4], func=AF.Sqrt), reads=[mv[2]], writes=[mv[2]])

        def A2BC(t):
            ht, hbuf, mv = self.h.t[:, t, :], self.h[t], self.lnmv[t % NB]
            kb.op("dve", lambda e: e.reciprocal(out=mv.t[:, 2:3], in_=mv.t[:, 3:4]), reads=[mv[2]], writes=[mv[1]])
            kb.op("dve", lambda e: e.scalar_tensor_tensor(out=mv.t[:, 4:5], in0=mv.t[:, 0:1], scalar=-1.0, in1=mv.t[:, 2:3], op0=ALU.mult, op1=ALU.mult),
                  reads=[mv[0], mv[1]], writes=[mv[3]])
            kb.op("act", lambda e: e.activation(out=ht, in_=ht, func=AF.Identity, scale=mv.t[:, 2:3], bias=mv.t[:, 4:5]),
                  reads=[hbuf, mv[1], mv[3]], writes=[hbuf])
            kb.op("pool", lambda e: e.tensor_tensor(out=ht, in0=ht, in1=lnp.t[:, 0, :], op=ALU.mult), reads=[hbuf, lnp[0]], writes=[hbuf])

        def DEF(t):
            ht, hbuf = self.h.t[:, t, :], self.h[t]
            kb.op("dve", lambda e: e.tensor_tensor(out=ht, in0=ht, in1=lnp.t[:, 1, :], op=ALU.add), reads=[hbuf, lnp[1]], writes=[hbuf])
            if last:
                return
            hb = self.hb[t % 2]
            kb.op("act", lambda e: e.activation(out=hb.t[:], in_=ht, func=AF.Identity, scale=1.0 / ALPHA), reads=[hbuf], writes=[hb[0]])
            bank = self.mbank()
            pT = bank.t[:].bitcast(BF16)
            for c in range(8):
                kb.op("pe", lambda e, c=c: e.transpose(out=pT[:, c * 128:(c + 1) * 128], in_=hb.t[:, c * 128:(c + 1) * 128], identity=self.ident.t[:]),
                      reads=[hb[0], self.ident[0]], writes=[bank[0]])
            banks[t] = (bank, pT)

        def G(t):
            if last:
                return
            bank, pT = banks.pop(t)
            kb.op("dve", lambda e: e.tensor_copy(out=self.hT.t[:, :, t * 128:(t + 1) * 128], in_=pT.rearrange("p (c n) -> p c n", c=8)),
                  reads=[bank[0]], writes=[self.hT[t]])

        for s in range(NT + 3):
            if s < NT:
                A1(s)
            if 0 <= s - 1 < NT:
                A2BC(s - 1)
            if 0 <= s - 2 < NT:
                DEF(s - 2)
            if 0 <= s - 3 < NT:
                G(s - 3)

    def store(self):
        kb = self.kb
        for t in range(NT):
            tok = kb.dma("sp", self.out[t * 128:(t + 1) * 128, :], self.h.t[:, t, :], reads=[self.h[t]])
            self.out_toks.append(tok)

    def attend(self, jobs):
        kb = self.kb
        ident = self.ident
        self.sbanks = self.banks[0:5] if self.attn_small else self.banks[0:6]
        self.obanks = self.banks[6:8]

        def bl(x):
            return list(x) if isinstance(x, (list, tuple)) else [x]
        groups = []
        for job in jobs:
            kts = job["kt"]
            n = len(kts)
            for g0 in range(0, n, 4):
                groups.append((job, kts[g0:g0 + 4], g0 == 0, g0 + 4 >= n))

        def emit_qk(grp):
            job, kts, first, last = grp
            sbk = self.sbank()
            for j, kt in enumerate(kts):
                reg = sbk.t[:, j * 128:(j + 1) * 128]
                nch = len(kt["k"])
                for ci in range(nch):
                    ka, kbuf = kt["k"][ci]
                    qa, qbuf = job["q"][ci]
                    lastmm = (ci == nch - 1) and kt.get("mask") is None
                    kb.op("pe", lambda e, reg=reg, ka=ka, qa=qa, s=(ci == 0), l=lastmm: e.matmul(reg, lhsT=ka, rhs=qa, start=s, stop=l),
                          reads=bl(kbuf) + bl(qbuf), writes=[sbk[0]])
                if kt.get("mask") is not None:
                    ma, mbuf = kt["mask"]
                    kb.op("pe", lambda e, reg=reg, ma=ma: e.matmul(reg, lhsT=ident.t[:], rhs=ma, start=False, stop=True),
                          reads=[ident[0]] + bl(mbuf), writes=[sbk[0]])
            return sbk

        def emit_exp(grp, sbk):
            job, kts, first, last = grp
            p = self.pbuf()
            w = len(kts) * 128
            kb.op("act", lambda e: e.activation(out=p.t[:, :w], in_=sbk.t[:, :w], func=AF.Exp, scale=job["scale"]),
                  reads=[sbk[0]], writes=[p[0]])
            return p

        def emit_pv(grp, p):
            job, kts, first, last = grp
            if first:
                job["_ob"] = self.obank()
            ob = job["_ob"]
            nv = job["nv"]
            for j, kt in enumerate(kts):
                va, vbuf = kt["v"]
                kb.op("pe", lambda e, j=j, va=va, s=(first and j == 0), l=(last and j == len(kts) - 1):
                      e.matmul(ob.t[:, :nv], lhsT=p.t[:, j * 128:(j + 1) * 128], rhs=va, start=s, stop=l),
                      reads=[p[0]] + bl(vbuf), writes=[ob[0]])
            if last:
                job["fin"](ob)

        pend = None
        for grp in groups:
            sbk = emit_qk(grp)
            p = emit_exp(grp, sbk)
            if pend is not None:
                emit_pv(*pend)
            pend = (grp, p)
        if pend is not None:
            emit_pv(*pend)

    def fin_plain(self, dst_ap, dst_buf, dv, extra=None):
        kb = self.kb

        def fin(ob):
            self._fi = (self._fi + 1) % len(self.fin_s)
            fs = self.fin_s[self._fi]
            if extra is None:
                kb.op("dve", lambda e: e.tensor_scalar(out=fs.t[:, 0:1], in0=ob.t[:, dv:dv + 1], scalar1=1e-30, scalar2=None, op0=ALU.max),
                      reads=[ob[0]], writes=[fs[0]])
            else:
                ea, ebuf = extra
                kb.op("dve", lambda e: e.tensor_scalar(out=fs.t[:, 0:1], in0=ob.t[:, dv:dv + 1], scalar1=ea, scalar2=None, op0=ALU.add),
                      reads=[ob[0], ebuf], writes=[fs[0]])
            kb.op("dve", lambda e: e.reciprocal(out=fs.t[:, 1:2], in_=fs.t[:, 0:1]), reads=[fs[0]], writes=[fs[1]])
            kb.op("dve", lambda e: e.tensor_scalar(out=dst_ap, in0=ob.t[:, 0:dv], scalar1=fs.t[:, 1:2], scalar2=None, op0=ALU.mult),
                  reads=[ob[0], fs[1]], writes=[dst_buf])
        return fin

    def rope(self, t, src3, srcbuf, dst3, dstbuf, nh, hd, ro, half, cs, sn):
        kb = self.kb
        x1 = src3[:, :, ro:ro + half]
        x2 = src3[:, :, ro + half:ro + 2 * half]
        cb = cs.t[:, t:t + 1, :].broadcast_to([128, nh, half])
        sb_ = sn.t[:, t:t + 1, :].broadcast_to([128, nh, half])
        tm = self.rtmp
        self._ri = (self._ri + 1) % 2
        base = self._ri * 4
        tv = [tm.t[:, base + i, 0:nh * half].rearrange("p (a b) -> p a b", a=nh) for i in range(4)]
        tb = [tm[base + i] for i in range(4)]
        kb.op("dve", lambda e: e.tensor_tensor(out=tv[0], in0=x1, in1=cb, op=ALU.mult), reads=[srcbuf, cs[0]], writes=[tb[0]])
        kb.op("dve", lambda e: e.tensor_tensor(out=tv[1], in0=x2, in1=sb_, op=ALU.mult), reads=[srcbuf, sn[0]], writes=[tb[1]])
        kb.op("dve", lambda e: e.tensor_tensor(out=tv[2], in0=x2, in1=cb, op=ALU.mult), reads=[srcbuf, cs[0]], writes=[tb[2]])
        kb.op("dve", lambda e: e.tensor_tensor(out=tv[3], in0=x1, in1=sb_, op=ALU.mult), reads=[srcbuf, sn[0]], writes=[tb[3]])
        kb.op("dve", lambda e: e.tensor_tensor(out=dst3[:, :, ro:ro + half], in0=tv[0], in1=tv[1], op=ALU.subtract), reads=[tb[0], tb[1]], writes=[dstbuf])
        kb.op("dve", lambda e: e.tensor_tensor(out=dst3[:, :, ro + half:ro + 2 * half], in0=tv[2], in1=tv[3], op=ALU.add), reads=[tb[2], tb[3]], writes=[dstbuf])
        if ro > 0:
            kb.op("dve", lambda e: e.tensor_copy(out=dst3[:, :, 0:ro], in_=src3[:, :, 0:ro]), reads=[srcbuf], writes=[dstbuf])
        if ro + 2 * half < hd:
            kb.op("dve", lambda e: e.tensor_copy(out=dst3[:, :, ro + 2 * half:hd], in_=src3[:, :, ro + 2 * half:hd]), reads=[srcbuf], writes=[dstbuf])

    def proj_tok(self, t, region, bank, wv, wbuf, ncols, kc=8, src=None):
        kb = self.kb
        for c in range(kc):
            if src is None:
                la, lbuf = self.hT.t[:, c, t * 128:(t + 1) * 128], self.hT[t]
            else:
                la, lbuf = src(c, t)
            kb.op("pe", lambda e, c=c, la=la: e.matmul(region, lhsT=la, rhs=wv[:, c, 0:ncols], start=(c == 0), stop=(c == kc - 1)),
                  reads=[lbuf, wbuf[0]], writes=[bank[0]])

    def pass_out(self, otok, ot, wo_tag, otok_is_f32=False):
        kb = self.kb
        for tb4 in range(4):
            bank = self.mbank()
            pT = bank.t[:].bitcast(BF16)
            for tl in range(4):
                t = tb4 * 4 + tl
                for c in range(2):
                    col = (tl * 2 + c) * 128
                    kb.op("pe", lambda e, t=t, c=c, col=col: e.transpose(out=pT[:, col:col + 128], in_=otok.t[:, t, c * 128:(c + 1) * 128], identity=self.ident.t[:]),
                          reads=[otok[t], self.ident[0]], writes=[bank[0]])
            pv = pT.rearrange("p (t c n) -> p t c n", t=4, c=2)
            for c in range(2):
                eng = "dve" if tb4 % 2 == 0 else "act"
                if eng == "dve":
                    kb.op("dve", lambda e, c=c: e.tensor_copy(out=ot.t[:, c, tb4 * 512:(tb4 + 1) * 512].rearrange("p (t n) -> p t n", t=4), in_=pv[:, :, c, :]),
                          reads=[bank[0]], writes=[ot[(c, tb4)]])
                else:
                    kb.op("act", lambda e, c=c: e.copy(out=ot.t[:, c, tb4 * 512:(tb4 + 1) * 512].rearrange("p (t n) -> p t n", t=4), in_=pv[:, :, c, :]),
                          reads=[bank[0]], writes=[ot[(c, tb4)]])
        wo = self.ws.next(wo_tag)
        wv = wo.t[:, 0:2048].rearrange("p (c n) -> p c n", c=2)
        import os
        PS = int(os.environ.get("PS", "9"))
        for t in (range(NT) if PS >= 2 else ()):
            for hf in range(2):
                bank = self.mbank()
                for c in range(2):
                    kb.op("pe", lambda e, c=c, t=t, hf=hf: e.matmul(bank.t[:, :], lhsT=ot.t[:, c, t * 128:(t + 1) * 128], rhs=wv[:, c, hf * 512:(hf + 1) * 512], start=(c == 0), stop=(c == 1)),
                          reads=[ot[(c, t // 4)], wo[0]], writes=[bank[0]])
                hs = self.h.t[:, t, hf * 512:(hf + 1) * 512]
                if PS >= 3:
                    kb.op("dve", lambda e, hs=hs, bank=bank: e.tensor_tensor(out=hs, in0=bank.t[:, :], in1=hs, op=ALU.add),
                          reads=[bank[0], self.h[t]], writes=[self.h[t]])

    def reg_xattn(self, li):
        for hd in range(4):
            self.reg_cols(f"x{li}k{hd}", f"l{li}_xkv", [(hd * 256, 256)])
            self.reg_cols(f"x{li}v{hd}", f"l{li}_xkv", [(1024 + hd * 256, 256)])
            self.reg_cols(f"x{li}q{hd}", f"l{li}_xq", [(hd * 256, 256)])
            self.reg_rows(f"x{li}o{hd}", self.dram[f"l{li}_xo"], hd * 256, 256)

    def xattn(self, li):
        kb = self.kb
        with ExitStack() as ls:
            qT = kb.sb([128, 2, S], BF16, "xqT", ls)
            kT = kb.sb([128, 2, 256], BF16, "xkT", ls)
            vx = kb.sb([128, 2, 257], BF16, "xvx", ls)
            otok = kb.sb([128, NT, 256], BF16, "xotok", ls)
            ot = kb.sb([128, 2, S], BF16, "xot", ls)
            kb.op("dve", lambda e: e.memset(vx.t[:, :, 256:257], 1.0), writes=[vx[0], vx[1]])
            for hd in range(4):
                wk = self.ws.next(f"x{li}k{hd}")
                wkv = wk.t[:, 0:2048].rearrange("p (c n) -> p c n", c=8)
                for c in range(2):
                    bank = self.mbank()
                    for dc in range(8):
                        kb.op("pe", lambda e, c=c, dc=dc, bank=bank: e.matmul(bank.t[:, 0:256], lhsT=wkv[:, dc, c * 128:(c + 1) * 128], rhs=self.memT.t[:, dc, :], start=(dc == 0), stop=(dc == 7)),
                              reads=[wk[0], self.memT[0], self.memT[1]], writes=[bank[0]])
                    kb.op("act", lambda e, c=c, bank=bank: e.copy(out=kT.t[:, c, :], in_=bank.t[:, 0:256]), reads=[bank[0]], writes=[kT[c]])
                wvb = self.ws.next(f"x{li}v{hd}")
                wvv = wvb.t[:, 0:2048].rearrange("p (c n) -> p c n", c=8)
                for mt in range(2):
                    bank = self.mbank()
                    for dc in range(8):
                        kb.op("pe", lambda e, mt=mt, dc=dc, bank=bank: e.matmul(bank.t[:, 0:256], lhsT=self.memT.t[:, dc, mt * 128:(mt + 1) * 128], rhs=wvv[:, dc, :], start=(dc == 0), stop=(dc == 7)),
                              reads=[wvb[0], self.memT[mt]], writes=[bank[0]])
                    kb.op("act", lambda e, mt=mt, bank=bank: e.copy(out=vx.t[:, mt, 0:256], in_=bank.t[:, 0:256]), reads=[bank[0]], writes=[vx[mt]])
                wq = self.ws.next(f"x{li}q{hd}")
                wqv = wq.t[:, 0:2048].rearrange("p (c n) -> p c n", c=8)
                for c in range(2):
                    for n in range(4):
                        bank = self.mbank()
                        for dc in range(8):
                            kb.op("pe", lambda e, c=c, n=n, dc=dc, bank=bank: e.matmul(bank.t[:, :], lhsT=wqv[:, dc, c * 128:(c + 1) * 128], rhs=self.hT.t[:, dc, n * 512:(n + 1) * 512], start=(dc == 0), stop=(dc == 7)),
                                  reads=[wq[0]] + self.hT_reads(4 * n, 4 * n + 4), writes=[bank[0]])
                        if (c + n) % 2 == 0:
                            kb.op("act", lambda e, c=c, n=n, bank=bank: e.copy(out=qT.t[:, c, n * 512:(n + 1) * 512], in_=bank.t[:, :]), reads=[bank[0]], writes=[qT[(c, n)]])
                        else:
                            kb.op("dve", lambda e, c=c, n=n, bank=bank: e.tensor_copy(out=qT.t[:, c, n * 512:(n + 1) * 512], in_=bank.t[:, :]), reads=[bank[0]], writes=[qT[(c, n)]])
                jobs = []
                for t in range(NT):
                    kts = []
                    for mt in range(2):
                        kts.append(dict(k=[(kT.t[:, c, mt * 128:(mt + 1) * 128], kT[c]) for c in range(2)],
                                        v=(vx.t[:, mt, :], vx[mt])))
                    jobs.append(dict(q=[(qT.t[:, c, t * 128:(t + 1) * 128], qT[(c, t // 4)]) for c in range(2)],
                                     kt=kts, nv=257, scale=1.0 / 16.0,
                                     fin=self.fin_plain(otok.t[:, t, :], otok[t], 256)))
                import os
                XS = int(os.environ.get("XSTOP", "9"))
                if XS >= 2:
                    self.attend(jobs)
                if XS >= 3:
                    self.pass_out(otok, ot, f"x{li}o{hd}")
                else:
                    self.ws.next(f"x{li}o{hd}")
        kb.barrier()

    def ffn_groups(self, li):
        moe = (li % 2 == 1)
        ne = NEXP if moe else 1
        if self.dbg_experts is not None and moe:
            ne = self.dbg_experts
        return [(ex, g) for ex in range(ne) for g in range(7)]

    def reg_ffn(self, li):
        moe = (li % 2 == 1)
        groups = self.ffn_groups(li)

        def wts(ex):
            if moe:
                return self.dram[f"l{li}_moe_w_in"][ex], self.dram[f"l{li}_moe_w_out"][ex]
            return self.dram[f"l{li}_ffn_w_in"], self.dram[f"l{li}_ffn_w_out"]

        def reg_gu(ex, g):
            Win, _ = wts(ex)
            for (nm, c0) in (("g", g * 512), ("u", DFF + g * 512)):
                def fn(buf, Win=Win, c0=c0):
                    v = buf.t[:, 0:4096].rearrange("p (c n) -> p c n", c=8)
                    self.wdma(buf, v, Win[:, c0:c0 + 512].rearrange("(c p) n -> p c n", p=128))
                self.ws.register(f"f{li}e{ex}{nm}{g}", fn)

        def reg_o(ex, g):
            _, Wout = wts(ex)
            self.reg_rows(f"f{li}e{ex}o{g}", Wout, g * 512, 512)

        for i, (ex, g) in enumerate(groups):
            reg_gu(ex, g)
            if i >= 1:
                reg_o(*groups[i - 1])
        reg_o(*groups[-1])

    def ffn(self, li):
        kb = self.kb
        moe = (li % 2 == 1)
        ne = NEXP if moe else 1
        if self.dbg_experts is not None and moe:
            ne = self.dbg_experts
        d = self.dram
        with ExitStack() as ls:
            comb = None
            if moe:
                comb = kb.sb([128, NT, 8], F32, "comb", ls)
                with ExitStack() as ls2:
                    rB = kb.sb([128, 8, D], F32, "rB", ls2)
                    junk2 = [kb.sb([128, D], F32, "junk", ls2) for _ in range(2)]
                    lg = kb.sb([128, NT, 8], F32, "lg", ls2)
                    bias = kb.sb([128, 8], F32, "rbias", ls2)
                    top = kb.sb([128, NT, 8], F32, "top", ls2)
                    gg = kb.sb([128, NT, 4], F32, "gg", ls2)
                    m12 = kb.sb([128, 2, 8], F32, "m12", ls2)
                    for ex in range(8):
                        kb.dma("sp", rB.t[:, ex, :], d[f"l{li}_moe_router"][ex:ex + 1, :].partition_broadcast(128), writes=[rB[ex]])
                    kb.dma("sp", bias.t[:], d[f"l{li}_moe_bias"].partition_broadcast(128), writes=[bias[0]])
                    for t in range(NT):
                        for ex in range(8):
                            jk = junk2[ex % 2]
                            kb.op("dve", lambda e, t=t, ex=ex, jk=jk: e.tensor_tensor(out=jk.t[:], in0=self.h.t[:, t, :], in1=rB.t[:, ex, :], op=ALU.mult),
                                  reads=[self.h[t], rB[ex]], writes=[jk[0]])
                            kb.op("dve", lambda e, t=t, ex=ex, jk=jk: e.reduce_sum(out=lg.t[:, t, ex:ex + 1], in_=jk.t[:], axis=AX.X),
                                  reads=[jk[0]], writes=[lg[(t, ex)]])
                        lgt = [lg[(t, ex)] for ex in range(8)]
                        kb.op("dve", lambda e, t=t: e.scalar_tensor_tensor(out=lg.t[:, t, :], in0=lg.t[:, t, :], scalar=1.0 / ALPHA, in1=bias.t[:], op0=ALU.mult, op1=ALU.add),
                              reads=lgt + [bias[0]], writes=lgt)
                        kb.op("dve", lambda e, t=t: e.max(out=top.t[:, t, :], in_=lg.t[:, t, :]), reads=lgt, writes=[top[t]])
                        kb.op("dve", lambda e, t=t: e.tensor_tensor(out=gg.t[:, t, 0:1], in0=top.t[:, t, 1:2], in1=top.t[:, t, 0:1], op=ALU.subtract),
                              reads=[top[t]], writes=[gg[(t, 0)]])
                        kb.op("act", lambda e, t=t: e.activation(out=gg.t[:, t, 1:2], in_=gg.t[:, t, 0:1], func=AF.Exp), reads=[gg[(t, 0)]], writes=[gg[(t, 1)]])
                        kb.op("dve", lambda e, t=t: e.tensor_scalar(out=gg.t[:, t, 2:3], in0=gg.t[:, t, 1:2], scalar1=1.0, scalar2=None, op0=ALU.add),
                              reads=[gg[(t, 1)]], writes=[gg[(t, 2)]])
                        kb.op("dve", lambda e, t=t: e.reciprocal(out=gg.t[:, t, 2:3], in_=gg.t[:, t, 2:3]), reads=[gg[(t, 2)]], writes=[gg[(t, 2)]])
                        kb.op("dve", lambda e, t=t: e.tensor_tensor(out=gg.t[:, t, 3:4], in0=gg.t[:, t, 1:2], in1=gg.t[:, t, 2:3], op=ALU.mult),
                              reads=[gg[(t, 1)], gg[(t, 2)]], writes=[gg[(t, 3)]])
                        kb.op("dve", lambda e, t=t: e.tensor_scalar(out=m12.t[:, 0, :], in0=lg.t[:, t, :], scalar1=top.t[:, t, 0:1], scalar2=gg.t[:, t, 2:3], op0=ALU.is_equal, op1=ALU.mult),
                              reads=lgt + [top[t], gg[(t, 2)]], writes=[m12[0]])
                        kb.op("dve", lambda e, t=t: e.tensor_scalar(out=m12.t[:, 1, :], in0=lg.t[:, t, :], scalar1=top.t[:, t, 1:2], scalar2=gg.t[:, t, 3:4], op0=ALU.is_equal, op1=ALU.mult),
                              reads=lgt + [top[t], gg[(t, 3)]], writes=[m12[1]])
                        kb.op("dve", lambda e, t=t: e.tensor_tensor(out=comb.t[:, t, :], in0=m12.t[:, 0, :], in1=m12.t[:, 1, :], op=ALU.add),
                              reads=[m12[0], m12[1]], writes=[comb[t]])
                kb.barrier()
            aT = [kb.sb([128, 4, S], BF16, "aT", ls) for _ in range(2)]
            sg = [kb.sb([128, 512], F32, "sg", ls) for _ in range(3)]
            sgi = 0
            pend = None

            def emit_out(ex, g, at, wo):
                wv = wo.t[:, 0:4096].rearrange("p (c n) -> p c n", c=4)
                for t in range(NT):
                    for hf in range(2):
                        self._fo = (getattr(self, '_fo', 0) + 1) % 4
                        bank = self.banks[4:8][self._fo]
                        for j in range(4):
                            kb.op("pe", lambda e, j=j, t=t, hf=hf, bank=bank: e.matmul(bank.t[:, :], lhsT=at.t[:, j, t * 128:(t + 1) * 128], rhs=wv[:, j, hf * 512:(hf + 1) * 512], start=(j == 0), stop=(j == 3)),
                                  reads=[at[(j, t // 4)], wo[0]], writes=[bank[0]])
                        hs = self.h.t[:, t, hf * 512:(hf + 1) * 512]
                        if moe:
                            kb.op("dve", lambda e, hs=hs, bank=bank, t=t: e.scalar_tensor_tensor(out=hs, in0=bank.t[:, :], scalar=comb.t[:, t, ex:ex + 1], in1=hs, op0=ALU.mult, op1=ALU.add),
                                  reads=[bank[0], self.h[t], comb[t]], writes=[self.h[t]])
                        else:
                            kb.op("dve", lambda e, hs=hs, bank=bank: e.tensor_tensor(out=hs, in0=bank.t[:, :], in1=hs, op=ALU.add),
                                  reads=[bank[0], self.h[t]], writes=[self.h[t]])

            gub = self.banks[0:4]
            gui = 0
            for gi, (ex, g) in enumerate(self.ffn_groups(li)):
                wg = self.ws.next(f"f{li}e{ex}g{g}")
                wu = self.ws.next(f"f{li}e{ex}u{g}")
                wgv = wg.t[:, 0:4096].rearrange("p (c n) -> p c n", c=8)
                wuv = wu.t[:, 0:4096].rearrange("p (c n) -> p c n", c=8)
                at = aT[gi % 2]
                for j in range(4):
                    for n in range(4):
                        bg = gub[gui % 4]
                        bu = gub[(gui + 1) % 4]
                        gui += 2
                        hr = self.hT_reads(4 * n, 4 * n + 4)
                        for dc in range(8):
                            kb.op("pe", lambda e, dc=dc, j=j, n=n, bg=bg: e.matmul(bg.t[:, :], lhsT=wgv[:, dc, j * 128:(j + 1) * 128], rhs=self.hT.t[:, dc, n * 512:(n + 1) * 512], start=(dc == 0), stop=(dc == 7)),
                                  reads=[wg[0]] + hr, writes=[bg[0]])
                        for dc in range(8):
                            kb.op("pe", lambda e, dc=dc, j=j, n=n, bu=bu: e.matmul(bu.t[:, :], lhsT=wuv[:, dc, j * 128:(j + 1) * 128], rhs=self.hT.t[:, dc, n * 512:(n + 1) * 512], start=(dc == 0), stop=(dc == 7)),
                                  reads=[wu[0]] + hr, writes=[bu[0]])
                        s_ = sg[sgi % 3]
                        sgi += 1
                        kb.op("act", lambda e, bg=bg, s_=s_: e.activation(out=s_.t[:], in_=bg.t[:, :], func=AF.Silu), reads=[bg[0]], writes=[s_[0]])
                        kb.op("dve", lambda e, bu=bu, s_=s_, j=j, n=n, at=at: e.tensor_tensor(out=at.t[:, j, n * 512:(n + 1) * 512], in0=bu.t[:, :], in1=s_.t[:], op=ALU.mult),
                              reads=[bu[0], s_[0]], writes=[at[(j, n)]])
                if pend is not None:
                    wo = self.ws.next(f"f{li}e{pend[0]}o{pend[1]}")
                    emit_out(pend[0], pend[1], pend[2], wo)
                pend = (ex, g, at)
            wo = self.ws.next(f"f{li}e{pend[0]}o{pend[1]}")
            emit_out(pend[0], pend[1], pend[2], wo)
        kb.barrier()

    def transposes(self, srcs, rows=128):
        kb = self.kb
        bank = self.mbank()
        pT = bank.t[:].bitcast(BF16)
        for i, (ap, buf) in enumerate(srcs):
            nc_ = ap.shape[-1]
            kb.op("pe", lambda e, i=i, ap=ap, nc_=nc_: e.transpose(out=pT[0:nc_, i * 128:(i + 1) * 128], in_=ap, identity=self.ident.t[:]),
                  reads=(list(buf) if isinstance(buf, (list, tuple)) else [buf]) + [self.ident[0]], writes=[bank[0]])
        return bank, pT

    def skewed(self, n, pe_part, mid_part, fin_part):
        a = pe_part(0)
        prev = None
        for t in range(n):
            nxt = pe_part(t + 1) if t + 1 < n else None
            m = mid_part(t, a)
            if prev is not None:
                fin_part(t - 1, prev)
            prev = m
            a = nxt
        fin_part(n - 1, prev)

    def causal_kts(self, t, kfn, vfn, lo=0, far_at=None):
        kts = []
        for kt in range(lo, t + 1):
            d = dict(k=kfn(kt), v=vfn(kt))
            if kt == t:
                d["mask"] = (self.causal.t[:], self.causal[0])
            elif far_at is not None and kt == far_at:
                d["mask"] = (self.far.t[:], self.far[0])
            kts.append(d)
        return kts

    def reg_moba(self, li):
        for p in range(4):
            self.reg_cols(f"m{li}qk{p}", "l2_moba_w_in", [(p * 256, 256), (1024 + p * 256, 256)])
            self.reg_cols(f"m{li}v{p}", "l2_moba_w_in", [(2048 + p * 256, 256)])
            self.reg_rows(f"m{li}o{p}", self.dram["l2_moba_w_out"], p * 256, 256)

    def moba(self, li):
        kb = self.kb
        d = self.dram
        with ExitStack() as ls:
            qaT = kb.sb([128, 4, S], BF16, "qaT", ls)
            kaT = kb.sb([128, 4, S], BF16, "kaT", ls)
            vx = kb.sb([128, NT, 4, 65], BF16, "vx", ls)
            stg = [kb.sb([128, 512], BF16, "stg", ls) for _ in range(2)]
            otok = kb.sb([128, NT, 256], BF16, "otok", ls)
            ot = kb.sb([128, 2, S], BF16, "ot", ls)
            nm = kb.sb([128, NT, 4, 32], BF16, "nm", ls)
            ksf = kb.sb([128, 4, 8], F32, "ksf", ls)
            ksb = kb.sb([128, 4, 8], BF16, "ksb", ls)
            tB = kb.sb([128, NT, 8], F32, "tB", ls)
            tV = kb.sb([128, NT, 8], F32, "tV", ls)
            tO = kb.sb([128, NT, 8], F32, "tO", ls)
            gm = [kb.sb([128, 4, 8], F32, "gm", ls) for _ in range(2)]
            sel = [kb.sb([128, 4, 8], F32, "sel", ls) for _ in range(2)]
            top8 = [kb.sb([128, 8], F32, "top8", ls) for _ in range(4)]
            self.rtmp = kb.sb([128, 8, 64], F32, "rtmp", ls)
            self._ri = 0
            kb.dma("sp", tB.t[:], d["c_mobaB"][:, :, :], writes=[tB[0]])
            kb.dma("sp", tV.t[:], d["c_mobaV"][:, :, :], writes=[tV[0]])
            kb.dma("sp", tO.t[:], d["c_mobaO"][:, :, :], writes=[tO[0]])
            for r in range(4):
                kb.dma("pool", kaT.t[64:96, r, :], d["c_e8"][:, :], writes=[kaT[("e", r)]])
            kb.op("dve", lambda e: e.memset(vx.t[:, :, :, 64:65], 1.0), writes=[vx[t] for t in range(NT)])
            kb.op("dve", lambda e: e.memset(nm.t[:], 0.0), writes=[nm[t] for t in range(NT)])
            for p in range(4):
                wqk = self.ws.next(f"m{li}qk{p}")
                wv_ = self.ws.next(f"m{li}v{p}")
                wqkv = wqk.t[:, 0:4096].rearrange("p (c n) -> p c n", c=8)
                wvv = wv_.t[:, 0:2048].rearrange("p (c n) -> p c n", c=8)
                def pe_part(t):
                    bA = self.mbank()
                    self.proj_tok(t, bA.t[:, 0:256], bA, wqkv[:, :, 0:256], wqk, 256)
                    self.proj_tok(t, bA.t[:, 256:512], bA, wqkv[:, :, 256:512], wqk, 256)
                    bB = self.mbank()
                    self.proj_tok(t, bB.t[:, 0:256], bB, wvv, wv_, 256)
                    return bA, bB

                def mid_part(t, ab):
                    bA, bB = ab
                    sg_ = stg[t % 2]
                    self.rope(t, bA.t[:, 0:512].rearrange("p (a b) -> p a b", a=8), bA[0],
                              sg_.t[:, :].rearrange("p (a b) -> p a b", a=8), sg_[0], 8, 64, 0, 8, self.cosp, self.sinp)
                    kb.op("act", lambda e, t=t, bB=bB: e.activation(out=vx.t[:, t, :, 0:64], in_=bB.t[:, 0:256].rearrange("p (a b) -> p a b", a=4), func=AF.Identity),
                          reads=[bB[0]], writes=[vx[t]])
                    return self.transposes([(sg_.t[:, i * 128:(i + 1) * 128], sg_[0]) for i in range(4)])

                def fin_part(t, bp):
                    bT, pT = bp
                    tsl = slice(t * 128, (t + 1) * 128)
                    for (dstT, c0) in ((qaT, 0), (kaT, 256)):
                        for par in range(2):
                            kb.op("dve", lambda e, dstT=dstT, c0=c0, par=par, pT=pT, tsl=tsl: e.tensor_copy(
                                out=dstT.t[0:64, par:4:2, tsl], in_=pT[par * 64:par * 64 + 64, c0:c0 + 256].rearrange("p (a n) -> p a n", a=2)),
                                reads=[bT[0]], writes=[dstT[(par, t)], dstT[(par + 2, t)]])
                self.skewed(NT, pe_part, mid_part, fin_part)
                kb.op("dve", lambda e: e.tensor_reduce(out=ksf.t[0:64, :, :], in_=kaT.t[0:64, :, :].rearrange("p r (j n) -> p r j n", n=256), axis=AX.X, op=ALU.add),
                      reads=[kaT[(r, t)] for r in range(4) for t in range(NT)], writes=[ksf[0]])
                kb.op("dve", lambda e: e.tensor_copy(out=ksb.t[0:64, :, :], in_=ksf.t[0:64, :, :]), reads=[ksf[0]], writes=[ksb[0]])
                for t in range(NT):
                    tsl = slice(t * 128, (t + 1) * 128)
                    bG = self.mbank()
                    for r in range(4):
                        kb.op("pe", lambda e, r=r, bG=bG, tsl=tsl: e.matmul(bG.t[:, r * 8:(r + 1) * 8], lhsT=qaT.t[0:64, r, tsl], rhs=ksb.t[0:64, r, :], start=True, stop=True),
                              reads=[qaT[(r, t)], ksb[0]], writes=[bG[0]])
                    g_ = gm[t % 2]
                    s_ = sel[t % 2]
                    kb.op("dve", lambda e, bG=bG, g_=g_, t=t: e.tensor_tensor(out=g_.t[:], in0=bG.t[:, 0:32].rearrange("p (a b) -> p a b", a=4),
                                                                          in1=tB.t[:, t:t + 1, :].broadcast_to([128, 4, 8]), op=ALU.add),
                          reads=[bG[0], tB[0]], writes=[g_[0]])
                    for r in range(4):
                        tp = top8[r]
                        kb.op("dve", lambda e, r=r, tp=tp, g_=g_: e.max(out=tp.t[:], in_=g_.t[:, r, :]), reads=[g_[0]], writes=[tp[0]])
                        kb.op("dve", lambda e, r=r, tp=tp, g_=g_, s_=s_: e.tensor_scalar(out=s_.t[:, r, :], in0=g_.t[:, r, :], scalar1=tp.t[:, 2:3], scalar2=None, op0=ALU.is_ge),
                              reads=[g_[0], tp[0]], writes=[s_[r]])
                    sr = [s_[r] for r in range(4)]
                    kb.op("dve", lambda e, s_=s_, t=t: e.tensor_tensor(out=s_.t[:], in0=s_.t[:], in1=tV.t[:, t:t + 1, :].broadcast_to([128, 4, 8]), op=ALU.mult),
                          reads=sr + [tV[0]], writes=sr)
                    kb.op("dve", lambda e, s_=s_, t=t: e.tensor_tensor(out=s_.t[:], in0=s_.t[:], in1=tO.t[:, t:t + 1, :].broadcast_to([128, 4, 8]), op=ALU.add),
                          reads=sr + [tO[0]], writes=sr)
                    kb.op("dve", lambda e, s_=s_, t=t: e.tensor_scalar(out=nm.t[:, t, :, 0:8], in0=s_.t[:], scalar1=-1.0, scalar2=-NEG, op0=ALU.add, op1=ALU.mult),
                          reads=sr, writes=[nm[t]])
                for r in range(4):
                    for tb in range(2):
                        bT, pT = self.transposes([(nm.t[:, tb * 8 + j, r, :], nm[tb * 8 + j]) for j in range(8)])
                        kb.op("dve", lambda e, r=r, tb=tb, pT=pT: e.tensor_copy(out=qaT.t[64:96, r, tb * 1024:(tb + 1) * 1024], in_=pT[0:32, 0:1024]),
                              reads=[bT[0]], writes=[qaT[("m", r, tb)]])
                jobs = []
                for r in range(4):
                    for t in range(NT):
                        tsl = slice(t * 128, (t + 1) * 128)
                        kts = self.causal_kts(
                            t,
                            lambda kt, r=r: [(kaT.t[0:96, r, kt * 128:(kt + 1) * 128], [kaT[(r, kt)], kaT[("e", r)]])],
                            lambda kt, r=r: (vx.t[:, kt, r, :], vx[kt]))
                        jobs.append(dict(q=[(qaT.t[0:96, r, tsl], [qaT[(r, t)], qaT[("m", r, t // 8)]])], kt=kts, nv=65, scale=0.125,
                                         fin=self.fin_plain(otok.t[:, t, r * 64:(r + 1) * 64], otok[t], 64)))
                self.attend(jobs)
                self.pass_out(otok, ot, f"m{li}o{p}")
        kb.barrier()

    def reg_swa(self, li):
        for p in range(4):
            g = p // 2
            self.reg_cols(f"s{li}qkv{p}", "l3_swa_w_in", [(p * 256, 256), (1024 + g * 64, 64), (1024 + 128 + g * 64, 64)])
            self.reg_rows(f"s{li}o{p}", self.dram["l3_swa_w_out"], p * 256, 256)

    def swa(self, li):
        kb = self.kb
        d = self.dram
        with ExitStack() as ls:
            qT = kb.sb([128, 4, S], BF16, "qT", ls)
            kT = kb.sb([128, S], BF16, "kT", ls)
            vx = kb.sb([128, NT, 65], BF16, "vx", ls)
            stg = [kb.sb([128, 320], BF16, "stg", ls) for _ in range(2)]
            otok = kb.sb([128, NT, 256], BF16, "otok", ls)
            ot = kb.sb([128, 2, S], BF16, "ot", ls)
            snk = kb.sb([128, 16], F32, "snk", ls)
            esink = kb.sb([128, 16], F32, "esink", ls)
            self.rtmp = kb.sb([128, 8, 64], F32, "rtmp", ls)
            self._ri = 0
            kb.dma("sp", snk.t[:], d["l3_swa_sinks"].partition_broadcast(128), writes=[snk[0]])
            kb.op("act", lambda e: e.activation(out=esink.t[:], in_=snk.t[:], func=AF.Exp), reads=[snk[0]], writes=[esink[0]])
            kb.op("dve", lambda e: e.memset(vx.t[:, :, 64:65], 1.0), writes=[vx[t] for t in range(NT)])
            for p in range(4):
                w = self.ws.next(f"s{li}qkv{p}")
                wv = w.t[:, 0:8 * 384].rearrange("p (c n) -> p c n", c=8)
                def pe_part(t):
                    bA = self.mbank()
                    self.proj_tok(t, bA.t[:, 0:256], bA, wv[:, :, 0:256], w, 256)
                    self.proj_tok(t, bA.t[:, 256:384], bA, wv[:, :, 256:384], w, 128)
                    return bA

                def mid_part(t, bA):
                    sg_ = stg[t % 2]
                    self.rope(t, bA.t[:, 0:320].rearrange("p (a b) -> p a b", a=5), bA[0],
                              sg_.t[:, :].rearrange("p (a b) -> p a b", a=5), sg_[0], 5, 64, 0, 8, self.cosp, self.sinp)
                    kb.op("dve", lambda e, t=t, bA=bA: e.tensor_copy(out=vx.t[:, t, 0:64], in_=bA.t[:, 320:384]), reads=[bA[0]], writes=[vx[t]])
                    return self.transposes([(sg_.t[:, 0:128], sg_[0]), (sg_.t[:, 128:256], sg_[0]), (sg_.t[:, 256:320], sg_[0])])

                def fin_part(t, bp):
                    bT, pT = bp
                    tsl = slice(t * 128, (t + 1) * 128)
                    for par in range(2):
                        kb.op("dve", lambda e, par=par, pT=pT, tsl=tsl: e.tensor_copy(
                            out=qT.t[0:64, par:4:2, tsl], in_=pT[par * 64:par * 64 + 64, 0:256].rearrange("p (a n) -> p a n", a=2)),
                            reads=[bT[0]], writes=[qT[(par, t)], qT[(par + 2, t)]])
                    kb.op("dve", lambda e, pT=pT, tsl=tsl: e.tensor_copy(out=kT.t[0:64, tsl], in_=pT[0:64, 256:384]), reads=[bT[0]], writes=[kT[t]])
                self.skewed(NT, pe_part, mid_part, fin_part)
                jobs = []
                for r in range(4):
                    hh = p * 4 + r
                    for t in range(NT):
                        tsl = slice(t * 128, (t + 1) * 128)
                        kts = self.causal_kts(
                            t,
                            lambda kt: [(kT.t[0:64, kt * 128:(kt + 1) * 128], kT[kt])],
                            lambda kt: (vx.t[:, kt, :], vx[kt]),
                            lo=max(0, t - 1), far_at=t - 1)
                        jobs.append(dict(q=[(qT.t[0:64, r, tsl], qT[(r, t)])], kt=kts, nv=65, scale=0.125,
                                         fin=self.fin_plain(otok.t[:, t, r * 64:(r + 1) * 64], otok[t], 64, extra=(esink.t[:, hh:hh + 1], esink[0]))))
                self.attend(jobs)
                self.pass_out(otok, ot, f"s{li}o{p}")
        kb.barrier()

    def reg_mla(self, li):
        self.reg_cols(f"a{li}d", "l1_mla_w_down", [(0, 416)])
        for sp in range(8):
            self.reg_cols(f"a{li}uq{sp}", "l1_mla_w_uq", [(sp * 192, 192)], kc=2)
            self.reg_cols(f"a{li}ukv{sp}", "l1_mla_w_ukv", [(sp * 256, 256)], kc=1)
            if sp % 2 == 1:
                self.reg_rows(f"a{li}o{sp // 2}", self.dram["l1_mla_w_out"], (sp // 2) * 256, 256)

    def mla(self, li):
        kb = self.kb
        d = self.dram
        with ExitStack() as ls:
            cqnT = kb.sb([128, 2, S], BF16, "cqnT", ls)
            ckvnT = kb.sb([128, S], BF16, "ckvnT", ls)
            krope = kb.sb([128, NT, 32], BF16, "krope", ls)
            qaT = kb.sb([128, 2, S], BF16, "qaT", ls)
            kaT = kb.sb([128, 2, S], BF16, "kaT", ls)
            vx = kb.sb([128, NT, 2, 65], BF16, "vx", ls)
            qtok = [kb.sb([128, 2, 96], BF16, "qtok", ls) for _ in range(2)]
            ktok = [kb.sb([128, 2, 96], BF16, "ktok", ls) for _ in range(2)]
            otok = kb.sb([128, NT, 256], BF16, "otok", ls)
            ot = kb.sb([128, 2, S], BF16, "ot", ls)
            dn = [kb.sb([128, 416], F32, "dn", ls) for _ in range(2)]
            junk = kb.sb([128, 256], F32, "junk", ls)
            gq = kb.sb([128, 384], F32, "gq", ls)
            nb = [kb.sb([128, 384], BF16, "nb", ls) for _ in range(2)]
            ss = [kb.sb([128, 6], F32, "ss", ls) for _ in range(2)]
            self.rtmp = kb.sb([128, 8, 64], F32, "rtmp", ls)
            self._ri = 0
            kb.dma("sp", gq.t[:, 0:256], d["l1_mla_q_norm"].partition_broadcast(128), writes=[gq[0]])
            kb.dma("sp", gq.t[:, 256:384], d["l1_mla_kv_norm"].partition_broadcast(128), writes=[gq[1]])
            kb.op("dve", lambda e: e.memset(vx.t[:, :, :, 64:65], 1.0), writes=[vx[t] for t in range(NT)])
            wd = self.ws.next(f"a{li}d")
            wdv = wd.t[:, 0:8 * 416].rearrange("p (c n) -> p c n", c=8)
            def pe_part0(t):
                bA = self.mbank()
                self.proj_tok(t, bA.t[:, 0:416], bA, wdv, wd, 416)
                return bA

            def mid_part0(t, bA):
                dn_ = dn[t % 2]
                ss_ = ss[t % 2]
                nb_ = nb[t % 2]
                kb.op("act", lambda e, bA=bA, dn_=dn_: e.copy(out=dn_.t[:], in_=bA.t[:, 0:416]), reads=[bA[0]], writes=[dn_[0]])
                for (i, c0, n) in ((0, 0, 256), (1, 256, 128)):
                    kb.op("dve", lambda e, c0=c0, n=n, dn_=dn_: e.tensor_tensor(out=junk.t[:, 0:n], in0=dn_.t[:, c0:c0 + n], in1=dn_.t[:, c0:c0 + n], op=ALU.mult),
                          reads=[dn_[0]], writes=[junk[0]])
                    kb.op("dve", lambda e, i=i, n=n, ss_=ss_: e.reduce_sum(out=ss_.t[:, i:i + 1], in_=junk.t[:, 0:n], axis=AX.X), reads=[junk[0]], writes=[ss_[i]])
                kb.op("dve", lambda e, ss_=ss_: e.tensor_scalar(out=ss_.t[:, 0:1], in0=ss_.t[:, 0:1], scalar1=1.0 / 256, scalar2=RMS_EPS, op0=ALU.mult, op1=ALU.add),
                      reads=[ss_[0]], writes=[ss_[0]])
                kb.op("dve", lambda e, ss_=ss_: e.tensor_scalar(out=ss_.t[:, 1:2], in0=ss_.t[:, 1:2], scalar1=1.0 / 128, scalar2=RMS_EPS, op0=ALU.mult, op1=ALU.add),
                      reads=[ss_[1]], writes=[ss_[1]])
                kb.op("act", lambda e, ss_=ss_: e.activation(out=ss_.t[:, 2:4], in_=ss_.t[:, 0:2], func=AF.Sqrt), reads=[ss_[0], ss_[1]], writes=[ss_[2]])
                kb.op("dve", lambda e, ss_=ss_: e.reciprocal(out=ss_.t[:, 4:6], in_=ss_.t[:, 2:4]), reads=[ss_[2]], writes=[ss_[3]])
                for (i, c0, n) in ((0, 0, 256), (1, 256, 128)):
                    kb.op("dve", lambda e, i=i, c0=c0, n=n, dn_=dn_, ss_=ss_, nb_=nb_: e.scalar_tensor_tensor(
                        out=nb_.t[:, c0:c0 + n], in0=dn_.t[:, c0:c0 + n], scalar=ss_.t[:, 4 + i:5 + i], in1=gq.t[:, c0:c0 + n], op0=ALU.mult, op1=ALU.mult),
                        reads=[dn_[0], ss_[3], gq[0], gq[1]], writes=[nb_[0]])
                self.rope(t, dn_.t[:, 384:416].rearrange("p (a b) -> p a b", a=1), dn_[0],
                          krope.t[:, t, :].rearrange("p (a b) -> p a b", a=1), krope[t], 1, 32, 0, 16, self.cosm, self.sinm)
                return self.transposes([(nb_.t[:, i * 128:(i + 1) * 128], nb_[0]) for i in range(3)])

            def fin_part0(t, bp):
                bT, pT = bp
                tsl = slice(t * 128, (t + 1) * 128)
                kb.op("dve", lambda e, pT=pT, tsl=tsl: e.tensor_copy(out=cqnT.t[:, :, tsl], in_=pT[:, 0:256].rearrange("p (a n) -> p a n", a=2)),
                      reads=[bT[0]], writes=[cqnT[t]])
                kb.op("dve", lambda e, pT=pT, tsl=tsl: e.tensor_copy(out=ckvnT.t[:, tsl], in_=pT[:, 256:384]), reads=[bT[0]], writes=[ckvnT[t]])
            self.skewed(NT, pe_part0, mid_part0, fin_part0)
            for sp in range(8):
                p = sp // 2
                hf2 = sp % 2
                wq = self.ws.next(f"a{li}uq{sp}")
                wkv = self.ws.next(f"a{li}ukv{sp}")
                wqv = wq.t[:, 0:384].rearrange("p (c n) -> p c n", c=2)
                wkvv = wkv.t[:, 0:256].rearrange("p (c n) -> p c n", c=1)
                def pe_part(t):
                    bA = self.mbank()
                    self.proj_tok(t, bA.t[:, 0:192], bA, wqv, wq, 192, kc=2, src=lambda c, t: (cqnT.t[:, c, t * 128:(t + 1) * 128], cqnT[t]))
                    bB = self.mbank()
                    self.proj_tok(t, bB.t[:, 0:256], bB, wkvv, wkv, 256, kc=1, src=lambda c, t: (ckvnT.t[:, t * 128:(t + 1) * 128], ckvnT[t]))
                    return bA, bB

                def mid_part(t, ab):
                    bA, bB = ab
                    q_ = qtok[t % 2]
                    k_ = ktok[t % 2]
                    self.rope(t, bA.t[:, 0:192].rearrange("p (a b) -> p a b", a=2), bA[0], q_.t[:], q_[0], 2, 96, 64, 16, self.cosm, self.sinm)
                    bB3 = bB.t[:, 0:256].rearrange("p (a b) -> p a b", a=2)
                    kb.op("act", lambda e, k_=k_, bB3=bB3: e.activation(out=k_.t[:, :, 0:64], in_=bB3[:, :, 0:64], func=AF.Identity), reads=[bB[0]], writes=[k_[0]])
                    kb.op("act", lambda e, t=t, bB3=bB3: e.activation(out=vx.t[:, t, :, 0:64], in_=bB3[:, :, 64:128], func=AF.Identity), reads=[bB[0]], writes=[vx[t]])
                    kb.op("dve", lambda e, t=t, k_=k_: e.tensor_copy(out=k_.t[:, :, 64:96], in_=krope.t[:, t:t + 1, :].broadcast_to([128, 2, 32])),
                          reads=[krope[t]], writes=[k_[1]])
                    return self.transposes([(q_.t[:, r, :], q_[0]) for r in range(2)] + [(k_.t[:, r, :], [k_[0], k_[1]]) for r in range(2)])

                def fin_part(t, bp):
                    bT, pT = bp
                    tsl = slice(t * 128, (t + 1) * 128)
                    kb.op("dve", lambda e, pT=pT, tsl=tsl: e.tensor_copy(out=qaT.t[0:96, :, tsl], in_=pT[0:96, 0:256].rearrange("p (a n) -> p a n", a=2)),
                          reads=[bT[0]], writes=[qaT[t]])
                    kb.op("dve", lambda e, pT=pT, tsl=tsl: e.tensor_copy(out=kaT.t[0:96, :, tsl], in_=pT[0:96, 256:512].rearrange("p (a n) -> p a n", a=2)),
                          reads=[bT[0]], writes=[kaT[t]])
                self.skewed(NT, pe_part, mid_part, fin_part)
                jobs = []
                for r in range(2):
                    for t in range(NT):
                        tsl = slice(t * 128, (t + 1) * 128)
                        kts = self.causal_kts(
                            t,
                            lambda kt, r=r: [(kaT.t[0:96, r, kt * 128:(kt + 1) * 128], kaT[kt])],
                            lambda kt, r=r: (vx.t[:, kt, r, :], vx[kt]))
                        oc = (hf2 * 2 + r) * 64
                        jobs.append(dict(q=[(qaT.t[0:96, r, tsl], qaT[t])], kt=kts, nv=65, scale=96.0 ** -0.5,
                                         fin=self.fin_plain(otok.t[:, t, oc:oc + 64], otok[t], 64)))
                self.attend(jobs)
                if hf2 == 1:
                    self.pass_out(otok, ot, f"a{li}o{p}")
        kb.barrier()

    def reg_nsa(self, li):
        W = "l0_nsa_w_in"
        w1 = self.dram["l0_nsa_cmp_w1"]
        for g in range(4):
            self.reg_cols(f"n_q{g}", W, [(g * 256, 256)])
            self.reg_cols(f"n_kv{g}", W, [(1024 + j * 256 + g * 64, 64) for j in range(6)] + [(2560 + j * 16 + g * 4, 4) for j in range(3)])

            def fn(buf):
                for j in range(2):
                    self.wdma(buf, buf.t[j * 64:(j + 1) * 64, 0:4096].rearrange("p (l h) -> p l h", l=32),
                              w1[j].rearrange("(l d) h -> d l h", d=64))
            self.ws.register(f"n_w1_{g}", fn)
            self.reg_rows(f"n_o{g}", self.dram["l0_nsa_w_out"], g * 256, 256)

    def nsa(self, li):
        kb = self.kb
        d = self.dram
        with ExitStack() as ls:
            qaT = kb.sb([128, 4, S], BF16, "qaT", ls)
            cvT = kb.sb([128, S], BF16, "cvT", ls)
            ksaT = kb.sb([128, S], BF16, "ksaT", ls)
            kwT = kb.sb([128, S], BF16, "kwT", ls)
            vsx = kb.sb([128, NT, 65], BF16, "vsx", ls)
            vwx = kb.sb([128, NT, 65], BF16, "vwx", ls)
            w2sb = kb.sb([128, 2, 64], BF16, "w2sb", ls)
            peT = kb.sb([128, 32], BF16, "peT", ls)
            cbias = kb.sb([128, 2], F32, "cbias", ls)
            hid = kb.sb([128, 2, 128], BF16, "hid", ls)
            kcT = kb.sb([128, 128], BF16, "kcT", ls)
            vcx = kb.sb([128, 97], BF16, "vcx", ls)
            gx = [kb.sb([128, 128], F32, "gx", ls) for _ in range(2)]
            cmpmask = kb.sb([128, S], BF16, "cmpmask", ls)
            imp = kb.sb([128, NT, 32], F32, "imp", ls)
            tA = kb.sb([128, NT, 32], F32, "tA", ls)
            tB = kb.sb([128, NT, 32], F32, "tB", ls)
            acc = [kb.sb([128, 4, 64], F32, "acc", ls) for _ in range(1)]
            otok = kb.sb([128, NT, 256], BF16, "otok", ls)
            ot = kb.sb([128, 2, S], BF16, "ot", ls)
            stgs = [kb.sb([128, 640], BF16, "stg", ls) for _ in range(2)]
            nmt = kb.sb([128, NT, 32], BF16, "nmt", ls)
            graw = kb.sb([128, NT, 12], F32, "graw", ls)
            gates = kb.sb([128, NT, 12], F32, "gates", ls)
            sco = [kb.sb([128, 32], F32, "sco", ls) for _ in range(2)]
            selm = [kb.sb([128, 32], F32, "selm", ls) for _ in range(2)]
            top8 = [kb.sb([128, 8], F32, "top8", ls) for _ in range(2)]
            self.rtmp = kb.sb([128, 8, 32], F32, "rtmp", ls)
            self._ri = 0
            kb.dma("pool", peT.t[:], d["l0_nsa_cmp_pe"][:, :], writes=[peT[0]])
            for j in range(2):
                kb.dma("pool", w2sb.t[:, j, :], d["l0_nsa_cmp_w2"][j], writes=[w2sb[j]])
            kb.dma("pool", cmpmask.t[:], d["c_cmpmask"][:, :], writes=[cmpmask[0]])
            kb.dma("pool", ksaT.t[64:96, :], d["c_e32"][:, :], writes=[ksaT["e"]])
            kb.dma("sp", tA.t[:], d["c_nsaA"][:, :, :], writes=[tA[0]])
            kb.dma("sp", tB.t[:], d["c_nsaB"][:, :, :], writes=[tB[0]])
            kb.op("dve", lambda e: e.memset(vsx.t[:, :, 64:65], 1.0), writes=[vsx[t] for t in range(NT)])
            kb.op("dve", lambda e: e.memset(vwx.t[:, :, 64:65], 1.0), writes=[vwx[t] for t in range(NT)])
            kb.op("dve", lambda e: e.memset(vcx.t[:], 0.0), writes=[vcx[0]])
            kb.dma("pool", vcx.t[:, 64:97], d["c_ovx"][:, :], reads=[], writes=[vcx[0]])
            kb.op("dve", lambda e: e.memset(kcT.t[:], 0.0), writes=[kcT[0]])
            kb.op("dve", lambda e: e.memset(hid.t[:], 0.0), writes=[hid[0], hid[1]])

            def fin_nsa(branch, r, t, g):
                acc_ = acc[0]

                def fin(ob):
                    self._fi = (self._fi + 1) % len(self.fin_s)
                    fs = self.fin_s[self._fi]
                    kb.op("dve", lambda e: e.tensor_scalar(out=fs.t[:, 0:1], in0=ob.t[:, 64:65], scalar1=1e-30, scalar2=None, op0=ALU.max),
                          reads=[ob[0]], writes=[fs[0]])
                    kb.op("dve", lambda e: e.reciprocal(out=fs.t[:, 1:2], in_=fs.t[:, 0:1]), reads=[fs[0]], writes=[fs[1]])
                    gc = branch * 4 + r
                    kb.op("dve", lambda e: e.tensor_tensor(out=fs.t[:, 2:3], in0=fs.t[:, 1:2], in1=gates.t[:, t, gc:gc + 1], op=ALU.mult),
                          reads=[fs[1], gates[0]], writes=[fs[2]])
                    if branch == 0:
                        kb.op("dve", lambda e: e.tensor_scalar(out=acc_.t[:, r, :], in0=ob.t[:, 0:64], scalar1=fs.t[:, 2:3], scalar2=None, op0=ALU.mult),
                              reads=[ob[0], fs[2]], writes=[acc_[r]])
                        if r == 0:
                            kb.op("dve", lambda e: e.tensor_scalar(out=imp.t[:, t, :], in0=ob.t[:, 65:97], scalar1=fs.t[:, 1:2], scalar2=None, op0=ALU.mult),
                                  reads=[ob[0], fs[1]], writes=[imp[t]])
                        else:
                            kb.op("dve", lambda e: e.scalar_tensor_tensor(out=imp.t[:, t, :], in0=ob.t[:, 65:97], scalar=fs.t[:, 1:2], in1=imp.t[:, t, :], op0=ALU.mult, op1=ALU.add),
                                  reads=[ob[0], fs[1], imp[t]], writes=[imp[t]])
                    elif branch == 2:
                        kb.op("dve", lambda e: e.scalar_tensor_tensor(out=acc_.t[:, r, :], in0=ob.t[:, 0:64], scalar=fs.t[:, 2:3], in1=acc_.t[:, r, :], op0=ALU.mult, op1=ALU.add),
                              reads=[ob[0], fs[2], acc_[r]], writes=[acc_[r]])
                    else:
                        kb.op("dve", lambda e: e.scalar_tensor_tensor(out=otok.t[:, t, r * 64:(r + 1) * 64], in0=ob.t[:, 0:64], scalar=fs.t[:, 2:3], in1=acc_.t[:, r, :], op0=ALU.mult, op1=ALU.add),
                              reads=[ob[0], fs[2], acc_[r]], writes=[otok[t]])
                return fin

            for g in range(4):
                wq = self.ws.next(f"n_q{g}")
                wkv = self.ws.next(f"n_kv{g}")
                wqv = wq.t[:, 0:2048].rearrange("p (c n) -> p c n", c=8)
                wkvv = wkv.t[:, 0:8 * 396].rearrange("p (c n) -> p c n", c=8)
                def pe_part(t):
                    bA = self.mbank()
                    self.proj_tok(t, bA.t[:, 0:256], bA, wqv, wq, 256)
                    bB = self.mbank()
                    self.proj_tok(t, bB.t[:, 0:396], bB, wkvv, wkv, 396)
                    return bA, bB

                def mid_part(t, ab):
                    bA, bB = ab
                    stg = stgs[t % 2]
                    self.rope(t, bA.t[:, 0:256].rearrange("p (a b) -> p a b", a=4), bA[0],
                              stg.t[:, 0:256].rearrange("p (a b) -> p a b", a=4), stg[0], 4, 64, 0, 8, self.cosp, self.sinp)
                    self.rope(t, bB.t[:, 0:384].rearrange("p (a b) -> p a b", a=3)[:, :, 0:64], bB[0],
                              stg.t[:, 256:640].rearrange("p (a b) -> p a b", a=3)[:, :, 0:64], stg[0], 3, 64, 0, 8, self.cosp, self.sinp)
                    kb.op("dve", lambda e, bB=bB: e.tensor_copy(out=stg.t[:, 320:384], in_=bB.t[:, 64:128]), reads=[bB[0]], writes=[stg[0]])
                    kb.op("dve", lambda e, bB=bB, t=t: e.tensor_copy(out=vsx.t[:, t, 0:64], in_=bB.t[:, 192:256]), reads=[bB[0]], writes=[vsx[t]])
                    kb.op("dve", lambda e, bB=bB, t=t: e.tensor_copy(out=vwx.t[:, t, 0:64], in_=bB.t[:, 320:384]), reads=[bB[0]], writes=[vwx[t]])
                    kb.op("dve", lambda e, bB=bB, t=t: e.tensor_copy(out=graw.t[:, t, :], in_=bB.t[:, 384:396]), reads=[bB[0]], writes=[graw[0]])
                    return self.transposes([(stg.t[:, 0:128], stg[0]), (stg.t[:, 128:256], stg[0]), (stg.t[:, 256:384], stg[0]),
                                            (stg.t[:, 384:448], stg[0]), (stg.t[:, 512:576], stg[0])])

                def fin_part(t, bp):
                    bT, pT = bp
                    tsl = slice(t * 128, (t + 1) * 128)
                    for par in range(2):
                        kb.op("dve", lambda e, par=par, pT=pT, tsl=tsl: e.tensor_copy(
                            out=qaT.t[0:64, par:4:2, tsl], in_=pT[par * 64:par * 64 + 64, 0:256].rearrange("p (a n) -> p a n", a=2)),
                            reads=[bT[0]], writes=[qaT[(par, t)], qaT[(par + 2, t)]])
                    kb.op("dve", lambda e, pT=pT, tsl=tsl: e.tensor_copy(out=cvT.t[:, tsl], in_=pT[:, 256:384]), reads=[bT[0]], writes=[cvT[t]])
                    kb.op("dve", lambda e, pT=pT, tsl=tsl: e.tensor_copy(out=ksaT.t[0:64, tsl], in_=pT[0:64, 384:512]), reads=[bT[0]], writes=[ksaT[t]])
                    kb.op("dve", lambda e, pT=pT, tsl=tsl: e.tensor_copy(out=kwT.t[0:64, tsl], in_=pT[0:64, 512:640]), reads=[bT[0]], writes=[kwT[t]])
                self.skewed(NT, pe_part, mid_part, fin_part)
                kb.op("act", lambda e: e.activation(out=gates.t[:], in_=graw.t[:], func=AF.Exp, scale=-1.0), reads=[graw[0]], writes=[gates[0]])
                kb.op("dve", lambda e: e.tensor_scalar(out=gates.t[:], in0=gates.t[:], scalar1=1.0, scalar2=None, op0=ALU.add), reads=[gates[0]], writes=[gates[0]])
                kb.op("dve", lambda e: e.reciprocal(out=gates.t[:], in_=gates.t[:]), reads=[gates[0]], writes=[gates[0]])
                w1 = self.ws.next(f"n_w1_{g}")
                w1v = w1.t[:, 0:4096].rearrange("p (l h) -> p l h", l=32)
                cv_all = [cvT[t] for t in range(NT)]
                if g == 0:
                    bank = self.mbank()
                    for j in range(2):
                        for l in range(32):
                            kb.op("pe", lambda e, j=j, l=l, bank=bank: e.matmul(bank.t[:, j:j + 1], lhsT=w1v[j * 64:(j + 1) * 64, l, :], rhs=peT.t[j * 64:(j + 1) * 64, l:l + 1], start=(l == 0), stop=(l == 31)),
                                  reads=[w1[0], peT[0]], writes=[bank[0]])
                    kb.op("dve", lambda e, bank=bank: e.tensor_copy(out=cbias.t[:], in_=bank.t[:, 0:2]), reads=[bank[0]], writes=[cbias[0]])
                for j in range(2):
                    bank = self.mbank()
                    for l in range(32):
                        kb.op("pe", lambda e, j=j, l=l, bank=bank: e.matmul(bank.t[:, 0:127], lhsT=w1v[j * 64:(j + 1) * 64, l, :], rhs=cvT.t[j * 64:(j + 1) * 64, l:l + 2017:16], start=(l == 0), stop=(l == 31)),
                              reads=[w1[0]] + cv_all, writes=[bank[0]])
                    xf, u_ = gx
                    e_ = u_
                    kb.op("dve", lambda e, j=j, bank=bank: e.tensor_scalar(out=xf.t[:, 0:127], in0=bank.t[:, 0:127], scalar1=cbias.t[:, j:j + 1], scalar2=None, op0=ALU.add),
                          reads=[bank[0], cbias[0]], writes=[xf[0]])
                    kb.op("dve", lambda e: e.tensor_tensor(out=u_.t[:, 0:127], in0=xf.t[:, 0:127], in1=xf.t[:, 0:127], op=ALU.mult), reads=[xf[0]], writes=[u_[0]])
                    kb.op("dve", lambda e: e.tensor_scalar(out=u_.t[:, 0:127], in0=u_.t[:, 0:127], scalar1=0.044715, scalar2=1.0, op0=ALU.mult, op1=ALU.add), reads=[u_[0]], writes=[u_[0]])
                    kb.op("dve", lambda e: e.tensor_tensor(out=u_.t[:, 0:127], in0=u_.t[:, 0:127], in1=xf.t[:, 0:127], op=ALU.mult), reads=[u_[0], xf[0]], writes=[u_[0]])
                    kb.op("act", lambda e: e.activation(out=e_.t[:, 0:127], in_=u_.t[:, 0:127], func=AF.Exp, scale=-1.5957691216), reads=[u_[0]], writes=[e_[0]])
                    kb.op("dve", lambda e: e.tensor_scalar(out=e_.t[:, 0:127], in0=e_.t[:, 0:127], scalar1=1.0, scalar2=None, op0=ALU.add), reads=[e_[0]], writes=[e_[0]])
                    kb.op("dve", lambda e: e.reciprocal(out=e_.t[:, 0:127], in_=e_.t[:, 0:127]), reads=[e_[0]], writes=[e_[0]])
                    kb.op("dve", lambda e, j=j: e.tensor_tensor(out=hid.t[:, j, 0:127], in0=xf.t[:, 0:127], in1=e_.t[:, 0:127], op=ALU.mult), reads=[xf[0], e_[0]], writes=[hid[j]])
                bank = self.mbank()
                kb.op("pe", lambda e, bank=bank: e.matmul(bank.t[0:64, 0:127], lhsT=w2sb.t[:, 0, :], rhs=hid.t[:, 0, 0:127], start=True, stop=True),
                      reads=[w2sb[0], hid[0]], writes=[bank[0]])
                kb.op("dve", lambda e, bank=bank: e.tensor_copy(out=kcT.t[0:64, 0:127], in_=bank.t[0:64, 0:127]), reads=[bank[0]], writes=[kcT[0]])
                bank = self.mbank()
                kb.op("pe", lambda e, bank=bank: e.matmul(bank.t[0:127, 0:64], lhsT=hid.t[:, 1, 0:127], rhs=w2sb.t[:, 1, :], start=True, stop=True),
                      reads=[w2sb[1], hid[1]], writes=[bank[0]])
                kb.op("dve", lambda e, bank=bank: e.tensor_copy(out=vcx.t[0:127, 0:64], in_=bank.t[0:127, 0:64]), reads=[bank[0]], writes=[vcx[0]])
                self.attn_small = True
                self.mbanks = [self.banks[5]]
                self._mi = 0
                for t in range(NT):
                    tsl = slice(t * 128, (t + 1) * 128)
                    jobs = []
                    for r in range(4):
                        jobs.append(dict(q=[(qaT.t[0:64, r, tsl], qaT[(r, t)])],
                                         kt=[dict(k=[(kcT.t[0:64, :], kcT[0])], v=(vcx.t[:, :], vcx[0]), mask=(cmpmask.t[:, tsl], cmpmask[0]))],
                                         nv=97, scale=0.125, fin=fin_nsa(0, r, t, g)))
                    self.attend(jobs)
                    sc_ = sco[t % 2]
                    sm_ = selm[t % 2]
                    tp_ = top8[t % 2]
                    kb.op("dve", lambda e, t=t, sc_=sc_: e.tensor_tensor(out=sc_.t[:], in0=imp.t[:, t, :], in1=tA.t[:, t, :], op=ALU.mult), reads=[imp[t], tA[0]], writes=[sc_[0]])
                    kb.op("dve", lambda e, t=t, sc_=sc_: e.tensor_tensor(out=sc_.t[:], in0=sc_.t[:], in1=tB.t[:, t, :], op=ALU.add), reads=[sc_[0], tB[0]], writes=[sc_[0]])
                    kb.op("dve", lambda e, sc_=sc_, tp_=tp_: e.max(out=tp_.t[:], in_=sc_.t[:]), reads=[sc_[0]], writes=[tp_[0]])
                    kb.op("dve", lambda e, sc_=sc_, tp_=tp_, sm_=sm_: e.tensor_scalar(out=sm_.t[:], in0=sc_.t[:], scalar1=tp_.t[:, 7:8], scalar2=None, op0=ALU.is_ge),
                          reads=[sc_[0], tp_[0]], writes=[sm_[0]])
                    kb.op("dve", lambda e, t=t, sm_=sm_: e.tensor_scalar(out=nmt.t[:, t, :], in0=sm_.t[:], scalar1=-1.0, scalar2=-NEG, op0=ALU.add, op1=ALU.mult),
                          reads=[sm_[0]], writes=[nmt[t]])
                    jobs = []
                    for r in range(4):
                        kts = self.causal_kts(t, lambda kt: [(kwT.t[0:64, kt * 128:(kt + 1) * 128], kwT[kt])], lambda kt: (vwx.t[:, kt, :], vwx[kt]),
                                              lo=max(0, t - 4), far_at=t - 4)
                        jobs.append(dict(q=[(qaT.t[0:64, r, tsl], qaT[(r, t)])], kt=kts, nv=65, scale=0.125, fin=fin_nsa(2, r, t, g)))
                    self.attend(jobs)
                    bT, pT = self.transposes([(nmt.t[:, t, :], nmt[t])])
                    kb.op("dve", lambda e, pT=pT, tsl=tsl: e.tensor_copy(out=qaT.t[64:96, :, tsl], in_=pT[0:32, 0:128].unsqueeze(1).broadcast_to([32, 4, 128])),
                          reads=[bT[0]], writes=[qaT[("m", t)]])
                    jobs = []
                    for r in range(4):
                        kts = self.causal_kts(t, lambda kt: [(ksaT.t[0:96, kt * 128:(kt + 1) * 128], [ksaT[kt], ksaT["e"]])], lambda kt: (vsx.t[:, kt, :], vsx[kt]))
                        jobs.append(dict(q=[(qaT.t[0:96, r, tsl], [qaT[(r, t)], qaT[("m", t)]])], kt=kts, nv=65, scale=0.125, fin=fin_nsa(1, r, t, g)))
                    self.attend(jobs)
                self.attn_small = False
                self.mbanks = self.banks[0:8]
                self.pass_out(otok, ot, f"n_o{g}")
        kb.barrier()


def _layout_weight(name, arr):
    a = np.asarray(arr)
    if name == "l0_nsa_cmp_pe":
        return np.ascontiguousarray(np.concatenate([a[0].T, a[1].T], axis=0)).astype(np.float32)
    if name.endswith("_moe_router"):
        return np.ascontiguousarray(a.T)
    shp = WSHAPES[name]
    return np.ascontiguousarray(a.reshape(shp))


_PROG_CACHE = {}


def _get_prog(plan_key, plan, dbg=None):
    p = _PROG_CACHE.get(plan_key)
    if p is None:
        p = Prog(plan, dbg)
        p.build()
        _PROG_CACHE[plan_key] = p
    return p


def run_plan(inputs, plan=None, cores=8, dbg=None, trace=False):
    key = (None if plan is None else tuple(plan), dbg)
    prog = _get_prog(key, plan, dbg)
    shared = dict(prog.consts)
    for k in prog.used:
        shared[k] = _layout_weight(k, inputs[k])
    x = np.asarray(inputs["x"], dtype=np.float32)
    mem = np.asarray(inputs["mem"], dtype=np.float32)
    pos = np.asarray(inputs["positions"]).astype(np.int32)
    in_maps = []
    for b in range(cores):
        m = dict(shared)
        m["x"] = np.ascontiguousarray(x[b])
        m["mem"] = np.ascontiguousarray(mem[b])
        m["posT"] = np.ascontiguousarray(pos[b].reshape(NT, 128).T)
        in_maps.append(m)
    res = run_bass_kernel_spmd(prog.nc, in_maps, core_ids=list(range(cores)), trace=trace)
    out = np.stack([np.asarray(r["out"]) for r in res.results], axis=0)
    return out, res


def kernel(**inputs):
    out, _ = run_plan(inputs, None, cores=8)
    return out.astype(np.float32)
```
